# Optimizing a Trainium2 kernel written in Bass

```python
import numpy as np
import jax
import jax.numpy as jnp
from jax import lax

D_MODEL = 1024
BATCH = 32
SEQ = 2048
DEPTH = 4

N_MIXERS = 3
N_NSA_LAYERS = (DEPTH + 2) // 3
N_MLSTM_LAYERS = (DEPTH + 1) // 3
N_HGRN_LAYERS = DEPTH // 3

NSA_HEADS = 16
NSA_KV_GROUPS = 4
NSA_HEAD_DIM = D_MODEL // NSA_HEADS
NSA_HPG = NSA_HEADS // NSA_KV_GROUPS
NSA_KV_WIDTH = NSA_KV_GROUPS * NSA_HEAD_DIM
NSA_IN = D_MODEL + 6 * NSA_KV_WIDTH + 3 * NSA_HEADS
CMP_BLOCK = 32
CMP_STRIDE = 16
CMP_HIDDEN = 4 * NSA_HEAD_DIM
SEL_BLOCK = 64
SEL_TOPK = 8
WINDOW = 512
NSA_QCHUNK = 32

MLSTM_HEADS = 4
MLSTM_QK_DIM = D_MODEL // (2 * MLSTM_HEADS)
MLSTM_V_DIM = D_MODEL // MLSTM_HEADS
MLSTM_QK_WIDTH = 2 * MLSTM_HEADS * MLSTM_QK_DIM
MLSTM_IN = MLSTM_QK_WIDTH + MLSTM_HEADS * MLSTM_V_DIM + D_MODEL + 2 * MLSTM_HEADS
MLSTM_CONV = 4
MLSTM_CHUNK = 64

HGRN_HEADS = 8
HGRN_DIM = D_MODEL // HGRN_HEADS
HGRN_CHUNK = 32

N_EXPERTS = 32
TOP_K = 4
D_FF = D_MODEL
SWIGLU_LIMIT = 7.0
SWIGLU_ALPHA = 1.702
MOE_BLOCK = 256

LN_EPS = 1e-5
RMS_EPS = 1e-6
DEEPNORM_ALPHA = (2 * DEPTH) ** 0.25
DEEPNORM_BETA = (8 * DEPTH) ** -0.25
NEG = -1e30

kernel_name = 'hybrid_nsa_mlstm_hgrn2_moe'


def layer_norm(x, g, b):
    xf = x.astype(jnp.float32)
    mu = jnp.mean(xf, axis=-1, keepdims=True)
    var = jnp.mean(jnp.square(xf - mu), axis=-1, keepdims=True)
    return ((xf - mu) * lax.rsqrt(var + LN_EPS)).astype(x.dtype) * g + b


def head_rms_norm(h, g):
    hf = h.astype(jnp.float32)
    hf = hf * lax.rsqrt(jnp.mean(jnp.square(hf), axis=-1, keepdims=True) + RMS_EPS)
    return hf.reshape(*h.shape[:-2], -1).astype(g.dtype) * g


def masked_softmax(s, mask):
    p = jax.nn.softmax(jnp.where(mask, s.astype(jnp.float32), NEG), axis=-1)
    return p * mask


def causal_depthwise_conv(x, w, b):
    width, ch = w.shape
    y = lax.conv_general_dilated(x, w[:, None, :], window_strides=(1,), padding=[(width - 1, 0)],
                                 dimension_numbers=('NWC', 'WIO', 'NWC'), feature_group_count=ch)
    return y + b


def nsa_mixer(x, w_in, cmp_pos, cmp_w1, cmp_b1, cmp_w2, cmp_b2, gate_b, w_out):
    B, T, _ = x.shape
    G, HPG, dh, QC = NSA_KV_GROUPS, NSA_HPG, NSA_HEAD_DIM, NSA_QCHUNK
    proj = x @ w_in
    splits = [D_MODEL + n * NSA_KV_WIDTH for n in range(7)]
    q, kc, vc, ks, vs, kw, vw, gates = jnp.split(proj, splits, axis=-1)
    q = q.reshape(B, T, G, HPG, dh).transpose(0, 2, 3, 1, 4) * dh ** -0.5

    def groups(a):
        return a.reshape(B, T, G, dh).transpose(0, 2, 1, 3)

    kc, vc, ks, vs, kw, vw = (groups(a) for a in (kc, vc, ks, vs, kw, vw))
    gates = jax.nn.sigmoid(gates + gate_b).reshape(B, T, G, HPG, 3).transpose(0, 2, 3, 1, 4)

    n_cmp = (T - CMP_BLOCK) // CMP_STRIDE + 1
    cmp_start = np.arange(n_cmp) * CMP_STRIDE
    cmp_idx = cmp_start[:, None] + np.arange(CMP_BLOCK)[None, :]

    def compress(a, j):
        blocks = (a[:, :, cmp_idx] + cmp_pos[j]).reshape(B, G, n_cmp, CMP_BLOCK * dh)
        return jax.nn.gelu(blocks @ cmp_w1[j] + cmp_b1[j]) @ cmp_w2[j] + cmp_b2[j]

    k_cmp, v_cmp = compress(kc, 0), compress(vc, 1)
    cmp_end = jnp.asarray(cmp_start + CMP_BLOCK - 1, jnp.int32)

    n_sel = T // SEL_BLOCK
    topk = min(SEL_TOPK, n_sel)
    sel_start = np.arange(n_sel) * SEL_BLOCK
    overlap = (np.minimum(cmp_start[:, None] + CMP_BLOCK, sel_start[None, :] + SEL_BLOCK)
               - np.maximum(cmp_start[:, None], sel_start[None, :]))
    cmp_to_sel = jnp.asarray((np.clip(overlap, 0, None) / CMP_STRIDE).astype(np.float32))
    ks_blk = ks.reshape(B, G, n_sel, SEL_BLOCK, dh)
    vs_blk = vs.reshape(B, G, n_sel, SEL_BLOCK, dh)

    kw_pad = jnp.pad(kw, ((0, 0), (0, 0), (WINDOW, 0), (0, 0)))
    vw_pad = jnp.pad(vw, ((0, 0), (0, 0), (WINDOW, 0), (0, 0)))

    b_ix = jnp.arange(B)[:, None, None, None]
    g_ix = jnp.arange(G)[None, :, None, None]
    blk = jnp.arange(n_sel)
    win_off = jnp.arange(WINDOW + QC)

    def chunk(c):
        t0 = c * QC
        t = t0 + jnp.arange(QC)
        qc = lax.dynamic_slice_in_dim(q, t0, QC, axis=3)
        gc = lax.dynamic_slice_in_dim(gates, t0, QC, axis=3)
        p_cmp = masked_softmax(jnp.einsum('bghqd,bgcd->bghqc', qc, k_cmp), cmp_end[None, :] <= t[:, None])
        o_cmp = jnp.einsum('bghqc,bgcd->bghqd', p_cmp.astype(v_cmp.dtype), v_cmp)
        imp = jnp.einsum('bghqc,cs->bgqs', p_cmp, cmp_to_sel)
        cur = (t // SEL_BLOCK)[:, None]
        forced = (blk == 0) | (blk == cur) | (blk == cur - 1)
        score = jnp.where(forced, -NEG, jnp.where(blk <= cur, imp, NEG))
        top_score, top_idx = lax.top_k(score, topk)
        k_sel = ks_blk[b_ix, g_ix, top_idx]
        v_sel = vs_blk[b_ix, g_ix, top_idx]
        key_pos = top_idx[..., None] * SEL_BLOCK + jnp.arange(SEL_BLOCK)
        m_sel = (key_pos <= t[:, None, None]) & (top_score > 0.5 * NEG)[..., None]
        s_sel = jnp.einsum('bghqd,bgqnld->bghqnl', qc, k_sel).reshape(B, G, HPG, QC, topk * SEL_BLOCK)
        p_sel = masked_softmax(s_sel, m_sel.reshape(B, G, 1, QC, topk * SEL_BLOCK))
        o_sel = jnp.einsum('bghqnl,bgqnld->bghqd',
                           p_sel.reshape(B, G, HPG, QC, topk, SEL_BLOCK).astype(v_sel.dtype), v_sel)
        k_win = lax.dynamic_slice_in_dim(kw_pad, t0, WINDOW + QC, axis=2)
        v_win = lax.dynamic_slice_in_dim(vw_pad, t0, WINDOW + QC, axis=2)
        win_pos = t0 - WINDOW + win_off
        lag = t[:, None] - win_pos[None, :]
        m_win = (lag >= 0) & (lag < WINDOW) & (win_pos >= 0)[None, :]
        p_win = masked_softmax(jnp.einsum('bghqd,bgkd->bghqk', qc, k_win), m_win)
        o_win = jnp.einsum('bghqk,bgkd->bghqd', p_win.astype(v_win.dtype), v_win)
        return gc[..., 0:1] * o_cmp + gc[..., 1:2] * o_sel + gc[..., 2:3] * o_win

    o = lax.map(chunk, jnp.arange(T // QC))
    o = o.transpose(1, 0, 4, 2, 3, 5).reshape(B, T, D_MODEL)
    return o @ w_out


def mlstm_mixer(x, w_in, conv_w, conv_b, gate_b, norm_g, w_out):
    B, T, _ = x.shape
    H, dk, dv, L = MLSTM_HEADS, MLSTM_QK_DIM, MLSTM_V_DIM, MLSTM_CHUNK
    nc = T // L
    f32 = jnp.float32
    qk, v, o_gate, gif = jnp.split(
        x @ w_in, [MLSTM_QK_WIDTH, MLSTM_QK_WIDTH + H * dv, MLSTM_QK_WIDTH + H * dv + D_MODEL], axis=-1)
    q, k = jnp.split(jax.nn.silu(causal_depthwise_conv(qk, conv_w, conv_b)), 2, axis=-1)

    def heads(a, d):
        return a.reshape(B, T, H, d).transpose(0, 2, 1, 3).astype(f32)

    q, k, v = heads(q, dk), heads(k, dk) * dk ** -0.5, heads(v, dv)
    gif = (gif + gate_b).astype(f32).transpose(0, 2, 1)
    log_i, log_f = gif[:, :H], jax.nn.log_sigmoid(gif[:, H:])

    def chunks(a):
        return jnp.moveaxis(a.reshape(B, H, nc, L, *a.shape[3:]), 2, 0)

    causal = jnp.tril(jnp.ones((L, L), dtype=bool))

    def step(carry, inp):
        C, n, m = carry
        qc, kc, vc, li, lf = inp
        b = jnp.cumsum(lf, axis=-1)
        d_mat = jnp.where(causal, b[..., :, None] - b[..., None, :] + li[..., None, :], NEG)
        m_t = jnp.maximum(b + m[..., None], jnp.max(d_mat, axis=-1))
        w_inter = jnp.exp(b + m[..., None] - m_t)
        s = jnp.einsum('bhtd,bhsd->bhts', qc, kc) * jnp.exp(d_mat - m_t[..., None])
        num = jnp.einsum('bhts,bhsv->bhtv', s, vc) + w_inter[..., None] * jnp.einsum('bhtk,bhvk->bhtv', qc, C)
        den = jnp.sum(s, axis=-1) + w_inter * jnp.einsum('bhtk,bhk->bht', qc, n)
        h = num / jnp.maximum(jnp.abs(den), jnp.exp(-m_t))[..., None]
        b_end = b[..., -1]
        decay = b_end[..., None] - b + li
        m_new = jnp.maximum(b_end + m, jnp.max(decay, axis=-1))
        w_k = jnp.exp(decay - m_new[..., None])
        carry_scale = jnp.exp(b_end + m - m_new)
        C_new = carry_scale[..., None, None] * C + jnp.einsum('bhsv,bhsk->bhvk', vc * w_k[..., None], kc)
        n_new = carry_scale[..., None] * n + jnp.einsum('bhs,bhsk->bhk', w_k, kc)
        return (C_new, n_new, m_new), h

    init = (jnp.zeros((B, H, dv, dk), f32), jnp.zeros((B, H, dk), f32), jnp.zeros((B, H), f32))
    _, h = lax.scan(step, init, (chunks(q), chunks(k), chunks(v), chunks(log_i), chunks(log_f)))
    h = jnp.moveaxis(h, 0, 2).reshape(B, H, T, dv).transpose(0, 2, 1, 3)
    h = head_rms_norm(h, norm_g) * jax.nn.sigmoid(o_gate)
    return h @ w_out


def hgrn2_mixer(x, layer_idx, w_in, lower_table, norm_g, w_out):
    B, T, _ = x.shape
    H, d, L = HGRN_HEADS, HGRN_DIM, HGRN_CHUNK
    nc = T // L
    f32 = jnp.float32
    q, f, i, g = jnp.split(x @ w_in, 4, axis=-1)
    lb_soft = jax.nn.softmax(lower_table.astype(f32), axis=0)
    lb = jnp.cumsum(lb_soft, axis=0)[layer_idx] - lb_soft[0]
    f = lb + (1.0 - lb) * jax.nn.sigmoid(f.astype(f32))

    def chunked(a):
        return a.astype(f32).reshape(B, nc, L, H, d).transpose(1, 0, 3, 2, 4)

    xs = (chunked(jax.nn.silu(q)), chunked(1.0 - f), chunked(i), chunked(jnp.log(f)))
    causal = jnp.tril(jnp.ones((L, L), dtype=bool))

    def step(S, inp):
        qc, kc, vc, lf = inp
        G = jnp.cumsum(lf, axis=2)
        q_dec = qc * jnp.exp(G)
        a_mat = jnp.where(causal, jnp.einsum('bhtk,bhsk->bhts', q_dec, kc * jnp.exp(-G)), 0.0)
        o = jnp.einsum('bhts,bhsv->bhtv', a_mat, vc) + jnp.einsum('bhtk,bhkv->bhtv', q_dec, S)
        G_end = G[:, :, -1]
        S_new = jnp.exp(G_end)[..., None] * S + jnp.einsum('bhsk,bhsv->bhkv', kc * jnp.exp(G_end[:, :, None] - G), vc)
        return S_new, o

    _, o = lax.scan(step, jnp.zeros((B, H, d, d), f32), xs)
    o = o.transpose(1, 0, 3, 2, 4).reshape(B, T, H, d)
    o = head_rms_norm(o, norm_g) * jax.nn.sigmoid(g)
    return o @ w_out


def moe_ffn(x, router_w, router_b, w_gu, b_gu, w_down, b_down):
    B, T, D = x.shape
    N = B * T
    A = N * TOP_K
    xf = x.reshape(N, D)
    logits = xf.astype(jnp.float32) @ router_w.astype(jnp.float32) + router_b.astype(jnp.float32)
    top_val, top_idx = lax.top_k(logits, TOP_K)
    gate = jax.nn.softmax(top_val, axis=-1)
    e_flat = top_idx.reshape(A)
    order = jnp.argsort(e_flat)
    e_sorted = e_flat[order]
    counts = jnp.bincount(e_flat, length=N_EXPERTS)
    padded = (counts + MOE_BLOCK - 1) // MOE_BLOCK * MOE_BLOCK
    start = jnp.cumsum(counts) - counts
    padded_end = jnp.cumsum(padded)
    padded_start = padded_end - padded
    dest = padded_start[e_sorted] + jnp.arange(A) - start[e_sorted]
    n_blocks = (A + N_EXPERTS * (MOE_BLOCK - 1) + MOE_BLOCK - 1) // MOE_BLOCK
    rows = n_blocks * MOE_BLOCK
    row_tok = jnp.full((rows,), N, jnp.int32).at[dest].set((order // TOP_K).astype(jnp.int32))
    row_gate = jnp.zeros((rows,), jnp.float32).at[dest].set(gate.reshape(A)[order])
    block_expert = jnp.minimum(
        jnp.searchsorted(padded_end, jnp.arange(n_blocks) * MOE_BLOCK, side='right'), N_EXPERTS - 1)
    x_pad = jnp.concatenate([xf, jnp.zeros((1, D), xf.dtype)], axis=0)

    def expert_block(args):
        e, tok, gw = args
        xb = x_pad[tok]
        h_gate, h_up = jnp.split(xb @ w_gu[e] + b_gu[e], 2, axis=-1)
        h_gate = jnp.minimum(h_gate, SWIGLU_LIMIT)
        h_up = jnp.clip(h_up, -SWIGLU_LIMIT, SWIGLU_LIMIT)
        h = (h_up + 1.0) * h_gate * jax.nn.sigmoid(SWIGLU_ALPHA * h_gate)
        return (h @ w_down[e] + b_down[e]) * gw[:, None].astype(xb.dtype)

    y = lax.map(expert_block, (block_expert, row_tok.reshape(n_blocks, MOE_BLOCK),
                               row_gate.reshape(n_blocks, MOE_BLOCK)))
    out = jax.ops.segment_sum(y.reshape(rows, D), row_tok, num_segments=N + 1)
    return out[:N].reshape(B, T, D)


def setup_inputs(seed: int = 0) -> dict:
    key = jax.random.key(seed)
    keys = iter(jax.random.split(key, 32))

    def rnd(shape, scale):
        return jax.random.normal(next(keys), shape, jnp.float32) * scale

    D, E, F = D_MODEL, N_EXPERTS, D_FF
    dh, H = NSA_HEAD_DIM, MLSTM_HEADS
    inv = D ** -0.5
    return {
        'x': rnd((BATCH, SEQ, D), 1.0),
        'ln_g': 1.0 + rnd((DEPTH, 2, D), 0.02),
        'ln_b': rnd((DEPTH, 2, D), 0.02),
        'nsa_w_in': rnd((N_NSA_LAYERS, D, NSA_IN), inv),
        'nsa_cmp_pos': rnd((N_NSA_LAYERS, 2, CMP_BLOCK, dh), 0.1),
        'nsa_cmp_w1': rnd((N_NSA_LAYERS, 2, CMP_BLOCK * dh, CMP_HIDDEN), (CMP_BLOCK * dh) ** -0.5),
        'nsa_cmp_b1': rnd((N_NSA_LAYERS, 2, CMP_HIDDEN), 0.02),
        'nsa_cmp_w2': rnd((N_NSA_LAYERS, 2, CMP_HIDDEN, dh), CMP_HIDDEN ** -0.5),
        'nsa_cmp_b2': rnd((N_NSA_LAYERS, 2, dh), 0.02),
        'nsa_gate_b': rnd((N_NSA_LAYERS, 3 * NSA_HEADS), 0.02),
        'nsa_w_out': rnd((N_NSA_LAYERS, D, D), inv * DEEPNORM_BETA),
        'ml_w_in': rnd((N_MLSTM_LAYERS, D, MLSTM_IN), inv),
        'ml_conv_w': rnd((N_MLSTM_LAYERS, MLSTM_CONV, MLSTM_QK_WIDTH), MLSTM_CONV ** -0.5),
        'ml_conv_b': rnd((N_MLSTM_LAYERS, MLSTM_QK_WIDTH), 0.02),
        'ml_gate_b': jnp.concatenate([rnd((N_MLSTM_LAYERS, H), 0.1),
                                      3.0 + rnd((N_MLSTM_LAYERS, H), 0.1)], axis=-1),
        'ml_norm_g': 1.0 + rnd((N_MLSTM_LAYERS, H * MLSTM_V_DIM), 0.02),
        'ml_w_out': rnd((N_MLSTM_LAYERS, D, D), inv * DEEPNORM_BETA),
        'hg_w_in': rnd((N_HGRN_LAYERS, D, 4 * D), inv),
        'hg_lower': rnd((DEPTH, D), 0.1),
        'hg_norm_g': 1.0 + rnd((N_HGRN_LAYERS, D), 0.02),
        'hg_w_out': rnd((N_HGRN_LAYERS, D, D), inv * DEEPNORM_BETA),
        'router_w': rnd((DEPTH, D, E), inv),
        'router_b': rnd((DEPTH, E), 0.01),
        'moe_w_gu': rnd((DEPTH, E, D, 2 * F), inv),
        'moe_b_gu': rnd((DEPTH, E, 2 * F), 0.02),
        'moe_w_down': rnd((DEPTH, E, F, D), F ** -0.5 * DEEPNORM_BETA),
        'moe_b_down': rnd((DEPTH, E, D), 0.02),
    }


def reference(x, ln_g, ln_b, nsa_w_in, nsa_cmp_pos, nsa_cmp_w1, nsa_cmp_b1, nsa_cmp_w2, nsa_cmp_b2,
              nsa_gate_b, nsa_w_out, ml_w_in, ml_conv_w, ml_conv_b, ml_gate_b, ml_norm_g, ml_w_out,
              hg_w_in, hg_lower, hg_norm_g, hg_w_out, router_w, router_b, moe_w_gu, moe_b_gu,
              moe_w_down, moe_b_down):
    for layer in range(DEPTH):
        kind, slot = layer % N_MIXERS, layer // N_MIXERS
        if kind == 0:
            h = nsa_mixer(x, nsa_w_in[slot], nsa_cmp_pos[slot], nsa_cmp_w1[slot], nsa_cmp_b1[slot],
                          nsa_cmp_w2[slot], nsa_cmp_b2[slot], nsa_gate_b[slot], nsa_w_out[slot])
        elif kind == 1:
            h = mlstm_mixer(x, ml_w_in[slot], ml_conv_w[slot], ml_conv_b[slot], ml_gate_b[slot],
                            ml_norm_g[slot], ml_w_out[slot])
        else:
            h = hgrn2_mixer(x, layer, hg_w_in[slot], hg_lower, hg_norm_g[slot], hg_w_out[slot])
        x = layer_norm(DEEPNORM_ALPHA * x + h, ln_g[layer, 0], ln_b[layer, 0])
        h = moe_ffn(x, router_w[layer], router_b[layer], moe_w_gu[layer], moe_b_gu[layer],
                    moe_w_down[layer], moe_b_down[layer])
        x = layer_norm(DEEPNORM_ALPHA * x + h, ln_g[layer, 1], ln_b[layer, 1])
    return x
```

```python
from contextlib import ExitStack
import numpy as np
import concourse.bass as bass
import concourse.mybir as mybir
from concourse.bass_utils import run_bass_kernel_spmd

F32 = mybir.dt.float32
BF16 = mybir.dt.bfloat16
I32 = mybir.dt.int32
AF = mybir.ActivationFunctionType
ALU = mybir.AluOpType
AX = mybir.AxisListType

COMPUTE = ("pe", "dve", "act", "pool")
EPOCH = 24000
DMA_SLOTS = 12


class FW:
    def __init__(self, nc, es):
        self.nc = nc
        self.es = es
        self.e = {"pe": nc.tensor, "dve": nc.vector, "act": nc.scalar, "pool": nc.gpsimd, "sp": nc.sync}
        self.csem = {k: [] for k in COMPUTE}
        self.ccnt = {k: 0 for k in COMPUTE}
        self.dsem = {}
        self.dn = {}
        self.seen = {k: {} for k in self.e}
        self.state = {}
        self.nsem = 0
        self.ninst = 0
        self.persistent = set()
        for k in COMPUTE:
            self._new_epoch(k)

    def _sem(self, name):
        self.nsem += 1
        return self.es.enter_context(self.nc.semaphore(name))

    def _new_epoch(self, k):
        self.csem[k].append(self._sem(f"c_{k}_{len(self.csem[k])}"))
        self.ccnt[k] = 0

    def _deps(self, reads, writes):
        deps = []
        for key in reads:
            name, tag = key
            st = self.state.get(name)
            if not st:
                continue
            tags = list(st.keys()) if tag is None else [tag, None]
            for t in tags:
                s = st.get(t)
                if s and s[0] is not None:
                    deps.append(s[0])
        for key in writes:
            name, tag = key
            st = self.state.get(name)
            if not st:
                continue
            tags = list(st.keys()) if tag is None else [tag, None]
            for t in tags:
                s = st.get(t)
                if s:
                    if s[0] is not None:
                        deps.append(s[0])
                    deps.extend(s[1].values())
        return deps

    def _record(self, reads, writes, tok):
        for name, tag in reads:
            st = self.state.setdefault(name, {})
            s = st.setdefault(tag, [None, {}])
            s[1][tok[0]] = tok
        for name, tag in writes:
            st = self.state.setdefault(name, {})
            if tag is None:
                st.clear()
            st[tag] = [tok, {}]

    def _wait(self, stream, deps, skip_self=None):
        seen = self.seen[stream]
        need = {}
        for (semkey, val, sem) in deps:
            if skip_self is not None and semkey[0] == "c" and semkey[1] == skip_self:
                continue
            if seen.get(semkey, 0) >= val:
                continue
            if semkey[0] == "c":
                if any(k2[0] == "c" and k2[1] == semkey[1] and k2[2] > semkey[2] for k2 in seen):
                    continue
            if need.get(semkey, (0, None))[0] < val:
                need[semkey] = (val, sem)
        for semkey, (val, sem) in need.items():
            self.e[stream].wait_ge(sem, val)
            seen[semkey] = val
            self.ninst += 1

    def op(self, eng, emit, reads=(), writes=(), inc=True):
        deps = self._deps(reads, writes)
        self._wait(eng, deps, skip_self="pe" if eng == "pe" else None)
        ins = emit()
        self.ninst += 1
        ep = len(self.csem[eng]) - 1
        if inc:
            self.ccnt[eng] += 1
            ins.then_inc(self.csem[eng][ep], 1)
            tok = (("c", eng, ep), self.ccnt[eng], self.csem[eng][ep])
            if self.ccnt[eng] >= EPOCH:
                self._new_epoch(eng)
        else:
            tok = (("c", eng, ep), self.ccnt[eng] + 1, self.csem[eng][ep])
        self._record(reads, writes, tok)
        return ins

    def dma(self, q, emit, reads=(), writes=()):
        if q not in self.dsem:
            self.dsem[q] = [self._sem(f"d_{q}_{i}") for i in range(DMA_SLOTS)]
            self.dn[q] = 0
        i = self.dn[q]
        slot = i % DMA_SLOTS
        sem = self.dsem[q][slot]
        deps = self._deps(reads, writes)
        if i >= DMA_SLOTS:
            deps.append((("d", q, slot), 16 * (i // DMA_SLOTS), sem))
        self._wait(q, deps)
        ins = emit(self.e[q])
        ins.then_inc(sem, 16)
        self.ninst += 1
        self.dn[q] = i + 1
        tok = (("d", q, slot), 16 * (i // DMA_SLOTS + 1), sem)
        self._record(reads, writes, tok)
        return ins

    def barrier(self):
        deps = []
        for k in COMPUTE:
            ep = len(self.csem[k]) - 1
            if self.ccnt[k] > 0:
                deps.append((("c", k, ep), self.ccnt[k], self.csem[k][ep]))
            elif ep > 0:
                deps.append((("c", k, ep - 1), EPOCH, self.csem[k][ep - 1]))
        for q, n in self.dn.items():
            for slot in range(min(n, DMA_SLOTS)):
                last = ((n - 1 - slot) // DMA_SLOTS) * DMA_SLOTS + slot
                deps.append((("d", q, slot), 16 * (last // DMA_SLOTS + 1), self.dsem[q][slot]))
        for stream in self.e:
            self._wait(stream, deps)
        self.state = {k: v for k, v in self.state.items() if k in self.persistent}

    def finish(self, out_names):
        deps = []
        for name in out_names:
            st = self.state.get(name, {})
            for s in st.values():
                if s[0] is not None:
                    deps.append(s[0])
        self._wait("sp", deps)


def K(t, tag=None):
    return (t if isinstance(t, str) else t.name, tag)


D = 1024
NE = 32
ALPHA = 8 ** 0.25
LN_EPS = 1e-5


def moe_nb(NT, B):
    return (4 * NT + NE * (B - 1) + B - 1) // B


class G:
    pass


_UID = [0]


def sbt(g, es, name, shape, dt):
    _UID[0] += 1
    return es.enter_context(g.nc.sbuf_tensor(f"{name}_{_UID[0]}", shape, dt))


def layer_norm_tile(g, xt, lng, lnb, out, tmpname, es, rows=128):
    nc, fw = g.nc, g.fw
    st = g.ln_st
    mv = g.ln_mv
    R = slice(0, rows)
    for c in range(2):
        fw.op("dve", lambda: nc.vector.bn_stats(out=st[R, c, :], in_=xt[R, c * 512:(c + 1) * 512]),
              reads=[K(xt)], writes=[K(st, c)])
    fw.op("dve", lambda: nc.vector.bn_aggr(out=mv[R, 0:2], in_=st[R]), reads=[K(st)], writes=[K(mv, 0)])
    fw.op("dve", lambda: nc.vector.tensor_scalar_add(out=mv[R, 3:4], in0=mv[R, 1:2], scalar1=LN_EPS),
          reads=[K(mv, 0)], writes=[K(mv, 2)])
    fw.op("act", lambda: nc.scalar.sqrt(out=mv[R, 3:4], in_=mv[R, 3:4]), reads=[K(mv, 2)], writes=[K(mv, 2)])
    fw.op("dve", lambda: nc.vector.reciprocal(out=mv[R, 2:3], in_=mv[R, 3:4]), reads=[K(mv, 2)], writes=[K(mv, 1)])
    fw.op("dve", lambda: nc.vector.tensor_scalar(out=xt[R], in0=xt[R], scalar1=mv[R, 0:1], scalar2=mv[R, 2:3],
                                                 op0=ALU.subtract, op1=ALU.mult),
          reads=[K(xt), K(mv, 0), K(mv, 1)], writes=[K(xt)])
    fw.op("pool", lambda: nc.gpsimd.tensor_tensor(out=xt[R], in0=xt[R], in1=lng[R], op=ALU.mult),
          reads=[K(xt), K(lng)], writes=[K(xt)])
    fw.op("pool", lambda: nc.gpsimd.tensor_tensor(out=out[R], in0=xt[R], in1=lnb[R], op=ALU.add),
          reads=[K(xt), K(lnb)], writes=[K(out)])


def phase_route(g, l, X1, X1name, keep):
    nc, fw, NT, B = g.nc, g.fw, g.NT, g.B
    NTT = NT // 128
    NB = moe_nb(NT, B)
    ps = g.ps
    POS4i, G4, EB = keep["POS4i"], keep["G4"], keep["EB"]
    with ExitStack() as es:
        L = sbt(g, es, "rL", [128, NTT, 32], F32)
        RW = sbt(g, es, "rW", [128, 8, 32], F32)
        RB = sbt(g, es, "rB", [128, 32], F32)
        fw.dma("sp", lambda e: e.dma_start(out=RW[:], in_=g.w["router_w"][l].rearrange("(kc p) e -> p kc e", p=128)),
               writes=[K(RW)])
        fw.dma("sp", lambda e: e.dma_start(out=RB[:], in_=g.w["router_b"][l].partition_broadcast(128)),
               writes=[K(RB)])
        xt = [sbt(g, es, f"rx{i}", [128, D], F32) for i in range(2)]
        xb = [sbt(g, es, f"rxb{i}", [128, D], BF16) for i in range(2)]
        xT = [sbt(g, es, f"rxT{i}", [128, 8, 128], F32) for i in range(2)]
        for t in range(NTT):
            i = t % 2
            fw.dma("sp", lambda e: e.dma_start(out=xt[i][:], in_=X1[t * 128:(t + 1) * 128, :]),
                   reads=[K(X1name, t)], writes=[K(xt[i])])
            fw.op("act", lambda: nc.scalar.copy(out=xb[i][:], in_=xt[i][:]), reads=[K(xt[i])], writes=[K(xb[i])])
            fw.dma("sp", lambda e: e.dma_start(out=g.x1b[t * 128:(t + 1) * 128, :], in_=xb[i][:]),
                   reads=[K(xb[i])], writes=[K("x1b", t)])
            for h in range(2):
                pst = ps[6 + h]
                for c in range(4):
                    kc = h * 4 + c
                    fw.op("pe", lambda: nc.tensor.transpose(out=pst[:, c * 128:(c + 1) * 128],
                                                            in_=xt[i][:, kc * 128:(kc + 1) * 128],
                                                            identity=g.ident[:]),
                          reads=[K(xt[i]), K(g.ident)], writes=[K(pst)], inc=(c == 3))
                fw.op("dve", lambda: nc.vector.tensor_copy(out=xT[i][:, h * 4:(h + 1) * 4, :],
                                                           in_=pst[:].rearrange("p (c n) -> p c n", c=4)),
                      reads=[K(pst)], writes=[K(xT[i], h)])
            for kc in range(8):
                fw.op("pe", lambda: nc.tensor.matmul(out=ps[2][:, 0:32], lhsT=xT[i][:, kc, :], rhs=RW[:, kc, :],
                                                     start=(kc == 0), stop=(kc == 7)),
                      reads=[K(xT[i]), K(RW)], writes=[K(ps[2])], inc=(kc == 7))
            fw.op("dve", lambda: nc.vector.tensor_tensor(out=L[:, t, :], in0=ps[2][:, 0:32], in1=RB[:], op=ALU.add),
                  reads=[K(ps[2]), K(RB)], writes=[K(L, t)])
        TOP = sbt(g, es, "rTOP", [128, NTT, 8], F32)
        SEL = sbt(g, es, "rSEL", [128, NTT, 32], F32)
        GT = sbt(g, es, "rGT", [128, NTT, 32], F32)
        CA = sbt(g, es, "rCA", [128, NTT, 32], F32)
        CB = sbt(g, es, "rCB", [128, NTT, 32], F32)
        Z = sbt(g, es, "rZ", [128, NTT], F32)
        SM = sbt(g, es, "rSM", [128, 8, 32], F32)
        SMb = sbt(g, es, "rSMb", [128, 32], BF16)
        for t in range(NTT):
            fw.op("dve", lambda: nc.vector.max(out=TOP[:, t, :], in_=L[:, t, :]), reads=[K(L, t)], writes=[K(TOP, t)])
        bshape = [128, NTT, 32]
        fw.op("dve", lambda: nc.vector.tensor_tensor(out=SEL[:], in0=L[:], in1=TOP[:, :, 3:4].to_broadcast(bshape),
                                                     op=ALU.is_ge), reads=[K(L), K(TOP)], writes=[K(SEL)])
        fw.op("dve", lambda: nc.vector.tensor_tensor(out=GT[:], in0=L[:], in1=TOP[:, :, 0:1].to_broadcast(bshape),
                                                     op=ALU.subtract), reads=[K(L), K(TOP)], writes=[K(GT)])
        fw.op("act", lambda: nc.scalar.activation(out=GT[:], in_=GT[:], func=AF.Exp), reads=[K(GT)], writes=[K(GT)])
        fw.op("dve", lambda: nc.vector.tensor_tensor(out=GT[:], in0=GT[:], in1=SEL[:], op=ALU.mult),
              reads=[K(GT), K(SEL)], writes=[K(GT)])
        fw.op("dve", lambda: nc.vector.tensor_reduce(out=Z[:], in_=GT[:], axis=AX.X, op=ALU.add),
              reads=[K(GT)], writes=[K(Z)])
        fw.op("dve", lambda: nc.vector.reciprocal(out=Z[:], in_=Z[:]), reads=[K(Z)], writes=[K(Z)])
        fw.op("dve", lambda: nc.vector.tensor_tensor(out=GT[:], in0=GT[:],
                                                     in1=Z[:].unsqueeze(2).to_broadcast(bshape), op=ALU.mult),
              reads=[K(GT), K(Z)], writes=[K(GT)])
        src, dst = SEL, CA
        s = 1
        while s < NTT:
            fw.op("dve", lambda: nc.vector.tensor_tensor(out=dst[:, s:, :], in0=src[:, s:, :], in1=src[:, :NTT - s, :],
                                                         op=ALU.add), reads=[K(src)], writes=[K(dst, "hi")])
            fw.op("dve", lambda: nc.vector.tensor_copy(out=dst[:, :s, :], in_=src[:, :s, :]),
                  reads=[K(src)], writes=[K(dst, "lo")])
            src = dst
            dst = CB if dst is CA else CA
            s *= 2
        INC = src
        EXC = dst
        fw.op("dve", lambda: nc.vector.tensor_copy(out=SMb[:], in_=INC[:, NTT - 1, :]), reads=[K(INC)], writes=[K(SMb)])
        fw.op("pe", lambda: nc.tensor.matmul(out=ps[3][:, 0:32], lhsT=g.trib[:], rhs=SMb[:], start=True, stop=True),
              reads=[K(g.trib), K(SMb)], writes=[K(ps[3], 0)])
        fw.op("pe", lambda: nc.tensor.matmul(out=ps[3][:, 32:64], lhsT=g.onesb[:], rhs=SMb[:], start=True, stop=True),
              reads=[K(g.onesb), K(SMb)], writes=[K(ps[3], 1)])
        PP, CNT, PAD, BASE, BEND, TMP = (SM[:, i, :] for i in range(6))
        fw.op("dve", lambda: nc.vector.tensor_copy(out=SM[:, 0:2, :], in_=ps[3][:, 0:64].rearrange("p (a e) -> p a e", a=2)),
              reads=[K(ps[3])], writes=[K(SM, 0), K(SM, 1)])
        fw.op("dve", lambda: nc.vector.tensor_scalar(out=TMP, in0=CNT, scalar1=1.0 / B, scalar2=(B - 1) / (2.0 * B),
                                                     op0=ALU.mult, op1=ALU.add), reads=[K(SM, 1)], writes=[K(SM, 5)])
        fw.op("dve", lambda: nc.vector.tensor_scalar_add(out=TMP, in0=TMP, scalar1=8388608.0),
              reads=[K(SM, 5)], writes=[K(SM, 5)])
        fw.op("dve", lambda: nc.vector.tensor_scalar(out=PAD, in0=TMP, scalar1=-8388608.0, scalar2=float(B),
                                                     op0=ALU.add, op1=ALU.mult), reads=[K(SM, 5)], writes=[K(SM, 2)])
        cur, oth = 2, 4
        s = 1
        while s < 32:
            fw.op("dve", lambda: nc.vector.tensor_tensor(out=SM[:, oth, s:], in0=SM[:, cur, s:], in1=SM[:, cur, :32 - s],
                                                         op=ALU.add), reads=[K(SM, cur)], writes=[K(SM, oth)])
            fw.op("dve", lambda: nc.vector.tensor_copy(out=SM[:, oth, :s], in_=SM[:, cur, :s]),
                  reads=[K(SM, cur)], writes=[K(SM, oth)])
            cur, oth = oth, (5 if oth == 4 else 4)
            s *= 2
        assert cur == 4
        fw.op("dve", lambda: nc.vector.tensor_tensor(out=BASE, in0=SM[:, 4, :], in1=PAD, op=ALU.subtract),
              reads=[K(SM, 4), K(SM, 2)], writes=[K(SM, 3)])
        CMP = sbt(g, es, "rCMP", [128, NB, 32], F32)
        EBf = sbt(g, es, "rEBf", [128, NB], F32)
        fw.op("dve", lambda: nc.vector.tensor_tensor(out=CMP[:], in0=SM[:, 4:5, :].to_broadcast([128, NB, 32]),
                                                     in1=g.iotaB[:, 0:NB].unsqueeze(2).to_broadcast([128, NB, 32]),
                                                     op=ALU.is_le), reads=[K(SM, 4), K(g.iotaB)], writes=[K(CMP)])
        fw.op("dve", lambda: nc.vector.tensor_reduce(out=EBf[:], in_=CMP[:], axis=AX.X, op=ALU.add),
              reads=[K(CMP)], writes=[K(EBf)])
        fw.op("dve", lambda: nc.vector.tensor_scalar_min(out=EBf[:], in0=EBf[:], scalar1=31.0),
              reads=[K(EBf)], writes=[K(EBf)])
        fw.op("dve", lambda: nc.vector.tensor_copy(out=EB[:], in_=EBf[:]), reads=[K(EBf)], writes=[K(EB)])
        IDXG, IDXB = keep["IDXG"], keep["IDXB"]
        IGf = sbt(g, es, "rIGf", [128, NB, 8], F32)
        fw.op("dve", lambda: nc.vector.tensor_scalar(out=EBf[:], in0=EBf[:], scalar1=float(l * NE), scalar2=None,
                                                     op0=ALU.add), reads=[K(EBf)], writes=[K(EBf)])
        fw.op("dve", lambda: nc.vector.tensor_copy(out=IDXB[:], in_=EBf[:]), reads=[K(EBf)], writes=[K(IDXB)])
        fw.op("dve", lambda: nc.vector.scalar_tensor_tensor(out=IGf[:], in0=EBf[:].unsqueeze(2).to_broadcast([128, NB, 8]),
                                                            scalar=1024.0, in1=g.kcp[:].unsqueeze(1).to_broadcast([128, NB, 8]),
                                                            op0=ALU.mult, op1=ALU.add),
              reads=[K(EBf), K(g.kcp)], writes=[K(IGf)])
        fw.op("dve", lambda: nc.vector.tensor_copy(out=IDXG[:], in_=IGf[:]), reads=[K(IGf)], writes=[K(IDXG)])
        fw.op("dve", lambda: nc.vector.tensor_tensor(out=SM[:, 5, :], in0=BASE, in1=PP, op=ALU.add),
              reads=[K(SM, 3), K(SM, 0)], writes=[K(SM, 5)])
        fw.op("dve", lambda: nc.vector.tensor_tensor(out=EXC[:], in0=INC[:], in1=SEL[:], op=ALU.subtract),
              reads=[K(INC), K(SEL)], writes=[K(EXC)])
        fw.op("dve", lambda: nc.vector.tensor_tensor(out=EXC[:], in0=EXC[:], in1=SM[:, 5:6, :].to_broadcast(bshape),
                                                     op=ALU.add), reads=[K(EXC), K(SM, 5)], writes=[K(EXC)])
        fw.op("dve", lambda: nc.vector.scalar_tensor_tensor(out=EXC[:], in0=EXC[:], scalar=1.0, in1=SEL[:],
                                                            op0=ALU.add, op1=ALU.mult),
              reads=[K(EXC), K(SEL)], writes=[K(EXC)])
        fw.op("dve", lambda: nc.vector.tensor_scalar_add(out=EXC[:], in0=EXC[:], scalar1=-1.0),
              reads=[K(EXC)], writes=[K(EXC)])
        POSM = EXC
        P8 = sbt(g, es, "rP8", [128, NTT, 8], F32)
        for t in range(NTT):
            fw.op("dve", lambda: nc.vector.max(out=P8[:, t, :], in_=POSM[:, t, :]), reads=[K(POSM)], writes=[K(P8, t)])
        fw.op("dve", lambda: nc.vector.tensor_copy(out=POS4i[:], in_=P8[:, :, 0:4]), reads=[K(P8)], writes=[K(POS4i)])
        EQ = INC
        for j in range(4):
            fw.op("dve", lambda: nc.vector.tensor_tensor(out=EQ[:], in0=POSM[:], in1=P8[:, :, j:j + 1].to_broadcast(bshape),
                                                         op=ALU.is_equal), reads=[K(POSM), K(P8)], writes=[K(EQ)])
            fw.op("dve", lambda: nc.vector.tensor_tensor(out=EQ[:], in0=EQ[:], in1=GT[:], op=ALU.mult),
                  reads=[K(EQ), K(GT)], writes=[K(EQ)])
            fw.op("dve", lambda: nc.vector.tensor_reduce(out=G4[:, :, j], in_=EQ[:], axis=AX.X, op=ALU.add),
                  reads=[K(EQ)], writes=[K(G4, j)])
        for t in range(NTT):
            i = t % 2
            fw.dma("sp", lambda e: e.dma_start(out=xb[i][:], in_=g.x1b[t * 128:(t + 1) * 128, :]),
                   reads=[K("x1b", t)], writes=[K(xb[i])])
            for j in range(4):
                fw.dma("pool", lambda e: e.indirect_dma_start(
                    out=g.xs[:, :], out_offset=bass.IndirectOffsetOnAxis(ap=POS4i[:, t, j:j + 1], axis=0),
                    in_=xb[i][:], in_offset=None), reads=[K(xb[i]), K(POS4i)], writes=[K("xs", ("s", t, j))])


def phase_experts(g, l, keep):
    nc, fw, NT, B = g.nc, g.fw, g.NT, g.B
    NB = moe_nb(NT, B)
    R = B // 128
    ps = g.ps
    w_gu, w_dn, b_gu, b_dn = g.w["moe_w_gu"], g.w["moe_w_down"], g.w["moe_b_gu"], g.w["moe_b_down"]
    with ExitStack() as es:
        WG = [sbt(g, es, f"eWG{i}", [128, 8, 2048], BF16) for i in range(2)]
        WD = [sbt(g, es, f"eWD{i}", [128, 8, 1024], BF16) for i in range(2)]
        ST = [sbt(g, es, f"eST{i}", [128, 2048], F32) for i in range(2)]
        BG = [sbt(g, es, f"eBG{i}", [128, 16], F32) for i in range(2)]
        BD = [sbt(g, es, f"eBD{i}", [128, 1024], F32) for i in range(2)]
        XS = [sbt(g, es, f"eXS{i}", [128, R, D], BF16) for i in range(2)]
        XT = [sbt(g, es, f"eXT{i}", [128, 8, B], BF16) for i in range(2)]
        HT = [sbt(g, es, f"eHT{i}", [128, 8, B], BF16) for i in range(2)]
        GC = [sbt(g, es, f"eGC{i}", [128, B], F32) for i in range(2)]
        SG = [sbt(g, es, f"eSG{i}", [128, B], F32) for i in range(2)]
        UC = [sbt(g, es, f"eUC{i}", [128, B], F32) for i in range(2)]
        Y = [sbt(g, es, f"eY{i}", [128, D], F32) for i in range(2)]
        BGR = sbt(g, es, "eBGR", [2, 2048], F32)
        wgu_rows = w_gu.rearrange("l e k f -> (l e k) f")
        wdn_rows = w_dn.rearrange("l e k f -> (l e k) f")
        bgu_rows = b_gu.rearrange("l e f -> (l e) f")
        bdn_rows = b_dn.rearrange("l e f -> (l e) f")
        IDXG, IDXB = keep["IDXG"], keep["IDXB"]

        def gather(out_ap, rows, idx_ap, reads, writes):
            fw.dma("pool", lambda e: e.indirect_dma_start(
                out=out_ap, out_offset=None, in_=rows[:, :],
                in_offset=bass.IndirectOffsetOnAxis(ap=idx_ap, axis=0)), reads=reads, writes=writes)

        def issue(b, k):
            st = ST[k % 2]
            if k < 8:
                gather(st[:], wgu_rows, IDXG[:, b, k:k + 1], [K(IDXG)], [K(st)])
            else:
                for a in range(2):
                    fc = 2 * (k - 8) + a
                    gather(st[:, a * 1024:(a + 1) * 1024], wdn_rows, IDXG[:, b, fc:fc + 1], [K(IDXG)], [K(st, a)])

        def cast(b, k):
            st, i = ST[k % 2], b % 2
            if k < 8:
                fw.op("act", lambda: nc.scalar.copy(out=WG[i][:, k, :], in_=st[:]), reads=[K(st)], writes=[K(WG[i], k)])
            else:
                fc2 = k - 8
                fw.op("act", lambda: nc.scalar.copy(out=WD[i][:, 2 * fc2:2 * fc2 + 2, :],
                                                    in_=st[:].rearrange("p (a f) -> p a f", a=2)),
                      reads=[K(st)], writes=[K(WD[i], fc2)])

        def prefetch_slot(b, k):
            if b >= NB:
                return
            if k == 0:
                gather(BGR[:], bgu_rows, IDXB[0:2, b:b + 1], [K(IDXB)], [K(BGR)])
                gather(BD[b % 2][:], bdn_rows, IDXB[:, b:b + 1], [K(IDXB)], [K(BD[b % 2])])
            if k >= 1:
                cast(b, k - 1)
            if k < 12:
                issue(b, k)

        def bias_transposes(b):
            i = b % 2
            for m in range(16):
                fw.op("pe", lambda: nc.tensor.transpose(out=ps[7][:, m:m + 1], in_=BGR[0:1, m * 128:(m + 1) * 128],
                                                        identity=g.ident[0:1, 0:1]),
                      reads=[K(BGR), K(g.ident)], writes=[K(ps[7])], inc=(m == 15))
            fw.op("dve", lambda: nc.vector.tensor_copy(out=BG[i][:], in_=ps[7][:, 0:16]),
                  reads=[K(ps[7])], writes=[K(BG[i])])

        def load_x(b):
            fw.dma("sp", lambda e: e.dma_start(out=XS[b % 2][:], in_=g.xs[b * B:(b + 1) * B, :].rearrange("(r p) d -> p r d", p=128)),
                   reads=[K("xs")], writes=[K(XS[b % 2])])

        def x_transposes(b):
            i = b % 2
            for kc in range(8):
                pst = ps[kc % 2]
                pv = pst[:] if kc % 2 == 0 else pst[:].bitcast(BF16)
                for r in range(R):
                    fw.op("pe", lambda: nc.tensor.transpose(out=pv[:, r * 128:(r + 1) * 128],
                                                            in_=XS[i][:, r, kc * 128:(kc + 1) * 128], identity=g.identb[:]),
                          reads=[K(XS[i]), K(g.identb)], writes=[K(pst)], inc=(r == R - 1))
                fw.op("dve", lambda: nc.vector.tensor_copy(out=XT[i][:, kc, :], in_=pv[:, 0:B]),
                      reads=[K(pst)], writes=[K(XT[i], kc)])

        for k in range(13):
            prefetch_slot(0, k)
        load_x(0)
        bias_transposes(0)
        for b in range(NB):
            i = b % 2
            if b + 1 < NB:
                load_x(b + 1)
            x_transposes(b)
            for m in range(8):
                prefetch_slot(b + 1, m)
                j = m % 2
                pg, pu = ps[2 + j], ps[4 + j]
                for (pp, col) in ((pg, m * 128), (pu, 1024 + m * 128)):
                    for kc in range(8):
                        fw.op("pe", lambda: nc.tensor.matmul(out=pp[:, 0:B], lhsT=WG[i][:, kc, col:col + 128],
                                                             rhs=XT[i][:, kc, :], start=(kc == 0), stop=(kc == 7)),
                              reads=[K(WG[i], kc), K(XT[i], kc)], writes=[K(pp)], inc=(kc == 7))
                fw.op("dve", lambda: nc.vector.tensor_scalar(out=GC[j][:], in0=pg[:, 0:B], scalar1=BG[i][:, m:m + 1],
                                                             scalar2=7.0, op0=ALU.add, op1=ALU.min),
                      reads=[K(pg), K(BG[i])], writes=[K(GC[j])])
                fw.op("act", lambda: nc.scalar.activation(out=SG[j][:], in_=GC[j][:], func=AF.Sigmoid, scale=1.702),
                      reads=[K(GC[j])], writes=[K(SG[j])])
                fw.op("act", lambda: nc.scalar.activation(out=UC[j][:], in_=pu[:, 0:B], func=AF.Identity, bias=BG[i][:, 8 + m:9 + m]),
                      reads=[K(pu), K(BG[i])], writes=[K(UC[j])])
                fw.op("dve", lambda: nc.vector.tensor_scalar(out=UC[j][:], in0=UC[j][:], scalar1=7.0, scalar2=-7.0,
                                                             op0=ALU.min, op1=ALU.max),
                      reads=[K(UC[j])], writes=[K(UC[j])])
                fw.op("dve", lambda: nc.vector.tensor_tensor(out=GC[j][:], in0=GC[j][:], in1=SG[j][:], op=ALU.mult),
                      reads=[K(GC[j]), K(SG[j])], writes=[K(GC[j])])
                fw.op("dve", lambda: nc.vector.scalar_tensor_tensor(out=HT[i][:, m, :], in0=UC[j][:], scalar=1.0, in1=GC[j][:],
                                                                    op0=ALU.add, op1=ALU.mult),
                      reads=[K(GC[j]), K(UC[j])], writes=[K(HT[i], m)])
            for r in range(R):
                prefetch_slot(b + 1, 8 + r)
                yb = Y[r % 2]
                for nh in range(2):
                    py = ps[6 + nh]
                    for fc in range(8):
                        fw.op("pe", lambda: nc.tensor.matmul(out=py[:, :], lhsT=HT[i][:, fc, r * 128:(r + 1) * 128],
                                                             rhs=WD[i][:, fc, nh * 512:(nh + 1) * 512],
                                                             start=(fc == 0), stop=(fc == 7)),
                              reads=[K(HT[i], fc), K(WD[i], fc // 2)], writes=[K(py)], inc=(fc == 7))
                    fw.op("dve", lambda: nc.vector.tensor_tensor(out=yb[:, nh * 512:(nh + 1) * 512], in0=py[:, :],
                                                                 in1=BD[i][:, nh * 512:(nh + 1) * 512], op=ALU.add),
                          reads=[K(py), K(BD[i])], writes=[K(yb, nh)])
                row0 = b * B + r * 128
                fw.dma("sp", lambda e: e.dma_start(out=g.ys[row0:row0 + 128, :], in_=yb[:]),
                       reads=[K(yb)], writes=[K("ys", ("b", b, r))])
            for k in range(8 + R, 13):
                prefetch_slot(b + 1, k)
            if b + 1 < NB:
                bias_transposes(b + 1)


def phase_combine(g, l, X1, X1name, XO, XOname, keep):
    nc, fw, NT = g.nc, g.fw, g.NT
    NTT = NT // 128
    POS4i, G4 = keep["POS4i"], keep["G4"]
    with ExitStack() as es:
        lng = sbt(g, es, "cLNG", [128, D], F32)
        lnb = sbt(g, es, "cLNB", [128, D], F32)
        fw.dma("sp", lambda e: e.dma_start(out=lng[:], in_=g.w["ln_g"][l, 1, :].partition_broadcast(128)), writes=[K(lng)])
        fw.dma("sp", lambda e: e.dma_start(out=lnb[:], in_=g.w["ln_b"][l, 1, :].partition_broadcast(128)), writes=[K(lnb)])
        xt = [sbt(g, es, f"cx{i}", [128, D], F32) for i in range(2)]
        yg = [[sbt(g, es, f"cy{i}_{j}", [128, D], F32) for j in range(4)] for i in range(2)]
        ot = [sbt(g, es, f"co{i}", [128, D], F32) for i in range(2)]
        def fetch(t):
            i = t % 2
            fw.dma("sp", lambda e: e.dma_start(out=xt[i][:], in_=X1[t * 128:(t + 1) * 128, :]),
                   reads=[K(X1name, t)], writes=[K(xt[i])])
            for j in range(4):
                fw.dma("pool", lambda e: e.indirect_dma_start(
                    out=yg[i][j][:], out_offset=None, in_=g.ys[:, :],
                    in_offset=bass.IndirectOffsetOnAxis(ap=POS4i[:, t, j:j + 1], axis=0)),
                    reads=[K("ys"), K(POS4i)], writes=[K(yg[i][j])])

        fetch(0)
        for t in range(NTT):
            i = t % 2
            if t + 1 < NTT:
                fetch(t + 1)
            fw.op("act", lambda: nc.scalar.mul(out=xt[i][:], in_=xt[i][:], mul=ALPHA), reads=[K(xt[i])], writes=[K(xt[i])])
            for j in range(4):
                fw.op("dve", lambda: nc.vector.scalar_tensor_tensor(out=xt[i][:], in0=yg[i][j][:], scalar=G4[:, t, j:j + 1],
                                                                    in1=xt[i][:], op0=ALU.mult, op1=ALU.add),
                      reads=[K(yg[i][j]), K(G4), K(xt[i])], writes=[K(xt[i])])
            layer_norm_tile(g, xt[i], lng, lnb, ot[i], None, es)
            fw.dma("sp", lambda e: e.dma_start(out=XO[t * 128:(t + 1) * 128, :], in_=ot[i][:]),
                   reads=[K(ot[i])], writes=[K(XOname, t)])


WEIGHT_SHAPES = {
    "ln_g": [4, 2, 1024], "ln_b": [4, 2, 1024],
    "nsa_w_in": [2, 1024, 2608], "nsa_cmp_pos": [2, 2, 32, 64], "nsa_cmp_w1": [2, 2, 2048, 256],
    "nsa_cmp_b1": [2, 2, 256], "nsa_cmp_w2": [2, 2, 256, 64], "nsa_cmp_b2": [2, 2, 64],
    "nsa_gate_b": [2, 48], "nsa_w_out": [2, 1024, 1024],
    "ml_w_in": [1, 1024, 3080], "ml_conv_w": [1, 4, 1024], "ml_conv_b": [1, 1024], "ml_gate_b": [1, 8],
    "ml_norm_g": [1, 1024], "ml_w_out": [1, 1024, 1024],
    "hg_w_in": [1, 1024, 4096], "hg_lower": [4, 1024], "hg_norm_g": [1, 1024], "hg_w_out": [1, 1024, 1024],
    "router_w": [4, 1024, 32], "router_b": [4, 32],
    "moe_w_gu": [4, 32, 1024, 2048], "moe_b_gu": [4, 32, 2048], "moe_w_down": [4, 32, 1024, 1024],
    "moe_b_down": [4, 32, 1024],
}
NB_MAX = 128


def make_consts(B):
    k_ = np.arange(128)
    c = np.zeros((128, 3 * 128 + NB_MAX + 8 + 128), np.float32)
    c[:, 3 * 128 + NB_MAX + 8:] = (k_[:, None] > k_[None, :]) & ((k_[:, None] // 64) == (k_[None, :] // 64))
    c[:, 0:128] = np.eye(128, dtype=np.float32)
    k = np.arange(128)
    c[:, 128:256] = (k[:, None] < k[None, :]).astype(np.float32)
    c[:, 256:384] = 1.0
    c[:, 384:384 + NB_MAX] = (np.arange(NB_MAX) * B)[None, :]
    c[:, 384 + NB_MAX:384 + NB_MAX + 8] = np.arange(8)[None, :] * 128 + k[:, None]
    return c


def build(NT, B, plan, wnames, wshapes=None):
    nc = bass.Bass("TRN2", target_bir_lowering=False)
    g = G()
    g.nc, g.NT, g.B = nc, NT, B
    NB = moe_nb(NT, B)
    wshapes = wshapes or WEIGHT_SHAPES
    g.w = {n: nc.dram_tensor(n, list(wshapes[n]), F32, kind="ExternalInput").ap() for n in wnames}
    xin = nc.dram_tensor("x", [NT, D], F32, kind="ExternalInput").ap()
    cst = nc.dram_tensor("cst", [128, 3 * 128 + NB_MAX + 8 + 128], F32, kind="ExternalInput").ap()
    out = nc.dram_tensor("out", [NT, D], F32, kind="ExternalOutput").ap()
    xa = nc.dram_tensor("xa", [NT, D], F32).ap()
    xbuf = nc.dram_tensor("xbuf", [NT, D], F32).ap()
    g.x1b = nc.dram_tensor("x1b", [NT, D], BF16).ap()
    g.xs = nc.dram_tensor("xs", [NB * B, D], BF16).ap()
    g.ys = nc.dram_tensor("ys", [NB * B, D], F32).ap()
    kinds = {ph[1] % 3 for ph in plan if ph[0] == "mixer"}
    if kinds:
        g.nsac = nc.dram_tensor("nsac", [128, NSAC_COLS], F32, kind="ExternalInput").ap()
        g.v2_scr = nc.dram_tensor("v2_scr", [T, 1024], BF16).ap()
        g.sg_scr = nc.dram_tensor("sg_scr", [T, 1024], BF16).ap()
        g.qT_scr = nc.dram_tensor("qT_scr", [1024, T], BF16).ap()
        g.kT_scr = nc.dram_tensor("kT_scr", [4, 256, T], BF16).ap()
        g.v_scr = nc.dram_tensor("v_scr", [T, 512], BF16).ap()
    g.o_scr = (nc.dram_tensor("o_scr", [T, 1024], BF16, kind="ExternalOutput") if DEBUG else nc.dram_tensor("o_scr", [T, 1024], BF16)).ap()
    with ExitStack() as es:
        fw = FW(nc, es)
        g.fw = fw
        g.ps = [es.enter_context(nc.psum_tensor("ps0", [128, 1024], BF16))]
        g.ps += [es.enter_context(nc.psum_tensor(f"ps{i}", [128, 512], F32)) for i in range(1, 8)]
        C = sbt(g, es, "cstf", [128, 3 * 128 + NB_MAX + 8 + 128], F32)
        g.C = C
        g.kcp = sbt(g, es, "kcp", [128, 8], F32)
        g.ident = sbt(g, es, "ident", [128, 128], F32)
        g.identb = sbt(g, es, "identb", [128, 128], BF16)
        g.trib = sbt(g, es, "trib", [128, 128], BF16)
        g.onesb = sbt(g, es, "onesb", [128, 128], BF16)
        g.iotaB = sbt(g, es, "iotaB", [128, NB_MAX], F32)
        g.ln_st = sbt(g, es, "ln_st", [128, 2, 6], F32)
        g.ln_mv = sbt(g, es, "ln_mv", [128, 4], F32)
        fw.dma("sp", lambda e: e.dma_start(out=C[:], in_=cst[:, :]), writes=[K(C)])
        fw.op("dve", lambda: nc.vector.tensor_copy(out=g.ident[:], in_=C[:, 0:128]), reads=[K(C)], writes=[K(g.ident)])
        fw.op("dve", lambda: nc.vector.tensor_copy(out=g.identb[:], in_=C[:, 0:128]), reads=[K(C)], writes=[K(g.identb)])
        fw.op("dve", lambda: nc.vector.tensor_copy(out=g.trib[:], in_=C[:, 128:256]), reads=[K(C)], writes=[K(g.trib)])
        fw.op("dve", lambda: nc.vector.tensor_copy(out=g.onesb[:], in_=C[:, 256:384]), reads=[K(C)], writes=[K(g.onesb)])
        fw.op("dve", lambda: nc.vector.tensor_copy(out=g.iotaB[:], in_=C[:, 384:384 + NB_MAX]), reads=[K(C)],
              writes=[K(g.iotaB)])
        fw.op("dve", lambda: nc.vector.tensor_copy(out=g.kcp[:], in_=C[:, 384 + NB_MAX:384 + NB_MAX + 8]), reads=[K(C)], writes=[K(g.kcp)])
        NTT = NT // 128
        keep = {"IDXG": sbt(g, es, "kIDXG", [128, NB, 8], I32), "IDXB": sbt(g, es, "kIDXB", [128, NB], I32),
                "POS4i": sbt(g, es, "kPOS", [128, NTT, 4], I32), "G4": sbt(g, es, "kG4", [128, NTT, 4], F32),
                "EB": sbt(g, es, "kEB", [128, NB], I32)}
        bufs = {"x": xin, "xa": xa, "xb": xbuf, "out": out}
        for ph in plan:
            kind = ph[0]
            if kind == "moe":
                _, l, src, dst = ph
                phase_route(g, l, bufs[src], src, keep)
                fw.barrier()
                phase_experts(g, l, keep)
                fw.barrier()
                phase_combine(g, l, bufs[src], src, bufs[dst], dst, keep)
                fw.barrier()
            elif kind == "mixer":
                _, l, src, dst = ph
                MIXERS[l % 3](g, l, bufs[src], src, bufs[dst], dst)
                fw.barrier()
        fw.finish(["out"])
        g.ninst = fw.ninst
    return nc, g


MIXERS = {}
DEBUG = False


T = 2048
TT = T // 128


def build_xT(g, es, X, Xname, row0, XT, xstage):
    nc, fw, ps = g.nc, g.fw, g.ps
    for t in range(TT):
        xt = xstage[t % 2]
        fw.dma("sp", lambda e: e.dma_start(out=xt[:], in_=X[row0 + t * 128:row0 + (t + 1) * 128, :]),
               reads=[K(Xname, (row0 // 128) + t)], writes=[K(xt)])
        for h in range(2):
            pst = ps[6 + h]
            for c in range(4):
                kc = h * 4 + c
                fw.op("pe", lambda: nc.tensor.transpose(out=pst[:, c * 128:(c + 1) * 128],
                                                        in_=xt[:, kc * 128:(kc + 1) * 128], identity=g.ident[:]),
                      reads=[K(xt), K(g.ident)], writes=[K(pst)], inc=(c == 3))
            eng = "dve" if h == 0 else "act"
            if h == 0:
                fw.op("dve", lambda: nc.vector.tensor_copy(out=XT[:, 0:4, t * 128:(t + 1) * 128],
                                                           in_=pst[:].rearrange("p (c n) -> p c n", c=4)),
                      reads=[K(pst)], writes=[K(XT, (t, 0))])
            else:
                fw.op("act", lambda: nc.scalar.copy(out=XT[:, 4:8, t * 128:(t + 1) * 128],
                                                    in_=pst[:].rearrange("p (c n) -> p c n", c=4)),
                      reads=[K(pst)], writes=[K(XT, (t, 1))])


def load_w_bf16(g, W, src, ncols, stage, col_ops):
    fw = g.fw
    for kc in range(8):
        st = stage[kc % 2]
        fw.dma("sp", lambda e: e.dma_start(out=st[:, 0:ncols], in_=src[kc * 128:(kc + 1) * 128, :]), writes=[K(st)])
        col_ops(kc, st)


def tail_load_x(g, X, Xname, row0, t, bufs, rows=128):
    xt = bufs["xt"][t % 2]
    r0 = row0 + t * rows
    g.fw.dma("sp", lambda e: e.dma_start(out=xt[0:rows, :], in_=X[r0:r0 + rows, :]), reads=[K(Xname, r0 // 128)], writes=[K(xt)])


def mixer_tail(g, es, l, X, Xname, XO, XOname, row0, t, OTOKt, WO, lng, lnb, bufs, okey, rows=128, xloaded=False):
    nc, fw, ps = g.nc, g.fw, g.ps
    OTt, xt, ot = bufs["OTt"][t % 2], bufs["xt"][t % 2], bufs["ot"][t % 2]
    pst = ps[0]
    r0 = row0 + t * rows
    xkey = K(Xname, r0 // 128)
    for kc in range(8):
        fw.op("pe", lambda: nc.tensor.transpose(out=pst[:, kc * 128:kc * 128 + rows], in_=OTOKt[:, kc * 128:(kc + 1) * 128],
                                                identity=g.identb[0:rows, 0:rows]),
              reads=[okey, K(g.identb)], writes=[K(pst)], inc=(kc == 7))
    fw.op("dve", lambda: nc.vector.tensor_copy(out=OTt[:, :, 0:rows], in_=pst[:].rearrange("p (c n) -> p c n", c=8)[:, :, 0:rows]),
          reads=[K(pst)], writes=[K(OTt)])
    if not xloaded:
        fw.dma("sp", lambda e: e.dma_start(out=xt[0:rows, :], in_=X[r0:r0 + rows, :]), reads=[xkey], writes=[K(xt)])
    for nh in range(2):
        py = ps[6 + nh]
        for kc in range(8):
            fw.op("pe", lambda: nc.tensor.matmul(out=py[0:rows, :], lhsT=OTt[:, kc, 0:rows], rhs=WO[:, kc, nh * 512:(nh + 1) * 512],
                                                 start=(kc == 0), stop=(kc == 7)),
                  reads=[K(OTt), K(WO)], writes=[K(py)], inc=(kc == 7))
        fw.op("dve", lambda: nc.vector.scalar_tensor_tensor(out=xt[0:rows, nh * 512:(nh + 1) * 512],
                                                            in0=xt[0:rows, nh * 512:(nh + 1) * 512], scalar=ALPHA, in1=py[0:rows, :],
                                                            op0=ALU.mult, op1=ALU.add),
              reads=[K(xt), K(py)], writes=[K(xt)])
    layer_norm_tile(g, xt, lng, lnb, ot, None, es, rows)
    fw.dma("sp", lambda e: e.dma_start(out=XO[r0:r0 + rows, :], in_=ot[0:rows, :]),
           reads=[K(ot)], writes=[K(XOname, r0 // 128)])


def load_tail_weights(g, es, l, w_out_ap, stage):
    nc, fw = g.nc, g.fw
    WO = sbt(g, es, "mWO", [128, 8, 1024], BF16)
    lng = sbt(g, es, "mLNG", [128, D], F32)
    lnb = sbt(g, es, "mLNB", [128, D], F32)
    fw.dma("sp", lambda e: e.dma_start(out=lng[:], in_=g.w["ln_g"][l, 0, :].partition_broadcast(128)), writes=[K(lng)])
    fw.dma("sp", lambda e: e.dma_start(out=lnb[:], in_=g.w["ln_b"][l, 0, :].partition_broadcast(128)), writes=[K(lnb)])
    load_w_bf16(g, WO, w_out_ap, 1024, stage,
                lambda kc, st: fw.op("act", lambda: nc.scalar.copy(out=WO[:, kc, :], in_=st[:, 0:1024]),
                                     reads=[K(st)], writes=[K(WO, kc)]))
    bufs = {"OTt": [sbt(g, es, f"mOTt{i}", [128, 8, 128], BF16) for i in range(2)],
            "xt": [sbt(g, es, f"mxt{i}", [128, D], F32) for i in range(2)],
            "ot": [sbt(g, es, "mot", [128, D], F32)] * 2}
    return WO, lng, lnb, bufs


NSAC_COLS = 2048 + 4096 + 2048 + 32 + 512 + 512


def make_nsa_consts():
    c = np.zeros((128, NSAC_COLS), np.float32)
    t = np.arange(T)
    cc = np.arange(128)
    c[:, 0:2048] = ((16 * cc[:, None] + 31) <= t[None, :]) & (cc[:, None] < 127)
    sl = np.arange(128)[:, None]
    tl = np.arange(512)[None, :]
    for di in range(8):
        delta = di * 128 - 384
        lag = delta + tl - sl
        c[:, 2048 + di * 512:2048 + (di + 1) * 512] = (lag >= 0) & (lag < 512)
    blk = np.arange(32)
    c[0:32, 6144:8192] = (t[None, :] // 64 == blk[:, None])
    cmp_start = np.arange(127) * 16
    sel_start = np.arange(32) * 64
    overlap = (np.minimum(cmp_start[:, None] + 32, sel_start[None, :] + 64) - np.maximum(cmp_start[:, None], sel_start[None, :]))
    c[0:127, 8192:8224] = np.clip(overlap, 0, None) / 16.0
    cur = (t // 64)[:, None]
    forced = (blk[None, :] == 0) | (blk[None, :] == cur) | (blk[None, :] == cur - 1)
    valid = blk[None, :] <= cur
    mul = (valid & ~forced).astype(np.float32)
    add = np.where(forced, 1e30, np.where(valid, 0.0, -1e30)).astype(np.float32)
    c[:, 8224:8736] = mul.reshape(16, 128, 32).transpose(1, 0, 2).reshape(128, 512)
    c[:, 8736:9248] = add.reshape(16, 128, 32).transpose(1, 0, 2).reshape(128, 512)
    return c


def phase_nsa(g, l, X, Xname, XO, XOname):
    nc, fw, ps = g.nc, g.fw, g.ps
    sl = l // 3
    nseq = g.NT // T
    w_in = g.w["nsa_w_in"][sl]
    qS, kS, vS, oS = g.qT_scr, g.kT_scr, g.v_scr, g.o_scr
    with ExitStack() as es:
        st1 = sbt(g, es, "nST", [128, 2608], F32)
        CMPM = sbt(g, es, "nCMPM", [128, 2048], BF16)
        WINM = sbt(g, es, "nWINM", [128, 8, 512], BF16)
        EXPB = sbt(g, es, "nEXPB", [32, 2048], BF16)
        MULA = sbt(g, es, "nMULA", [128, 2, 16, 32], F32)
        VCA = sbt(g, es, "nVCA", [128, 97], BF16)
        for (dst, key, c0, rows) in ((CMPM[:], K(CMPM), 0, 128), (WINM[:, 0:4, :], K(WINM, 0), 2048, 128),
                                     (WINM[:, 4:8, :], K(WINM, 1), 4096, 128), (EXPB[:], K(EXPB), 6144, 32)):
            fw.dma("sp", lambda e: e.dma_start(out=st1[0:rows, 0:2048], in_=g.nsac[0:rows, c0:c0 + 2048]), writes=[K(st1)])
            src = st1[0:rows, 0:2048] if len(dst.shape) == 2 else st1[:, 0:2048].rearrange("p (a b) -> p a b", a=4)
            fw.op("dve", lambda: nc.vector.tensor_copy(out=dst, in_=src), reads=[K(st1)], writes=[key])
        fw.dma("sp", lambda e: e.dma_start(out=st1[:, 0:1056], in_=g.nsac[:, 8192:9248]), writes=[K(st1)])
        fw.op("dve", lambda: nc.vector.tensor_copy(out=VCA[:, 65:97], in_=st1[:, 0:32]), reads=[K(st1)], writes=[K(VCA, "c")])
        fw.op("dve", lambda: nc.vector.tensor_copy(out=MULA[:].rearrange("p a t b -> p (a t b)"), in_=st1[:, 32:1056]),
              reads=[K(st1)], writes=[K(MULA)])
        fw.op("dve", lambda: nc.vector.memset(VCA[:, 64:65], 1.0), writes=[K(VCA, "o")])
        W2 = sbt(g, es, "nW2", [128, 2, 2, 64], BF16)
        B1 = sbt(g, es, "nB1", [128, 2, 2], F32)
        B2K = sbt(g, es, "nB2K", [64, 1], F32)
        B2V = sbt(g, es, "nB2V", [128, 64], F32)
        POST = sbt(g, es, "nPOST", [64, 2, 32], BF16)
        CONSTH = sbt(g, es, "nCONSTH", [128, 2, 2], F32)
        GB = sbt(g, es, "nGB", [128, 48], F32)
        SGT = sbt(g, es, "nSGT", [128, TT, 48], F32)
        w1 = g.w["nsa_cmp_w1"][sl]
        w2 = g.w["nsa_cmp_w2"][sl]
        fw.dma("sp", lambda e: e.dma_start(out=st1[:, 0:256].rearrange("p (j c d) -> p j c d", j=2, c=2),
                                           in_=w2.rearrange("j (c p) d -> p j c d", p=128)), writes=[K(st1)])
        fw.op("dve", lambda: nc.vector.tensor_copy(out=W2[:], in_=st1[:, 0:256].rearrange("p (j c d) -> p j c d", j=2, c=2)),
              reads=[K(st1)], writes=[K(W2)])
        fw.dma("sp", lambda e: e.dma_start(out=B1[:], in_=g.w["nsa_cmp_b1"][sl].rearrange("j (c p) -> p j c", p=128),
                                           allow_slow_non_contiguous=True), writes=[K(B1)])
        fw.dma("sp", lambda e: e.dma_start(out=B2K[:, :], in_=g.w["nsa_cmp_b2"][sl, 0, :].rearrange("(d o) -> d o", o=1),
                                           allow_slow_non_contiguous=True), writes=[K(B2K)])
        fw.dma("sp", lambda e: e.dma_start(out=B2V[:], in_=g.w["nsa_cmp_b2"][sl, 1, :].partition_broadcast(128)), writes=[K(B2V)])
        fw.dma("sp", lambda e: e.dma_start(out=GB[:], in_=g.w["nsa_gate_b"][sl, :].partition_broadcast(128)), writes=[K(GB)])
        fw.dma("sp", lambda e: e.dma_start(out=st1[0:64, 0:64].rearrange("d (j p) -> d j p", j=2),
                                           in_=g.w["nsa_cmp_pos"][sl].rearrange("j p d -> d j p"),
                                           allow_slow_non_contiguous=True), writes=[K(st1)])
        fw.op("dve", lambda: nc.vector.tensor_copy(out=POST[:], in_=st1[0:64, 0:64].rearrange("d (j p) -> d j p", j=2)),
              reads=[K(st1)], writes=[K(POST)])
        WO, lng, lnb, tb_bufs = load_tail_weights(g, es, l, g.w["nsa_w_out"][sl], [st1, st1])
        first = [True]

        for s in range(nseq):
            row0 = s * T
            with ExitStack() as esA:
                WQ = sbt(g, esA, "nWQ", [128, 8, 1024], BF16)
                WK = sbt(g, esA, "nWK", [128, 8, 4, 256], BF16)
                WVg = sbt(g, esA, "nWVg", [128, 8, 4, 128], BF16)
                WG = sbt(g, esA, "nWG", [128, 8, 48], BF16)
                XT = sbt(g, esA, "nXT", [128, 8, T], BF16)
                EV = [sbt(g, esA, f"nEV{i}", [128, 512], BF16) for i in range(3)]
                kcols = (1024, 1280, 1536, 2048)

                def cast_in(kc, st):
                    fw.op("act", lambda: nc.scalar.mul(out=WQ[:, kc, :], in_=st[:, 0:1024], mul=0.125), reads=[K(st)], writes=[K(WQ, kc)])
                    for ty, c0 in enumerate(kcols):
                        if ty % 2 == 0:
                            fw.op("dve", lambda: nc.vector.tensor_copy(out=WK[:, kc, ty, :], in_=st[:, c0:c0 + 256]),
                                  reads=[K(st)], writes=[K(WK, (kc, ty))])
                        else:
                            fw.op("pool", lambda: nc.gpsimd.tensor_copy(out=WK[:, kc, ty, :], in_=st[:, c0:c0 + 256]),
                                  reads=[K(st)], writes=[K(WK, (kc, ty))])
                    fw.op("dve", lambda: nc.vector.tensor_copy(out=WVg[:, kc, :, 0:64], in_=st[:, 1792:2048].rearrange("p (g d) -> p g d", g=4)),
                          reads=[K(st)], writes=[K(WVg, (kc, 0))])
                    fw.op("pool", lambda: nc.gpsimd.tensor_copy(out=WVg[:, kc, :, 64:128], in_=st[:, 2304:2560].rearrange("p (g d) -> p g d", g=4)),
                          reads=[K(st)], writes=[K(WVg, (kc, 1))])
                    fw.op("dve", lambda: nc.vector.tensor_copy(out=WG[:, kc, :], in_=st[:, 2560:2608]), reads=[K(st)], writes=[K(WG, kc)])

                load_w_bf16(g, None, w_in, 2608, [st1, st1], cast_in)
                build_xT(g, esA, X, Xname, row0, XT, tb_bufs["xt"])
                nev = [0]

                def evac_store(pS, ncol, dst_ap, dkey):
                    E = EV[nev[0] % 3]
                    if nev[0] % 2 == 0:
                        fw.op("act", lambda: nc.scalar.copy(out=E[:, 0:ncol], in_=pS[:, 0:ncol]), reads=[K(pS)], writes=[K(E)])
                    else:
                        fw.op("dve", lambda: nc.vector.tensor_copy(out=E[:, 0:ncol], in_=pS[:, 0:ncol]), reads=[K(pS)], writes=[K(E)])
                    nev[0] += 1
                    fw.dma("sp", lambda e: e.dma_start(out=dst_ap, in_=E[:, 0:ncol]), reads=[K(E)], writes=[dkey])

                for t in range(TT):
                    for kc in range(8):
                        fw.op("pe", lambda: nc.tensor.matmul(out=ps[4][:, 0:48], lhsT=XT[:, kc, t * 128:(t + 1) * 128], rhs=WG[:, kc, :],
                                                             start=(kc == 0), stop=(kc == 7)),
                              reads=[K(XT), K(WG)], writes=[K(ps[4])], inc=(kc == 7))
                    fw.op("dve", lambda: nc.vector.tensor_tensor(out=SGT[:, t, :], in0=ps[4][:, 0:48], in1=GB[:], op=ALU.add),
                          reads=[K(ps[4]), K(GB)], writes=[K(SGT, t)])
                fw.op("act", lambda: nc.scalar.activation(out=SGT[:], in_=SGT[:], func=AF.Sigmoid), reads=[K(SGT)], writes=[K(SGT)])
                np_ = [0]
                for c in range(8):
                    for tb in range(4):
                        pS = ps[2 + np_[0] % 2]
                        np_[0] += 1
                        for kc in range(8):
                            fw.op("pe", lambda: nc.tensor.matmul(out=pS[:, :], lhsT=WQ[:, kc, c * 128:(c + 1) * 128],
                                                                 rhs=XT[:, kc, tb * 512:(tb + 1) * 512], start=(kc == 0), stop=(kc == 7)),
                                  reads=[K(WQ), K(XT)], writes=[K(pS)], inc=(kc == 7))
                        evac_store(pS, 512, qS[c * 128:(c + 1) * 128, tb * 512:(tb + 1) * 512], K("qS", (c, tb)))
                for ty in range(4):
                    for c in range(2):
                        for tb in range(4):
                            pS = ps[2 + np_[0] % 2]
                            np_[0] += 1
                            for kc in range(8):
                                fw.op("pe", lambda: nc.tensor.matmul(out=pS[:, :], lhsT=WK[:, kc, ty, c * 128:(c + 1) * 128],
                                                                     rhs=XT[:, kc, tb * 512:(tb + 1) * 512], start=(kc == 0), stop=(kc == 7)),
                                      reads=[K(WK), K(XT)], writes=[K(pS)], inc=(kc == 7))
                            evac_store(pS, 512, kS[ty, c * 128:(c + 1) * 128, tb * 512:(tb + 1) * 512], K("kS", (ty, c, tb)))
                for t in range(TT):
                    pS = ps[2 + np_[0] % 2]
                    np_[0] += 1
                    for kc in range(8):
                        fw.op("pe", lambda: nc.tensor.matmul(out=pS[:, :], lhsT=XT[:, kc, t * 128:(t + 1) * 128],
                                                             rhs=WVg[:, kc, :, :].rearrange("p g d -> p (g d)"), start=(kc == 0), stop=(kc == 7)),
                              reads=[K(XT), K(WVg)], writes=[K(pS)], inc=(kc == 7))
                    evac_store(pS, 512, vS[t * 128:(t + 1) * 128, :], K("vS", t))
            fw.barrier()
            with ExitStack() as esB:
                W1 = sbt(g, esB, "nW1", [64, 2, 32, 256], BF16)
                QTg = sbt(g, esB, "nQTg", [64, 4, T], BF16)
                KT4 = sbt(g, esB, "nKT4", [64, 4, T], BF16)
                VA = sbt(g, esB, "nVA", [128, TT, 2, 65], BF16)
                HID = sbt(g, esB, "nHID", [128, 2, 2, 128], BF16)
                HU = [sbt(g, esB, f"nHU{i}", [128, 128], F32) for i in range(3)]
                KCMPT = sbt(g, esB, "nKCMPT", [64, 128], BF16)
                OACC = sbt(g, esB, "nOACC", [128, 4, 4, 64], F32)
                OB = sbt(g, esB, "nOB", [128, 4, 256], BF16)
                EB_ = [sbt(g, esB, f"nE{i}", [128, 512], BF16) for i in range(4)]
                PT = [sbt(g, esB, f"nPT{i}", [128, 512], BF16) for i in range(4)]
                PTC = [sbt(g, esB, f"nPTC{i}", [128, 512], BF16) for i in range(4)]
                SELXM = [sbt(g, esB, f"nSELXM{i}", [128, 512], BF16) for i in range(3)]
                SELT = sbt(g, esB, "nSELT", [32, 512], BF16)
                RC = sbt(g, esB, "nRC", [128, 4, 97], F32)
                AC = sbt(g, esB, "nAC", [128, 16, 65], F32)
                ZR = sbt(g, esB, "nZR", [128, 16], F32)
                CO = sbt(g, esB, "nCO", [128, 16], F32)
                TMPA = sbt(g, esB, "nTMPA", [128, 16, 64], F32)
                IMP = sbt(g, esB, "nIMP", [128, 32], F32)
                TOP8 = sbt(g, esB, "nTOP8", [128, 8], F32)
                SELM = sbt(g, esB, "nSELM", [128, 32], BF16)
                OTK = [sbt(g, esB, f"nOTK{i}", [128, D], BF16) for i in range(2)]
                fw.op("pool", lambda: nc.gpsimd.memset(VA[:, :, :, 64:65], 1.0), writes=[K(VA, "ones")])
                for j in range(2):
                    for pq in range(4):
                        fw.dma("sp", lambda e: e.dma_start(
                            out=st1[0:64, 0:2048].rearrange("d (p h) -> d p h", p=8),
                            in_=w1[j, pq * 512:(pq + 1) * 512, :].rearrange("(p d) h -> d p h", d=64)), writes=[K(st1)])
                        fw.op("act", lambda: nc.scalar.copy(out=W1[:, j, pq * 8:(pq + 1) * 8, :],
                                                            in_=st1[0:64, 0:2048].rearrange("d (p h) -> d p h", p=8)),
                              reads=[K(st1)], writes=[K(W1, (j, pq))])
                if first[0]:
                    first[0] = False
                    for j in range(2):
                        for hc in range(2):
                            for p in range(32):
                                fw.op("pe", lambda: nc.tensor.matmul(out=ps[4][:, (j * 2 + hc):(j * 2 + hc) + 1],
                                                                     lhsT=W1[:, j, p, hc * 128:(hc + 1) * 128],
                                                                     rhs=POST[:, j, p:p + 1], start=(p == 0), stop=(p == 31)),
                                      reads=[K(W1), K(POST)], writes=[K(ps[4])], inc=(p == 31))
                    fw.op("dve", lambda: nc.vector.tensor_tensor(out=CONSTH[:].rearrange("p j c -> p (j c)"), in0=ps[4][:, 0:4],
                                                                 in1=B1[:].rearrange("p j c -> p (j c)"), op=ALU.add),
                          reads=[K(ps[4]), K(B1)], writes=[K(CONSTH)])
                cnt = {"s": 0, "s3": 0, "e": 0, "p": 0, "x": 0}

                def accum_evac(tb, gi, which):
                    for bk in range(3):
                        n = 6 if bk < 2 else 4
                        fw.op("dve", lambda: nc.vector.tensor_copy(out=AC[:, bk * 6:bk * 6 + n, :],
                                                                   in_=ps[5 + bk][:, 0:n * 65].rearrange("p (a c) -> p a c", c=65)),
                              reads=[K(ps[5 + bk])], writes=[K(AC, bk)])
                    fw.op("dve", lambda: nc.vector.tensor_scalar_max(out=ZR[:], in0=AC[:, :, 64], scalar1=1e-30), reads=[K(AC)], writes=[K(ZR)])
                    fw.op("dve", lambda: nc.vector.reciprocal(out=ZR[:], in_=ZR[:]), reads=[K(ZR)], writes=[K(ZR)])
                    gview = SGT[:, tb * 4:(tb + 1) * 4, gi * 12 + which:gi * 12 + 12:3]
                    fw.op("dve", lambda: nc.vector.tensor_tensor(out=CO[:].rearrange("p (t h) -> p t h", t=4),
                                                                 in0=ZR[:].rearrange("p (t h) -> p t h", t=4), in1=gview, op=ALU.mult),
                          reads=[K(ZR), K(SGT)], writes=[K(CO)])
                    fw.op("dve", lambda: nc.vector.tensor_tensor(out=TMPA[:], in0=AC[:, :, 0:64],
                                                                 in1=CO[:].unsqueeze(2).to_broadcast([128, 16, 64]), op=ALU.mult),
                          reads=[K(AC), K(CO)], writes=[K(TMPA)])
                    fw.op("pool", lambda: nc.gpsimd.tensor_tensor(out=OACC[:].rearrange("p t h d -> p (t h) d"),
                                                                  in0=OACC[:].rearrange("p t h d -> p (t h) d"), in1=TMPA[:], op=ALU.add),
                          reads=[K(OACC), K(TMPA)], writes=[K(OACC)])

                def acc_ap(h, tt):
                    a = tt * 4 + h
                    return ps[5 + a // 6], (a % 6) * 65

                def attend(tb, kts, kty, vslot, maskfn, which, gi, span):
                    for bk in range(3):
                        fw.op("dve", lambda: nc.vector.memset(ps[5 + bk][:, :], 0.0), writes=[K(ps[5 + bk])])
                    units = [(kt, h) for kt in kts for h in range(4)]

                    def cols(kt):
                        lo, hi = span(kt)
                        return lo * 128, (hi + 1) * 128

                    def emit_S(u):
                        kt, h = u
                        c_lo, c_hi = cols(kt)
                        pS = ps[1 + cnt["s3"] % 3]
                        cnt["s3"] += 1
                        fw.op("pe", lambda: nc.tensor.matmul(out=pS[:, c_lo:c_hi], lhsT=KT4[:, kty, kt * 128:(kt + 1) * 128],
                                                             rhs=QTg[:, h, tb * 512 + c_lo:tb * 512 + c_hi], start=True, stop=True),
                              reads=[K(KT4), K(QTg)], writes=[K(pS)])
                        return pS

                    pend = [emit_S(units[0])]
                    if len(units) > 1:
                        pend.append(emit_S(units[1]))
                    mk = None
                    masks = {}
                    for i, (kt, h) in enumerate(units):
                        pS = pend.pop(0)
                        if i + 2 < len(units):
                            pend.append(emit_S(units[i + 2]))
                        c_lo, c_hi = cols(kt)
                        if h == 0:
                            if kt not in masks:
                                masks[kt] = maskfn(kt, c_lo, c_hi)
                            mk = masks.pop(kt)
                        E = EB_[cnt["e"] % 4]
                        cnt["e"] += 1
                        fw.op("act", lambda: nc.scalar.activation(out=E[:, c_lo:c_hi], in_=pS[:, c_lo:c_hi], func=AF.Exp),
                              reads=[K(pS)], writes=[K(E)])
                        P = PT[cnt["p"] % 4]
                        cnt["p"] += 1
                        fw.op("dve", lambda: nc.vector.tensor_tensor(out=P[:, c_lo:c_hi], in0=E[:, c_lo:c_hi], in1=mk[0], op=ALU.mult),
                              reads=[K(E), mk[1]], writes=[K(P)])
                        if h == 1:
                            nk = kts.index(kt) + 1
                            if nk < len(kts):
                                masks[kts[nk]] = maskfn(kts[nk], *cols(kts[nk]))
                        lo, hi = span(kt)
                        for tt in range(lo, hi + 1):
                            pa, c0 = acc_ap(h, tt)
                            last = max(k2 for k2 in kts if span(k2)[0] <= tt <= span(k2)[1])
                            fw.op("pe", lambda: nc.tensor.matmul(out=pa[:, c0:c0 + 65], lhsT=P[:, tt * 128:(tt + 1) * 128],
                                                                 rhs=VA[:, kt, vslot, :], start=False, stop=(kt == last),
                                                                 skip_group_check=True),
                                  reads=[K(P), K(VA)], writes=[K(pa, c0)], inc=(kt == last))
                    accum_evac(tb, gi, which)

                for gi in range(4):
                    for h in range(4):
                        hg = gi * 4 + h
                        fw.dma("sp", lambda e: e.dma_start(out=QTg[:, h, :], in_=qS[hg * 64:(hg + 1) * 64, :]),
                               reads=[K("qS")], writes=[K(QTg, h)])
                    for ty in range(4):
                        fw.dma("sp", lambda e: e.dma_start(out=KT4[:, ty, :], in_=kS[ty, gi * 64:(gi + 1) * 64, :]),
                               reads=[K("kS")], writes=[K(KT4, ty)])
                    for a in range(2):
                        fw.dma("sp", lambda e: e.dma_start(
                            out=VA[:, :, a, 0:64],
                            in_=vS[:, gi * 128 + a * 64:gi * 128 + (a + 1) * 64].rearrange("(t p) d -> p t d", p=128)),
                            reads=[K("vS")], writes=[K(VA, ("v", a))])
                    for j in range(2):
                        for hc in range(2):
                            pS = ps[2 + (j * 2 + hc) % 2]
                            for p in range(32):
                                fw.op("pe", lambda: nc.tensor.matmul(out=pS[:, 0:127], lhsT=W1[:, j, p, hc * 128:(hc + 1) * 128],
                                                                     rhs=KT4[:, j, p:p + 16 * 126 + 1:16], start=(p == 0), stop=(p == 31)),
                                      reads=[K(W1), K(KT4, j)], writes=[K(pS)], inc=(p == 31))
                            u, u2, sg = HU
                            fw.op("act", lambda: nc.scalar.activation(out=u[:, 0:127], in_=pS[:, 0:127], func=AF.Identity,
                                                                      bias=CONSTH[:, j, hc:hc + 1]), reads=[K(pS), K(CONSTH)], writes=[K(u)])
                            fw.op("dve", lambda: nc.vector.tensor_tensor(out=u2[:, 0:127], in0=u[:, 0:127], in1=u[:, 0:127], op=ALU.mult),
                                  reads=[K(u)], writes=[K(u2)])
                            fw.op("dve", lambda: nc.vector.tensor_scalar(out=u2[:, 0:127], in0=u2[:, 0:127], scalar1=0.044715, scalar2=1.0,
                                                                         op0=ALU.mult, op1=ALU.add), reads=[K(u2)], writes=[K(u2)])
                            fw.op("dve", lambda: nc.vector.tensor_tensor(out=u2[:, 0:127], in0=u2[:, 0:127], in1=u[:, 0:127], op=ALU.mult),
                                  reads=[K(u2), K(u)], writes=[K(u2)])
                            fw.op("act", lambda: nc.scalar.activation(out=sg[:, 0:127], in_=u2[:, 0:127], func=AF.Sigmoid, scale=1.5957691216),
                                  reads=[K(u2)], writes=[K(sg)])
                            fw.op("dve", lambda: nc.vector.tensor_tensor(out=HID[:, j, hc, 0:127], in0=u[:, 0:127], in1=sg[:, 0:127], op=ALU.mult),
                                  reads=[K(u), K(sg)], writes=[K(HID, (j, hc))])
                    pS = ps[2]
                    for hc in range(2):
                        fw.op("pe", lambda: nc.tensor.matmul(out=pS[0:64, 0:127], lhsT=W2[:, 0, hc, :], rhs=HID[:, 0, hc, 0:127],
                                                             start=(hc == 0), stop=(hc == 1)),
                              reads=[K(W2), K(HID)], writes=[K(pS)], inc=(hc == 1))
                    fw.op("act", lambda: nc.scalar.activation(out=KCMPT[:, 0:127], in_=pS[0:64, 0:127], func=AF.Identity, bias=B2K[:, 0:1]),
                          reads=[K(pS), K(B2K)], writes=[K(KCMPT)])
                    pS = ps[3]
                    for hc in range(2):
                        fw.op("pe", lambda: nc.tensor.matmul(out=pS[0:127, 0:64], lhsT=HID[:, 1, hc, 0:127], rhs=W2[:, 1, hc, :],
                                                             start=(hc == 0), stop=(hc == 1)),
                              reads=[K(W2), K(HID)], writes=[K(pS)], inc=(hc == 1))
                    fw.op("dve", lambda: nc.vector.tensor_tensor(out=VCA[0:127, 0:64], in0=pS[0:127, 0:64], in1=B2V[0:127, :], op=ALU.add),
                          reads=[K(pS), K(B2V)], writes=[K(VCA, "v")])
                    for tb in range(4):
                        for h in range(4):
                            pS = ps[2 + cnt["s"] % 2]
                            cnt["s"] += 1
                            fw.op("pe", lambda: nc.tensor.matmul(out=pS[0:127, :], lhsT=KCMPT[:, 0:127],
                                                                 rhs=QTg[:, h, tb * 512:(tb + 1) * 512], start=True, stop=True),
                                  reads=[K(KCMPT), K(QTg)], writes=[K(pS)])
                            E = EB_[cnt["e"] % 3]
                            cnt["e"] += 1
                            fw.op("act", lambda: nc.scalar.activation(out=E[0:127, :], in_=pS[0:127, :], func=AF.Exp), reads=[K(pS)], writes=[K(E)])
                            fw.op("dve", lambda: nc.vector.tensor_tensor(out=PTC[h][0:127, :], in0=E[0:127, :],
                                                                         in1=CMPM[0:127, tb * 512:(tb + 1) * 512], op=ALU.mult),
                                  reads=[K(E), K(CMPM)], writes=[K(PTC[h])])
                        for tt in range(4):
                            tile = tb * 4 + tt
                            for h in range(4):
                                fw.op("pe", lambda: nc.tensor.matmul(out=ps[4][:, h * 97:(h + 1) * 97], lhsT=PTC[h][0:127, tt * 128:(tt + 1) * 128],
                                                                     rhs=VCA[0:127, :], start=True, stop=True),
                                      reads=[K(PTC[h]), K(VCA)], writes=[K(ps[4])], inc=(h == 3))
                            fw.op("dve", lambda: nc.vector.tensor_copy(out=RC[:], in_=ps[4][:, 0:388].rearrange("p (h c) -> p h c", h=4)),
                                  reads=[K(ps[4])], writes=[K(RC)])
                            fw.op("dve", lambda: nc.vector.tensor_scalar_max(out=ZR[:, 0:4], in0=RC[:, :, 64], scalar1=1e-30),
                                  reads=[K(RC)], writes=[K(ZR)])
                            fw.op("dve", lambda: nc.vector.reciprocal(out=ZR[:, 0:4], in_=ZR[:, 0:4]), reads=[K(ZR)], writes=[K(ZR)])
                            fw.op("dve", lambda: nc.vector.tensor_scalar(out=IMP[:], in0=RC[:, 0, 65:97], scalar1=ZR[:, 0:1], scalar2=None,
                                                                         op0=ALU.mult), reads=[K(RC), K(ZR)], writes=[K(IMP)])
                            for h in range(1, 4):
                                fw.op("dve", lambda: nc.vector.scalar_tensor_tensor(out=IMP[:], in0=RC[:, h, 65:97], scalar=ZR[:, h:h + 1],
                                                                                    in1=IMP[:], op0=ALU.mult, op1=ALU.add),
                                      reads=[K(RC), K(ZR), K(IMP)], writes=[K(IMP)])
                            fw.op("dve", lambda: nc.vector.tensor_tensor(out=IMP[:], in0=IMP[:], in1=MULA[:, 0, tile, :], op=ALU.mult),
                                  reads=[K(IMP), K(MULA)], writes=[K(IMP)])
                            fw.op("dve", lambda: nc.vector.tensor_tensor(out=IMP[:], in0=IMP[:], in1=MULA[:, 1, tile, :], op=ALU.add),
                                  reads=[K(IMP), K(MULA)], writes=[K(IMP)])
                            fw.op("dve", lambda: nc.vector.max(out=TOP8[:], in_=IMP[:]), reads=[K(IMP)], writes=[K(TOP8)])
                            fw.op("dve", lambda: nc.vector.tensor_scalar(out=SELM[:], in0=IMP[:], scalar1=TOP8[:, 7:8], scalar2=None,
                                                                         op0=ALU.is_ge), reads=[K(IMP), K(TOP8)], writes=[K(SELM)])
                            fw.op("pe", lambda: nc.tensor.transpose(out=ps[0][0:32, tt * 128:(tt + 1) * 128], in_=SELM[:], identity=g.identb[:]),
                                  reads=[K(SELM), K(g.identb)], writes=[K(ps[0])])
                            fw.op("dve", lambda: nc.vector.tensor_tensor(out=CO[:, 0:4], in0=ZR[:, 0:4], in1=SGT[:, tile, gi * 12:gi * 12 + 12:3],
                                                                         op=ALU.mult), reads=[K(ZR), K(SGT)], writes=[K(CO)])
                            fw.op("dve", lambda: nc.vector.tensor_tensor(out=OACC[:, tt, :, :], in0=RC[:, :, 0:64],
                                                                         in1=CO[:, 0:4].unsqueeze(2).to_broadcast([128, 4, 64]), op=ALU.mult),
                                  reads=[K(RC), K(CO)], writes=[K(OACC)])
                        fw.op("act", lambda: nc.scalar.copy(out=SELT[:], in_=ps[0][0:32, 0:512]), reads=[K(ps[0])], writes=[K(SELT)])

                        def sel_mask(kt, c_lo, c_hi, tb=tb):
                            delta = tb * 512 - kt * 128
                            M = SELXM[cnt["x"] % 3]
                            cnt["x"] += 1
                            fw.op("pe", lambda: nc.tensor.matmul(out=ps[4][:, c_lo:c_hi], lhsT=EXPB[:, kt * 128:(kt + 1) * 128], rhs=SELT[:, c_lo:c_hi],
                                                                 start=True, stop=True), reads=[K(EXPB), K(SELT)], writes=[K(ps[4])])
                            if delta <= 0:
                                di = (delta + 384) // 128
                                fw.op("dve", lambda: nc.vector.tensor_tensor(out=M[:, c_lo:c_hi], in0=ps[4][:, c_lo:c_hi], in1=WINM[:, di, c_lo:c_hi], op=ALU.mult),
                                      reads=[K(ps[4]), K(WINM)], writes=[K(M)])
                            else:
                                fw.op("act", lambda: nc.scalar.copy(out=M[:, c_lo:c_hi], in_=ps[4][:, c_lo:c_hi]), reads=[K(ps[4])], writes=[K(M)])
                            return (M[:, c_lo:c_hi], K(M))

                        def win_mask(kt, c_lo, c_hi, tb=tb):
                            di = (tb * 512 - kt * 128 + 384) // 128
                            return (WINM[:, di, c_lo:c_hi], K(WINM))

                        attend(tb, list(range(max(0, tb * 4 - 4), tb * 4 + 4)), 3, 1, win_mask, 2, gi,
                               lambda kt, tb=tb: (max(0, kt - tb * 4), min(3, kt + 4 - tb * 4)))
                        attend(tb, list(range(0, tb * 4 + 4)), 2, 0, sel_mask, 1, gi,
                               lambda kt, tb=tb: (max(0, kt - tb * 4), 3))
                        fw.op("act", lambda: nc.scalar.copy(out=OB[:], in_=OACC[:].rearrange("p t h d -> p t (h d)")),
                              reads=[K(OACC)], writes=[K(OB)])
                        fw.dma("sp", lambda e: e.dma_start(
                            out=oS[tb * 512:(tb + 1) * 512, gi * 256:(gi + 1) * 256].rearrange("(t p) c -> p t c", p=128), in_=OB[:]),
                            reads=[K(OB)], writes=[K("oS", (tb, gi))])
                def pre(t):
                    fw.dma("sp", lambda e: e.dma_start(out=OTK[t % 2][:], in_=oS[t * 128:(t + 1) * 128, :]), reads=[K("oS")], writes=[K(OTK[t % 2])])
                    tail_load_x(g, X, Xname, row0, t, tb_bufs)

                pre(0)
                for t in range(TT):
                    ok = OTK[t % 2]
                    if t + 1 < TT:
                        pre(t + 1)
                    mixer_tail(g, esB, l, X, Xname, XO, XOname, row0, t, ok[:], WO, lng, lnb, tb_bufs, K(ok), xloaded=True)
            fw.barrier()


MIXERS[0] = phase_nsa


class ColView:
    def __init__(self, tile, n):
        self.tile, self.n, self.name = tile, n, tile.name

    def __getitem__(self, key):
        return self.tile[:, 0:self.n][key]


class RowView:
    def __init__(self, tile, n):
        self.tile, self.n, self.name = tile, n, tile.name

    def __getitem__(self, key):
        return self.tile[0:4, 0:self.n][key]


def row_scan(g, a, b, n, op):
    nc, fw = g.nc, g.fw
    s = 1
    src, dst = a, b
    while s < n:
        fw.op("dve", lambda: nc.vector.tensor_tensor(out=dst[:, s:n], in0=src[:, s:n], in1=src[:, 0:n - s], op=op),
              reads=[K(src)], writes=[K(dst, "hi")])
        fw.op("dve", lambda: nc.vector.tensor_copy(out=dst[:, 0:s], in_=src[:, 0:s]), reads=[K(src)], writes=[K(dst, "lo")])
        src, dst = dst, src
        s *= 2
    return src, dst


def phase_mlstm(g, l, X, Xname, XO, XOname):
    nc, fw, ps = g.nc, g.fw, g.ps
    sl = l // 3
    nseq = g.NT // T
    w_in = g.w["ml_w_in"][sl]
    qkS, vS, sgS = g.qT_scr, g.v2_scr, g.sg_scr
    LNS = float(np.log(128.0 ** -0.5))
    with ExitStack() as es:
        st1 = sbt(g, es, "mST", [128, 3080], F32)
        CAUS = sbt(g, es, "mCAUS", [128, 4, 512], BF16)
        fw.dma("sp", lambda e: e.dma_start(out=st1[:, 0:2048], in_=g.nsac[:, 2048:4096]), writes=[K(st1)])
        fw.op("dve", lambda: nc.vector.tensor_copy(out=CAUS[:], in_=st1[:, 0:2048].rearrange("p (a b) -> p a b", a=4)),
              reads=[K(st1)], writes=[K(CAUS)])
        CW = sbt(g, es, "mCW", [128, 8, 4], F32)
        CB = sbt(g, es, "mCB", [128, 8], F32)
        GBI = sbt(g, es, "mGBI", [4, 1], F32)
        GBF = sbt(g, es, "mGBF", [4, 1], F32)
        NG = sbt(g, es, "mNG", [128, D], F32)
        ONESB = sbt(g, es, "mONES", [128, 1], BF16)
        SELH = sbt(g, es, "mSELH", [4, 4, 128], F32)
        cT = sbt(g, es, "mcT", [128, TT, 4], F32)
        emmT = sbt(g, es, "memmT", [128, TT, 4], F32)
        NA = sbt(g, es, "mNA", [4, T], F32)
        for j in range(4):
            fw.dma("sp", lambda e: e.dma_start(out=CW[:, :, j], in_=g.w["ml_conv_w"][sl, j, :].rearrange("(c p) -> p c", p=128),
                                               allow_slow_non_contiguous=True), writes=[K(CW, j)])
        fw.dma("sp", lambda e: e.dma_start(out=CB[:], in_=g.w["ml_conv_b"][sl].rearrange("(c p) -> p c", p=128),
                                           allow_slow_non_contiguous=True), writes=[K(CB)])
        fw.dma("sp", lambda e: e.dma_start(out=GBI[:], in_=g.w["ml_gate_b"][sl, 0:4].rearrange("(h o) -> h o", o=1),
                                           allow_slow_non_contiguous=True), writes=[K(GBI)])
        fw.dma("sp", lambda e: e.dma_start(out=GBF[:], in_=g.w["ml_gate_b"][sl, 4:8].rearrange("(h o) -> h o", o=1),
                                           allow_slow_non_contiguous=True), writes=[K(GBF)])
        fw.dma("sp", lambda e: e.dma_start(out=NG[:], in_=g.w["ml_norm_g"][sl, :].partition_broadcast(128)), writes=[K(NG)])
        fw.op("dve", lambda: nc.vector.memset(ONESB[:], 1.0), writes=[K(ONESB)])
        fw.op("dve", lambda: nc.vector.tensor_copy(out=SELH[:], in_=g.ident[0:4, 0:4].unsqueeze(2).to_broadcast([4, 4, 128])),
              reads=[K(g.ident)], writes=[K(SELH)])
        fw.op("dve", lambda: nc.vector.tensor_scalar_mul(out=GBF[:], in0=GBF[:], scalar1=-1.0), reads=[K(GBF)], writes=[K(GBF)])
        WO, lng, lnb, tb_bufs = load_tail_weights(g, es, l, g.w["ml_w_out"][sl], [st1, st1])

        for s in range(nseq):
            row0 = s * T
            with ExitStack() as esA:
                WI = sbt(g, esA, "mWI", [128, 8, 3080], BF16)
                XT = sbt(g, esA, "mXT", [128, 8, T], BF16)
                PRE = sbt(g, esA, "mPRE", [128, T + 3], F32)
                ACC = sbt(g, esA, "mACC", [128, T], F32)
                QKB = sbt(g, esA, "mQKB", [128, T], BF16)
                EV = [sbt(g, esA, f"mEV{i}", [128, 512], BF16) for i in range(3)]
                R0 = sbt(g, esA, "mR0", [4, T], F32)
                R1 = sbt(g, esA, "mR1", [4, T], F32)
                R2 = RowView(ACC, T)
                R3 = RowView(PRE, T)
                load_w_bf16(g, None, w_in, 3080, [st1, st1],
                            lambda kc, st: (fw.op("act", lambda: nc.scalar.copy(out=WI[:, kc, 0:1540], in_=st[:, 0:1540]),
                                                  reads=[K(st)], writes=[K(WI, (kc, 0))]),
                                            fw.op("dve", lambda: nc.vector.tensor_copy(out=WI[:, kc, 1540:3080], in_=st[:, 1540:3080]),
                                                  reads=[K(st)], writes=[K(WI, (kc, 1))])))
                build_xT(g, esA, X, Xname, row0, XT, tb_bufs["xt"])
                fw.op("dve", lambda: nc.vector.memset(PRE[:, 0:3], 0.0), writes=[K(PRE, "pad")])
                np_ = [0]
                for c in range(8):
                    for tb in range(4):
                        pS = ps[2 + np_[0] % 2]
                        np_[0] += 1
                        for kc in range(8):
                            fw.op("pe", lambda: nc.tensor.matmul(out=pS[:, :], lhsT=WI[:, kc, c * 128:(c + 1) * 128],
                                                                 rhs=XT[:, kc, tb * 512:(tb + 1) * 512], start=(kc == 0), stop=(kc == 7)),
                                  reads=[K(WI), K(XT)], writes=[K(pS)], inc=(kc == 7))
                        fw.op("act", lambda: nc.scalar.copy(out=PRE[:, 3 + tb * 512:3 + (tb + 1) * 512], in_=pS[:, :]),
                              reads=[K(pS)], writes=[K(PRE, tb)])
                    fw.op("dve", lambda: nc.vector.tensor_scalar(out=ACC[:], in0=PRE[:, 3:T + 3], scalar1=CW[:, c, 3:4], scalar2=None,
                                                                 op0=ALU.mult), reads=[K(PRE), K(CW)], writes=[K(ACC)])
                    for j in range(3):
                        eng = ("dve", nc.vector)
                        fw.op(eng[0], lambda: eng[1].scalar_tensor_tensor(out=ACC[:], in0=PRE[:, j:T + j], scalar=CW[:, c, j:j + 1],
                                                                          in1=ACC[:], op0=ALU.mult, op1=ALU.add),
                              reads=[K(PRE), K(CW), K(ACC)], writes=[K(ACC)])
                    fw.op("act", lambda: nc.scalar.activation(out=QKB[:], in_=ACC[:], func=AF.Silu, bias=CB[:, c:c + 1]),
                          reads=[K(ACC), K(CB)], writes=[K(QKB)])
                    fw.dma("sp", lambda e: e.dma_start(out=qkS[c * 128:(c + 1) * 128, :], in_=QKB[:]), reads=[K(QKB)], writes=[K("qkS", c)])
                nev = [0]

                def evac_store(pS, dst_ap, dkey, sig):
                    E = EV[nev[0] % 3]
                    nev[0] += 1
                    if sig:
                        fw.op("act", lambda: nc.scalar.activation(out=E[:], in_=pS[:, :], func=AF.Sigmoid), reads=[K(pS)], writes=[K(E)])
                    else:
                        fw.op("dve", lambda: nc.vector.tensor_copy(out=E[:], in_=pS[:, :]), reads=[K(pS)], writes=[K(E)])
                    fw.dma("sp", lambda e: e.dma_start(out=dst_ap, in_=E[:]), reads=[K(E)], writes=[dkey])

                for t in range(TT):
                    for (c0, dstS, nm, sig) in ((1024, vS, "vS2", False), (2048, sgS, "sgS", True)):
                        for nh in range(2):
                            pS = ps[2 + np_[0] % 2]
                            np_[0] += 1
                            for kc in range(8):
                                fw.op("pe", lambda: nc.tensor.matmul(out=pS[:, :], lhsT=XT[:, kc, t * 128:(t + 1) * 128],
                                                                     rhs=WI[:, kc, c0 + nh * 512:c0 + (nh + 1) * 512],
                                                                     start=(kc == 0), stop=(kc == 7)),
                                      reads=[K(XT), K(WI)], writes=[K(pS)], inc=(kc == 7))
                            evac_store(pS, dstS[t * 128:(t + 1) * 128, nh * 512:(nh + 1) * 512], K(nm, (t, nh)), sig)
                for (c0, R) in ((3072, R0), (3076, R1)):
                    for tb in range(4):
                        pS = ps[2 + np_[0] % 2]
                        np_[0] += 1
                        for kc in range(8):
                            fw.op("pe", lambda: nc.tensor.matmul(out=pS[0:4, :], lhsT=WI[:, kc, c0:c0 + 4],
                                                                 rhs=XT[:, kc, tb * 512:(tb + 1) * 512], start=(kc == 0), stop=(kc == 7)),
                                  reads=[K(WI), K(XT)], writes=[K(pS)], inc=(kc == 7))
                        if c0 == 3072:
                            fw.op("act", lambda: nc.scalar.activation(out=R[:, tb * 512:(tb + 1) * 512], in_=pS[0:4, :], func=AF.Identity,
                                                                      bias=GBI[:, 0:1]), reads=[K(pS), K(GBI)], writes=[K(R, tb)])
                        else:
                            fw.op("act", lambda: nc.scalar.activation(out=R[:, tb * 512:(tb + 1) * 512], in_=pS[0:4, :], func=AF.Exp,
                                                                      bias=GBF[:, 0:1], scale=-1.0), reads=[K(pS), K(GBF)], writes=[K(R, tb)])
                fw.op("act", lambda: nc.scalar.activation(out=R1[:], in_=R1[:], func=AF.Ln, bias=1.0), reads=[K(R1)], writes=[K(R1)])
                fw.op("dve", lambda: nc.vector.tensor_scalar_mul(out=R1[:], in0=R1[:], scalar1=-1.0), reads=[K(R1)], writes=[K(R1)])
                Bt, free = row_scan(g, R1, R2, T, ALU.add)
                other = R3
                fw.op("dve", lambda: nc.vector.tensor_tensor(out=other[:], in0=R0[:], in1=Bt[:], op=ALU.subtract),
                      reads=[K(R0), K(Bt)], writes=[K(other)])
                Cm, free2 = row_scan(g, other, free, T, ALU.max)
                fw.op("dve", lambda: nc.vector.tensor_scalar(out=NA[:], in0=Cm[:], scalar1=0.0, scalar2=-1.0, op0=ALU.max, op1=ALU.mult),
                      reads=[K(Cm)], writes=[K(NA)])
                fw.op("dve", lambda: nc.vector.tensor_tensor(out=free2[:], in0=NA[:], in1=Bt[:], op=ALU.subtract),
                      reads=[K(NA), K(Bt)], writes=[K(free2)])
                fw.op("act", lambda: nc.scalar.activation(out=free2[:], in_=free2[:], func=AF.Exp), reads=[K(free2)], writes=[K(free2)])
                fw.op("dve", lambda: nc.vector.tensor_tensor(out=R0[:], in0=R0[:], in1=Bt[:], op=ALU.subtract),
                      reads=[K(R0), K(Bt)], writes=[K(R0)])
                fw.op("dve", lambda: nc.vector.tensor_scalar_add(out=R0[:], in0=R0[:], scalar1=LNS), reads=[K(R0)], writes=[K(R0)])
                for (src, dstT) in ((R0, cT), (free2, emmT)):
                    for t in range(TT):
                        fw.op("pe", lambda: nc.tensor.transpose(out=ps[4][:, t * 4:(t + 1) * 4], in_=src[0:4, t * 128:(t + 1) * 128],
                                                                identity=g.ident[0:4, 0:4]),
                              reads=[K(src), K(g.ident)], writes=[K(ps[4])], inc=(t == TT - 1))
                    fw.op("dve", lambda: nc.vector.tensor_copy(out=dstT[:].rearrange("p t h -> p (t h)"), in_=ps[4][:, 0:TT * 4]),
                          reads=[K(ps[4])], writes=[K(dstT)])
            fw.barrier()
            with ExitStack() as esB:
                QT = sbt(g, esB, "mQT", [128, 4, T], BF16)
                KT = sbt(g, esB, "mKT", [128, 4, T], BF16)
                V = sbt(g, esB, "mV", [128, TT, D], BF16)
                EF = [sbt(g, esB, f"mEF{i}", [128, 512], F32) for i in range(3)]
                PT = [sbt(g, esB, f"mPT{i}", [128, 512], BF16) for i in range(4)]
                HN = sbt(g, esB, "mHN", [128, 4, D], F32)
                DEN = sbt(g, esB, "mDEN", [128, 4], F32)
                SQ = sbt(g, esB, "mSQ", [128, D], F32)
                MS = sbt(g, esB, "mMS", [128, 4], F32)
                SGt = [sbt(g, esB, f"mSGt{i}", [128, D], BF16) for i in range(2)]
                OTK = [sbt(g, esB, f"mOTK{i}", [128, D], BF16) for i in range(2)]
                for h in range(4):
                    fw.dma("sp", lambda e: e.dma_start(out=QT[:, h, :], in_=qkS[h * 128:(h + 1) * 128, :]), reads=[K("qkS")], writes=[K(QT, h)])
                    fw.dma("sp", lambda e: e.dma_start(out=KT[:, h, :], in_=qkS[512 + h * 128:512 + (h + 1) * 128, :]),
                           reads=[K("qkS")], writes=[K(KT, h)])
                for t4 in range(4):
                    fw.dma("sp", lambda e: e.dma_start(out=V[:, t4 * 4:(t4 + 1) * 4, :],
                                                       in_=vS[t4 * 512:(t4 + 1) * 512, :].rearrange("(t p) d -> p t d", p=128)),
                           reads=[K("vS2")], writes=[K(V, t4)])
                cnt = {"s": 0, "e": 0, "p": 0}
                for tb in range(4):
                    for h in range(4):
                        fw.op("pe", lambda: nc.tensor.matmul(out=ps[4][:, :], lhsT=SELH[:, h, :], rhs=NA[:, tb * 512:(tb + 1) * 512],
                                                             start=True, stop=True), reads=[K(SELH), K(NA)], writes=[K(ps[4])])
                        for bk in (5, 6, 7):
                            fw.op("dve", lambda: nc.vector.memset(ps[bk][:, :], 0.0), writes=[K(ps[bk])])
                        def emit_S(kt, h=h, tb=tb):
                            pS_ = ps[1 + cnt["s"] % 3]
                            cnt["s"] += 1
                            cl = max(0, kt - tb * 4) * 128
                            fw.op("pe", lambda: nc.tensor.matmul(out=pS_[:, cl:512], lhsT=KT[:, h, kt * 128:(kt + 1) * 128],
                                                                 rhs=QT[:, h, tb * 512 + cl:(tb + 1) * 512], start=True, stop=True),
                                  reads=[K(KT), K(QT)], writes=[K(pS_)])
                            return pS_

                        pend = [emit_S(0), emit_S(1)]
                        for kt in range(tb * 4 + 4):
                            pS = pend.pop(0)
                            if kt + 2 < tb * 4 + 4:
                                pend.append(emit_S(kt + 2))
                            E = EF[cnt["e"] % 3]
                            cnt["e"] += 1
                            cl = max(0, kt - tb * 4) * 128
                            fw.op("act", lambda: nc.scalar.activation(out=E[:, cl:512], in_=ps[4][:, cl:512], func=AF.Exp, bias=cT[:, kt, h:h + 1]),
                                  reads=[K(ps[4]), K(cT)], writes=[K(E)])
                            P = PT[cnt["p"] % 4]
                            cnt["p"] += 1
                            fw.op("dve", lambda: nc.vector.tensor_tensor(out=P[:, cl:512], in0=pS[:, cl:512], in1=E[:, cl:512], op=ALU.mult),
                                  reads=[K(pS), K(E)], writes=[K(P)])
                            if kt >= tb * 4:
                                di = (tb * 512 - kt * 128 + 384) // 128
                                fw.op("pool", lambda: nc.gpsimd.tensor_tensor(out=P[:, cl:512], in0=P[:, cl:512], in1=CAUS[:, di, cl:512], op=ALU.mult),
                                      reads=[K(P), K(CAUS)], writes=[K(P)])
                            for tt in range(4):
                                if kt > tb * 4 + tt:
                                    continue
                                last = (kt == tb * 4 + tt)
                                pn = ps[5 + tt // 2]
                                c0 = (tt % 2) * 256
                                fw.op("pe", lambda: nc.tensor.matmul(out=pn[:, c0:c0 + 256], lhsT=P[:, tt * 128:(tt + 1) * 128],
                                                                     rhs=V[:, kt, h * 256:(h + 1) * 256], start=False, stop=last,
                                                                     skip_group_check=True),
                                      reads=[K(P), K(V)], writes=[K(pn, c0)])
                                fw.op("pe", lambda: nc.tensor.matmul(out=ps[7][:, tt:tt + 1], lhsT=P[:, tt * 128:(tt + 1) * 128],
                                                                     rhs=ONESB[:, 0:1], start=False, stop=last, skip_group_check=True),
                                      reads=[K(P), K(ONESB)], writes=[K(ps[7], tt)], inc=last)
                        fw.op("dve", lambda: nc.vector.tensor_scalar_mul(out=MS[:], in0=ps[7][:, 0:4], scalar1=-1.0),
                              reads=[K(ps[7])], writes=[K(MS)])
                        fw.op("dve", lambda: nc.vector.tensor_tensor(out=DEN[:], in0=ps[7][:, 0:4], in1=MS[:], op=ALU.max),
                              reads=[K(ps[7]), K(MS)], writes=[K(DEN)])
                        fw.op("dve", lambda: nc.vector.tensor_tensor(out=DEN[:], in0=DEN[:], in1=emmT[:, tb * 4:(tb + 1) * 4, h],
                                                                     op=ALU.max), reads=[K(DEN), K(emmT)], writes=[K(DEN)])
                        fw.op("dve", lambda: nc.vector.reciprocal(out=DEN[:], in_=DEN[:]), reads=[K(DEN)], writes=[K(DEN)])
                        for tt in range(4):
                            pn = ps[5 + tt // 2]
                            c0 = (tt % 2) * 256
                            fw.op("act", lambda: nc.scalar.activation(out=HN[:, tt, h * 256:(h + 1) * 256], in_=pn[:, c0:c0 + 256],
                                                                      func=AF.Copy, scale=DEN[:, tt:tt + 1]),
                                  reads=[K(pn), K(DEN)], writes=[K(HN, (tt, h))])
                    def pre(t):
                        fw.dma("sp", lambda e: e.dma_start(out=SGt[t % 2][:], in_=sgS[t * 128:(t + 1) * 128, :]), reads=[K("sgS")], writes=[K(SGt[t % 2])])
                        tail_load_x(g, X, Xname, row0, t, tb_bufs)

                    pre(tb * 4)
                    for tt in range(4):
                        t = tb * 4 + tt
                        sgt, ok = SGt[t % 2], OTK[t % 2]
                        if tt < 3:
                            pre(t + 1)
                        fw.op("pool", lambda: nc.gpsimd.tensor_tensor(out=SQ[:], in0=HN[:, tt, :], in1=HN[:, tt, :], op=ALU.mult),
                              reads=[K(HN)], writes=[K(SQ)])
                        fw.op("dve", lambda: nc.vector.tensor_reduce(out=MS[:], in_=SQ[:].rearrange("p (h d) -> p h d", h=4), axis=AX.X, op=ALU.add),
                              reads=[K(SQ)], writes=[K(MS)])
                        fw.op("dve", lambda: nc.vector.tensor_scalar(out=MS[:], in0=MS[:], scalar1=1.0 / 256, scalar2=1e-6, op0=ALU.mult, op1=ALU.add),
                              reads=[K(MS)], writes=[K(MS)])
                        fw.op("act", lambda: nc.scalar.sqrt(out=MS[:], in_=MS[:]), reads=[K(MS)], writes=[K(MS)])
                        fw.op("dve", lambda: nc.vector.reciprocal(out=MS[:], in_=MS[:]), reads=[K(MS)], writes=[K(MS)])
                        fw.op("dve", lambda: nc.vector.tensor_tensor(out=SQ[:].rearrange("p (h d) -> p h d", h=4),
                                                                     in0=HN[:, tt, :].rearrange("p (h d) -> p h d", h=4),
                                                                     in1=MS[:].unsqueeze(2).to_broadcast([128, 4, 256]), op=ALU.mult),
                              reads=[K(HN), K(MS)], writes=[K(SQ)])
                        fw.op("pool", lambda: nc.gpsimd.tensor_tensor(out=SQ[:], in0=SQ[:], in1=NG[:], op=ALU.mult),
                              reads=[K(SQ), K(NG)], writes=[K(SQ)])
                        fw.op("dve", lambda: nc.vector.tensor_tensor(out=ok[:], in0=SQ[:], in1=sgt[:], op=ALU.mult),
                              reads=[K(SQ), K(sgt)], writes=[K(ok)])
                        mixer_tail(g, esB, l, X, Xname, XO, XOname, row0, t, ok[:], WO, lng, lnb, tb_bufs, K(ok), xloaded=True)
            fw.barrier()


MIXERS[1] = phase_mlstm


HL = 64


def phase_hgrn(g, l, X, Xname, XO, XOname):
    nc, fw, ps = g.nc, g.fw, g.ps
    sl = l // 3
    nseq = g.NT // T
    w_in = g.w["hg_w_in"][sl]
    qdS = g.qT_scr
    kdS = g.kT_scr.rearrange("a c t -> (a c) t")
    vS, sgS, kkS = g.v2_scr, g.sg_scr, g.o_scr
    NCH = T // HL
    with ExitStack() as es:
        st1 = sbt(g, es, "hST", [128, 4096], F32)
        NG = sbt(g, es, "hNG", [128, D], F32)
        LBb = sbt(g, es, "hLBb", [128, D], F32)
        OMb = sbt(g, es, "hOMb", [128, D], F32)
        LBp = sbt(g, es, "hLBp", [128, 8], F32)
        OMp = sbt(g, es, "hOMp", [128, 8], F32)
        HLp = sbt(g, es, "hHLp", [128, 8, 4], F32)
        UM = sbt(g, es, "hUM", [128, 128], BF16)
        SUFU = sbt(g, es, "hSUFU", [128, 128], F32)
        EGE = sbt(g, es, "hEGE", [128, 8, NCH], F32)
        fw.dma("sp", lambda e: e.dma_start(out=NG[:], in_=g.w["hg_norm_g"][sl, :].partition_broadcast(128)), writes=[K(NG)])
        fw.op("dve", lambda: nc.vector.tensor_tensor(out=UM[:], in0=g.C[:, 0:128], in1=g.C[:, 128:256], op=ALU.add),
              reads=[K(g.C)], writes=[K(UM)])
        fw.op("dve", lambda: nc.vector.tensor_copy(out=SUFU[:], in_=g.C[:, 3 * 128 + NB_MAX + 8:3 * 128 + NB_MAX + 8 + 128]),
              reads=[K(g.C)], writes=[K(SUFU)])
        for r in range(4):
            fw.dma("sp", lambda e: e.dma_start(out=st1[:, r * 1024:(r + 1) * 1024], in_=g.w["hg_lower"][r, :].partition_broadcast(128)),
                   writes=[K(st1, r)])
            fw.dma("sp", lambda e: e.dma_start(out=HLp[:, :, r], in_=g.w["hg_lower"][r, :].rearrange("(c p) -> p c", p=128),
                                               allow_slow_non_contiguous=True), writes=[K(HLp, r)])
        fw.op("act", lambda: nc.scalar.activation(out=st1[:], in_=st1[:], func=AF.Exp), reads=[K(st1)], writes=[K(st1)])
        fw.op("act", lambda: nc.scalar.activation(out=HLp[:], in_=HLp[:], func=AF.Exp), reads=[K(HLp)], writes=[K(HLp)])
        e4 = st1[:].rearrange("p (r d) -> p r d", r=4)
        fw.op("dve", lambda: nc.vector.tensor_tensor(out=OMb[:], in0=e4[:, 0, :], in1=e4[:, 1, :], op=ALU.add), reads=[K(st1)], writes=[K(OMb)])
        fw.op("dve", lambda: nc.vector.tensor_tensor(out=OMb[:], in0=OMb[:], in1=e4[:, 2, :], op=ALU.add), reads=[K(st1), K(OMb)], writes=[K(OMb)])
        fw.op("dve", lambda: nc.vector.tensor_tensor(out=OMb[:], in0=OMb[:], in1=e4[:, 3, :], op=ALU.add), reads=[K(st1), K(OMb)], writes=[K(OMb)])
        fw.op("dve", lambda: nc.vector.reciprocal(out=OMb[:], in_=OMb[:]), reads=[K(OMb)], writes=[K(OMb)])
        fw.op("dve", lambda: nc.vector.tensor_copy(out=LBb[:], in_=e4[:, 1, :]), reads=[K(st1)], writes=[K(LBb)])
        for i in range(2, l + 1):
            fw.op("dve", lambda: nc.vector.tensor_tensor(out=LBb[:], in0=LBb[:], in1=e4[:, i, :], op=ALU.add), reads=[K(st1), K(LBb)], writes=[K(LBb)])
        fw.op("dve", lambda: nc.vector.tensor_tensor(out=LBb[:], in0=LBb[:], in1=OMb[:], op=ALU.mult), reads=[K(LBb), K(OMb)], writes=[K(LBb)])
        fw.op("dve", lambda: nc.vector.tensor_scalar(out=OMb[:], in0=LBb[:], scalar1=-1.0, scalar2=1.0, op0=ALU.mult, op1=ALU.add),
              reads=[K(LBb)], writes=[K(OMb)])
        fw.op("dve", lambda: nc.vector.tensor_reduce(out=OMp[:], in_=HLp[:], axis=AX.X, op=ALU.add), reads=[K(HLp)], writes=[K(OMp)])
        fw.op("dve", lambda: nc.vector.reciprocal(out=OMp[:], in_=OMp[:]), reads=[K(OMp)], writes=[K(OMp)])
        fw.op("dve", lambda: nc.vector.tensor_reduce(out=LBp[:], in_=HLp[:, :, 1:l + 1], axis=AX.X, op=ALU.add), reads=[K(HLp)], writes=[K(LBp)])
        fw.op("dve", lambda: nc.vector.tensor_tensor(out=LBp[:], in0=LBp[:], in1=OMp[:], op=ALU.mult), reads=[K(LBp), K(OMp)], writes=[K(LBp)])
        fw.op("dve", lambda: nc.vector.tensor_scalar(out=OMp[:], in0=LBp[:], scalar1=-1.0, scalar2=1.0, op0=ALU.mult, op1=ALU.add),
              reads=[K(LBp)], writes=[K(OMp)])
        WO, lng, lnb, tb_bufs = load_tail_weights(g, es, l, g.w["hg_w_out"][sl], [st1, st1])

        for s in range(nseq):
            row0 = s * T
            with ExitStack() as esA:
                WI = sbt(g, esA, "hWI", [128, 8, 4096], BF16)
                XT = sbt(g, esA, "hXT", [128, 8, T], BF16)
                FA = sbt(g, esA, "hFA", [128, T], F32)
                FB = sbt(g, esA, "hFB", [128, T], F32)
                FC = sbt(g, esA, "hFC", [128, T], F32)
                QB = sbt(g, esA, "hQB", [128, T], BF16)
                EV = [sbt(g, esA, f"hEV{i}", [128, 512], BF16) for i in range(3)]
                TM = [ColView(FA, D), ColView(FB, D), ColView(FC, D)]
                TMb = ColView(QB, D)
                load_w_bf16(g, None, w_in, 4096, [st1, st1],
                            lambda kc, st: (fw.op("act", lambda: nc.scalar.copy(out=WI[:, kc, 0:2048], in_=st[:, 0:2048]),
                                                  reads=[K(st)], writes=[K(WI, (kc, 0))]),
                                            fw.op("dve", lambda: nc.vector.tensor_copy(out=WI[:, kc, 2048:4096], in_=st[:, 2048:4096]),
                                                  reads=[K(st)], writes=[K(WI, (kc, 1))])))
                build_xT(g, esA, X, Xname, row0, XT, tb_bufs["xt"])
                np_ = [0]

                def proj_fm(c0, dst):
                    for tb in range(4):
                        pS = ps[2 + np_[0] % 2]
                        np_[0] += 1
                        for kc in range(8):
                            fw.op("pe", lambda: nc.tensor.matmul(out=pS[:, :], lhsT=WI[:, kc, c0:c0 + 128],
                                                                 rhs=XT[:, kc, tb * 512:(tb + 1) * 512], start=(kc == 0), stop=(kc == 7)),
                                  reads=[K(WI), K(XT)], writes=[K(pS)], inc=(kc == 7))
                        fw.op("act", lambda: nc.scalar.copy(out=dst[:, tb * 512:(tb + 1) * 512], in_=pS[:, :]),
                              reads=[K(pS)], writes=[K(dst, tb)])

                for h in range(8):
                    proj_fm(1024 + h * 128, FA)
                    fw.op("act", lambda: nc.scalar.activation(out=FA[:], in_=FA[:], func=AF.Sigmoid), reads=[K(FA)], writes=[K(FA)])
                    fw.op("dve", lambda: nc.vector.tensor_scalar(out=FA[:], in0=FA[:], scalar1=OMp[:, h:h + 1], scalar2=LBp[:, h:h + 1],
                                                                 op0=ALU.mult, op1=ALU.add), reads=[K(FA), K(OMp), K(LBp)], writes=[K(FA)])
                    fw.op("act", lambda: nc.scalar.activation(out=FB[:], in_=FA[:], func=AF.Ln), reads=[K(FA)], writes=[K(FB)])
                    src, dst = FB, FC
                    sft = 1
                    while sft < HL:
                        s3, d3 = (x_[:].rearrange("p (c j) -> p c j", j=HL) for x_ in (src, dst))
                        fw.op("dve", lambda: nc.vector.tensor_tensor(out=d3[:, :, sft:], in0=s3[:, :, sft:], in1=s3[:, :, :HL - sft], op=ALU.add),
                              reads=[K(src)], writes=[K(dst, "hi")])
                        fw.op("act", lambda: nc.scalar.copy(out=d3[:, :, :sft], in_=s3[:, :, :sft]), reads=[K(src)], writes=[K(dst, "lo")])
                        src, dst = dst, src
                        sft *= 2
                    Gt, tmp = src, dst
                    fw.op("act", lambda: nc.scalar.activation(out=EGE[:, h, :], in_=Gt[:].rearrange("p (c j) -> p c j", j=HL)[:, :, HL - 1],
                                                              func=AF.Exp), reads=[K(Gt)], writes=[K(EGE, h)])
                    fw.op("act", lambda: nc.scalar.activation(out=tmp[:], in_=Gt[:], func=AF.Exp, scale=-1.0), reads=[K(Gt)], writes=[K(tmp)])
                    fw.op("dve", lambda: nc.vector.tensor_scalar(out=FA[:], in0=FA[:], scalar1=-1.0, scalar2=1.0, op0=ALU.mult, op1=ALU.add),
                          reads=[K(FA)], writes=[K(FA)])
                    fw.op("dve", lambda: nc.vector.tensor_tensor(out=QB[:], in0=FA[:], in1=tmp[:], op=ALU.mult), reads=[K(FA), K(tmp)], writes=[K(QB)])
                    fw.dma("sp", lambda e: e.dma_start(out=kdS[h * 128:(h + 1) * 128, :], in_=QB[:]), reads=[K(QB)], writes=[K("kdS", h)])
                    fw.op("act", lambda: nc.scalar.activation(out=tmp[:], in_=Gt[:], func=AF.Exp), reads=[K(Gt)], writes=[K(tmp)])
                    proj_fm(h * 128, FA)
                    fw.op("act", lambda: nc.scalar.activation(out=FA[:], in_=FA[:], func=AF.Silu), reads=[K(FA)], writes=[K(FA)])
                    fw.op("dve", lambda: nc.vector.tensor_tensor(out=QB[:], in0=FA[:], in1=tmp[:], op=ALU.mult), reads=[K(FA), K(tmp)], writes=[K(QB)])
                    fw.dma("sp", lambda e: e.dma_start(out=qdS[h * 128:(h + 1) * 128, :], in_=QB[:]), reads=[K(QB)], writes=[K("qdS", h)])
                for t in range(TT):
                    for (c0, dstS, nm, sig) in ((2048, vS, "vS2", False), (3072, sgS, "sgS", True)):
                        for nh in range(2):
                            pS = ps[2 + np_[0] % 2]
                            np_[0] += 1
                            for kc in range(8):
                                fw.op("pe", lambda: nc.tensor.matmul(out=pS[:, :], lhsT=XT[:, kc, t * 128:(t + 1) * 128],
                                                                     rhs=WI[:, kc, c0 + nh * 512:c0 + (nh + 1) * 512],
                                                                     start=(kc == 0), stop=(kc == 7)),
                                      reads=[K(XT), K(WI)], writes=[K(pS)], inc=(kc == 7))
                            E = EV[np_[0] % 3]
                            if sig:
                                fw.op("act", lambda: nc.scalar.activation(out=E[:], in_=pS[:, :], func=AF.Sigmoid), reads=[K(pS)], writes=[K(E)])
                            else:
                                fw.op("dve", lambda: nc.vector.tensor_copy(out=E[:], in_=pS[:, :]), reads=[K(pS)], writes=[K(E)])
                            fw.dma("sp", lambda e: e.dma_start(out=dstS[t * 128:(t + 1) * 128, nh * 512:(nh + 1) * 512], in_=E[:]),
                                   reads=[K(E)], writes=[K(nm, (t, nh))])
                    fT, lfT, sfx = TM
                    for nh in range(2):
                        pS = ps[2 + np_[0] % 2]
                        np_[0] += 1
                        for kc in range(8):
                            fw.op("pe", lambda: nc.tensor.matmul(out=pS[:, :], lhsT=XT[:, kc, t * 128:(t + 1) * 128],
                                                                 rhs=WI[:, kc, 1024 + nh * 512:1024 + (nh + 1) * 512],
                                                                 start=(kc == 0), stop=(kc == 7)),
                                  reads=[K(XT), K(WI)], writes=[K(pS)], inc=(kc == 7))
                        fw.op("act", lambda: nc.scalar.activation(out=fT[:, nh * 512:(nh + 1) * 512], in_=pS[:, :], func=AF.Sigmoid),
                              reads=[K(pS)], writes=[K(fT, nh)])
                    fw.op("dve", lambda: nc.vector.tensor_tensor(out=fT[:], in0=fT[:], in1=OMb[:], op=ALU.mult), reads=[K(fT), K(OMb)], writes=[K(fT)])
                    fw.op("pool", lambda: nc.gpsimd.tensor_tensor(out=fT[:], in0=fT[:], in1=LBb[:], op=ALU.add), reads=[K(fT), K(LBb)], writes=[K(fT)])
                    fw.op("act", lambda: nc.scalar.activation(out=lfT[:], in_=fT[:], func=AF.Ln), reads=[K(fT)], writes=[K(lfT)])
                    for nh in range(2):
                        pS = ps[4 + nh]
                        fw.op("pe", lambda: nc.tensor.matmul(out=pS[:, :], lhsT=SUFU[:], rhs=lfT[:, nh * 512:(nh + 1) * 512], start=True, stop=True),
                              reads=[K(SUFU), K(lfT)], writes=[K(pS)])
                        fw.op("act", lambda: nc.scalar.activation(out=sfx[:, nh * 512:(nh + 1) * 512], in_=pS[:, :], func=AF.Exp),
                              reads=[K(pS)], writes=[K(sfx, nh)])
                    fw.op("dve", lambda: nc.vector.tensor_scalar(out=fT[:], in0=fT[:], scalar1=-1.0, scalar2=1.0, op0=ALU.mult, op1=ALU.add),
                          reads=[K(fT)], writes=[K(fT)])
                    fw.op("dve", lambda: nc.vector.tensor_tensor(out=TMb[:], in0=fT[:], in1=sfx[:], op=ALU.mult), reads=[K(fT), K(sfx)], writes=[K(TMb)])
                    fw.dma("sp", lambda e: e.dma_start(out=kkS[t * 128:(t + 1) * 128, :], in_=TMb[:]), reads=[K(TMb)], writes=[K("kkS", t)])
            fw.barrier()
            with ExitStack() as esB:
                S = sbt(g, esB, "hS", [128, 8, 128], F32)
                Sb = sbt(g, esB, "hSb", [128, 8, 128], BF16)
                QD = sbt(g, esB, "hQD", [128, 8, 512], BF16)
                KD = sbt(g, esB, "hKD", [128, 8, 512], BF16)
                V64 = sbt(g, esB, "hV64", [64, 8, D], BF16)
                KK64 = sbt(g, esB, "hKK64", [64, 8, D], BF16)
                AT = sbt(g, esB, "hAT", [64, 8, 64], BF16)
                HN = sbt(g, esB, "hHN", [64, D], F32)
                SQ = sbt(g, esB, "hSQ", [64, D], F32)
                MS = sbt(g, esB, "hMS", [64, 8], F32)
                SGt = [sbt(g, esB, f"hSGt{i}", [64, D], BF16) for i in range(2)]
                OTK = [sbt(g, esB, f"hOTK{i}", [64, D], BF16) for i in range(2)]
                fw.op("dve", lambda: nc.vector.memset(S[:], 0.0), writes=[K(S)])
                fw.op("dve", lambda: nc.vector.memset(Sb[:], 0.0), writes=[K(Sb)])
                for tb in range(4):
                    fw.dma("sp", lambda e: e.dma_start(out=QD[:], in_=qdS[:, tb * 512:(tb + 1) * 512].rearrange("(h k) t -> k h t", k=128)),
                           reads=[K("qdS")], writes=[K(QD)])
                    fw.dma("sp", lambda e: e.dma_start(out=KD[:], in_=kdS[:, tb * 512:(tb + 1) * 512].rearrange("(h k) t -> k h t", k=128)),
                           reads=[K("kdS")], writes=[K(KD)])
                    fw.dma("sp", lambda e: e.dma_start(out=V64[:], in_=vS[tb * 512:(tb + 1) * 512, :].rearrange("(c s) d -> s c d", s=HL)),
                           reads=[K("vS2")], writes=[K(V64)])
                    fw.dma("sp", lambda e: e.dma_start(out=KK64[:], in_=kkS[tb * 512:(tb + 1) * 512, :].rearrange("(c s) d -> s c d", s=HL)),
                           reads=[K("kkS")], writes=[K(KK64)])
                    for ci in range(8):
                        cg = tb * 8 + ci
                        cs = slice(ci * HL, (ci + 1) * HL)
                        fw.dma("sp", lambda e: e.dma_start(out=SGt[cg % 2][:], in_=sgS[cg * HL:(cg + 1) * HL, :]), reads=[K("sgS")], writes=[K(SGt[cg % 2])])
                        tail_load_x(g, X, Xname, row0, cg, tb_bufs, rows=HL)
                        for h in range(8):
                            fw.op("pe", lambda: nc.tensor.matmul(out=ps[2][0:64, h * 64:(h + 1) * 64], lhsT=KD[:, h, cs], rhs=QD[:, h, cs],
                                                                 start=True, stop=True), reads=[K(KD), K(QD)], writes=[K(ps[2])], inc=(h == 7))
                        fw.op("dve", lambda: nc.vector.tensor_tensor(out=AT[:], in0=ps[2][0:64, :].rearrange("p (h t) -> p h t", h=8),
                                                                     in1=UM[0:64, 0:64].unsqueeze(1).to_broadcast([64, 8, 64]), op=ALU.mult),
                              reads=[K(ps[2]), K(UM)], writes=[K(AT)])
                        for h in range(8):
                            po = ps[3 + h // 4]
                            c0 = (h % 4) * 128
                            fw.op("pe", lambda: nc.tensor.matmul(out=po[0:64, c0:c0 + 128], lhsT=AT[:, h, :], rhs=V64[:, ci, h * 128:(h + 1) * 128],
                                                                 start=True, stop=False), reads=[K(AT), K(V64)], writes=[K(po)], inc=False)
                            fw.op("pe", lambda: nc.tensor.matmul(out=po[0:64, c0:c0 + 128], lhsT=QD[:, h, cs], rhs=Sb[:, h, :],
                                                                 start=False, stop=True), reads=[K(QD), K(Sb)], writes=[K(po)], inc=(h % 4 == 3))
                        for hh in range(2):
                            fw.op("act", lambda: nc.scalar.copy(out=HN[:, hh * 512:(hh + 1) * 512], in_=ps[3 + hh][0:64, :]),
                                  reads=[K(ps[3 + hh])], writes=[K(HN, hh)])
                        for hh in range(2):
                            for h4 in range(4):
                                h = hh * 4 + h4
                                fw.op("pe", lambda: nc.tensor.matmul(out=ps[5][:, h4 * 128:(h4 + 1) * 128], lhsT=KK64[:, ci, h * 128:(h + 1) * 128],
                                                                     rhs=V64[:, ci, h * 128:(h + 1) * 128], start=True, stop=True),
                                      reads=[K(KK64), K(V64)], writes=[K(ps[5])], inc=(h4 == 3))
                            hs = slice(hh * 4, (hh + 1) * 4)
                            fw.op("dve", lambda: nc.vector.tensor_tensor(out=S[:, hs, :], in0=S[:, hs, :],
                                                                         in1=EGE[:, hs, cg:cg + 1].to_broadcast([128, 4, 128]), op=ALU.mult),
                                  reads=[K(S, hh), K(EGE)], writes=[K(S, hh)])
                            fw.op("dve", lambda: nc.vector.tensor_tensor(out=S[:, hs, :], in0=S[:, hs, :],
                                                                         in1=ps[5][:, :].rearrange("p (h v) -> p h v", h=4), op=ALU.add),
                                  reads=[K(S, hh), K(ps[5])], writes=[K(S, hh)])
                            fw.op("act", lambda: nc.scalar.copy(out=Sb[:, hs, :], in_=S[:, hs, :]), reads=[K(S, hh)], writes=[K(Sb, hh)])
                        sgt, ok = SGt[cg % 2], OTK[cg % 2]
                        fw.op("pool", lambda: nc.gpsimd.tensor_tensor(out=SQ[:], in0=HN[:], in1=HN[:], op=ALU.mult), reads=[K(HN)], writes=[K(SQ)])
                        fw.op("dve", lambda: nc.vector.tensor_reduce(out=MS[:], in_=SQ[:].rearrange("p (h d) -> p h d", h=8), axis=AX.X, op=ALU.add),
                              reads=[K(SQ)], writes=[K(MS)])
                        fw.op("dve", lambda: nc.vector.tensor_scalar(out=MS[:], in0=MS[:], scalar1=1.0 / 128, scalar2=1e-6, op0=ALU.mult, op1=ALU.add),
                              reads=[K(MS)], writes=[K(MS)])
                        fw.op("act", lambda: nc.scalar.sqrt(out=MS[:], in_=MS[:]), reads=[K(MS)], writes=[K(MS)])
                        fw.op("dve", lambda: nc.vector.reciprocal(out=MS[:], in_=MS[:]), reads=[K(MS)], writes=[K(MS)])
                        fw.op("dve", lambda: nc.vector.tensor_tensor(out=SQ[:].rearrange("p (h d) -> p h d", h=8),
                                                                     in0=HN[:].rearrange("p (h d) -> p h d", h=8),
                                                                     in1=MS[:].unsqueeze(2).to_broadcast([64, 8, 128]), op=ALU.mult),
                              reads=[K(HN), K(MS)], writes=[K(SQ)])
                        fw.op("pool", lambda: nc.gpsimd.tensor_tensor(out=SQ[:], in0=SQ[:], in1=NG[0:64, :], op=ALU.mult),
                              reads=[K(SQ), K(NG)], writes=[K(SQ)])
                        fw.op("dve", lambda: nc.vector.tensor_tensor(out=ok[:], in0=SQ[:], in1=sgt[:], op=ALU.mult),
                              reads=[K(SQ), K(sgt)], writes=[K(ok)])
                        mixer_tail(g, esB, l, X, Xname, XO, XOname, row0, cg, ok[:], WO, lng, lnb, tb_bufs, K(ok), rows=HL, xloaded=True)
            fw.barrier()


MIXERS[2] = phase_hgrn


N_CORES = 8
MOE_B = 512
W_NAMES = [n for n in WEIGHT_SHAPES]


def full_plan():
    plan = []
    cur = "x"
    for l in range(4):
        plan.append(("mixer", l, cur, "xa"))
        dst = "out" if l == 3 else "xb"
        plan.append(("moe", l, "xa", dst))
        cur = dst
    return plan


def run_forward(inputs, n_cores=N_CORES, seq_per_core=4):
    x = np.ascontiguousarray(np.asarray(inputs["x"], dtype=np.float32))
    NT = seq_per_core * T
    nc, g = build(NT, MOE_B, full_plan(), W_NAMES)
    cst = make_consts(MOE_B)
    nsac = make_nsa_consts()
    weights = {n: np.ascontiguousarray(np.asarray(inputs[n], dtype=np.float32)) for n in W_NAMES}
    in_maps = []
    for c in range(n_cores):
        m = dict(weights)
        m["x"] = x[c * seq_per_core:(c + 1) * seq_per_core].reshape(NT, D)
        m["cst"] = cst
        m["nsac"] = nsac
        in_maps.append(m)
    res = run_bass_kernel_spmd(nc, in_maps, core_ids=list(range(n_cores)))
    outs = [np.asarray(r["out"]).reshape(seq_per_core, T, D) for r in res.results]
    return np.concatenate(outs, axis=0).astype(np.float32)


def kernel(**inputs):
    return run_forward(inputs)
```

```python
from contextlib import ExitStack
import numpy as np
import concourse.bass as bass
import concourse.mybir as mybir
from concourse.bass_utils import run_bass_kernel_spmd

F32 = mybir.dt.float32
BF16 = mybir.dt.bfloat16
I32 = mybir.dt.int32
AF = mybir.ActivationFunctionType
ALU = mybir.AluOpType
AX = mybir.AxisListType

COMPUTE = ("pe", "dve", "act", "pool")
EPOCH = 24000
DMA_SLOTS = 12


class FW:
    def __init__(self, nc, es):
        self.nc = nc
        self.es = es
        self.e = {"pe": nc.tensor, "dve": nc.vector, "act": nc.scalar, "pool": nc.gpsimd, "sp": nc.sync}
        self.csem = {k: [] for k in COMPUTE}
        self.ccnt = {k: 0 for k in COMPUTE}
        self.dsem = {}
        self.dn = {}
        self.seen = {k: {} for k in self.e}
        self.state = {}
        self.nsem = 0
        self.ninst = 0
        self.persistent = set()
        for k in COMPUTE:
            self._new_epoch(k)

    def _sem(self, name):
        self.nsem += 1
        return self.es.enter_context(self.nc.semaphore(name))

    def _new_epoch(self, k):
        self.csem[k].append(self._sem(f"c_{k}_{len(self.csem[k])}"))
        self.ccnt[k] = 0

    def _deps(self, reads, writes):
        deps = []
        for key in reads:
            name, tag = key
            st = self.state.get(name)
            if not st:
                continue
            tags = list(st.keys()) if tag is None else [tag, None]
            for t in tags:
                s = st.get(t)
                if s and s[0] is not None:
                    deps.append(s[0])
        for key in writes:
            name, tag = key
            st = self.state.get(name)
            if not st:
                continue
            tags = list(st.keys()) if tag is None else [tag, None]
            for t in tags:
                s = st.get(t)
                if s:
                    if s[0] is not None:
                        deps.append(s[0])
                    deps.extend(s[1].values())
        return deps

    def _record(self, reads, writes, tok):
        for name, tag in reads:
            st = self.state.setdefault(name, {})
            s = st.setdefault(tag, [None, {}])
            s[1][tok[0]] = tok
        for name, tag in writes:
            st = self.state.setdefault(name, {})
            if tag is None:
                st.clear()
            st[tag] = [tok, {}]

    def _wait(self, stream, deps, skip_self=None):
        seen = self.seen[stream]
        need = {}
        for (semkey, val, sem) in deps:
            if skip_self is not None and semkey[0] == "c" and semkey[1] == skip_self:
                continue
            if seen.get(semkey, 0) >= val:
                continue
            if semkey[0] == "c":
                if any(k2[0] == "c" and k2[1] == semkey[1] and k2[2] > semkey[2] for k2 in seen):
                    continue
            if need.get(semkey, (0, None))[0] < val:
                need[semkey] = (val, sem)
        for semkey, (val, sem) in need.items():
            self.e[stream].wait_ge(sem, val)
            seen[semkey] = val
            self.ninst += 1

    def op(self, eng, emit, reads=(), writes=(), inc=True):
        deps = self._deps(reads, writes)
        self._wait(eng, deps, skip_self="pe" if eng == "pe" else None)
        ins = emit()
        self.ninst += 1
        ep = len(self.csem[eng]) - 1
        if inc:
            self.ccnt[eng] += 1
            ins.then_inc(self.csem[eng][ep], 1)
            tok = (("c", eng, ep), self.ccnt[eng], self.csem[eng][ep])
            if self.ccnt[eng] >= EPOCH:
                self._new_epoch(eng)
        else:
            tok = (("c", eng, ep), self.ccnt[eng] + 1, self.csem[eng][ep])
        self._record(reads, writes, tok)
        return ins

    def dma(self, q, emit, reads=(), writes=()):
        if q not in self.dsem:
            self.dsem[q] = [self._sem(f"d_{q}_{i}") for i in range(DMA_SLOTS)]
            self.dn[q] = 0
        i = self.dn[q]
        slot = i % DMA_SLOTS
        sem = self.dsem[q][slot]
        deps = self._deps(reads, writes)
        if i >= DMA_SLOTS:
            deps.append((("d", q, slot), 16 * (i // DMA_SLOTS), sem))
        self._wait(q, deps)
        ins = emit(self.e[q])
        ins.then_inc(sem, 16)
        self.ninst += 1
        self.dn[q] = i + 1
        tok = (("d", q, slot), 16 * (i // DMA_SLOTS + 1), sem)
        self._record(reads, writes, tok)
        return ins

    def barrier(self):
        deps = []
        for k in COMPUTE:
            ep = len(self.csem[k]) - 1
            if self.ccnt[k] > 0:
                deps.append((("c", k, ep), self.ccnt[k], self.csem[k][ep]))
            elif ep > 0:
                deps.append((("c", k, ep - 1), EPOCH, self.csem[k][ep - 1]))
        for q, n in self.dn.items():
            for slot in range(min(n, DMA_SLOTS)):
                last = ((n - 1 - slot) // DMA_SLOTS) * DMA_SLOTS + slot
                deps.append((("d", q, slot), 16 * (last // DMA_SLOTS + 1), self.dsem[q][slot]))
        for stream in self.e:
            self._wait(stream, deps)
        self.state = {k: v for k, v in self.state.items() if k in self.persistent}

    def finish(self, out_names):
        deps = []
        for name in out_names:
            st = self.state.get(name, {})
            for s in st.values():
                if s[0] is not None:
                    deps.append(s[0])
        self._wait("sp", deps)


def K(t, tag=None):
    return (t if isinstance(t, str) else t.name, tag)


D = 1024
NE = 32
ALPHA = 8 ** 0.25
LN_EPS = 1e-5


def moe_nb(NT, B):
    return (4 * NT + NE * (B - 1) + B - 1) // B


class G:
    pass


_UID = [0]


def sbt(g, es, name, shape, dt):
    _UID[0] += 1
    return es.enter_context(g.nc.sbuf_tensor(f"{name}_{_UID[0]}", shape, dt))


def layer_norm_tile(g, xt, lng, lnb, out, tmpname, es, rows=128):
    nc, fw = g.nc, g.fw
    st = g.ln_st
    mv = g.ln_mv
    R = slice(0, rows)
    for c in range(2):
        fw.op("dve", lambda: nc.vector.bn_stats(out=st[R, c, :], in_=xt[R, c * 512:(c + 1) * 512]),
              reads=[K(xt)], writes=[K(st, c)])
    fw.op("dve", lambda: nc.vector.bn_aggr(out=mv[R, 0:2], in_=st[R]), reads=[K(st)], writes=[K(mv, 0)])
    fw.op("dve", lambda: nc.vector.tensor_scalar_add(out=mv[R, 3:4], in0=mv[R, 1:2], scalar1=LN_EPS),
          reads=[K(mv, 0)], writes=[K(mv, 2)])
    fw.op("act", lambda: nc.scalar.sqrt(out=mv[R, 3:4], in_=mv[R, 3:4]), reads=[K(mv, 2)], writes=[K(mv, 2)])
    fw.op("dve", lambda: nc.vector.reciprocal(out=mv[R, 2:3], in_=mv[R, 3:4]), reads=[K(mv, 2)], writes=[K(mv, 1)])
    fw.op("dve", lambda: nc.vector.tensor_scalar(out=xt[R], in0=xt[R], scalar1=mv[R, 0:1], scalar2=mv[R, 2:3],
                                                 op0=ALU.subtract, op1=ALU.mult),
          reads=[K(xt), K(mv, 0), K(mv, 1)], writes=[K(xt)])
    fw.op("pool", lambda: nc.gpsimd.tensor_tensor(out=xt[R], in0=xt[R], in1=lng[R], op=ALU.mult),
          reads=[K(xt), K(lng)], writes=[K(xt)])
    fw.op("pool", lambda: nc.gpsimd.tensor_tensor(out=out[R], in0=xt[R], in1=lnb[R], op=ALU.add),
          reads=[K(xt), K(lnb)], writes=[K(out)])


def phase_route(g, l, X1, X1name, keep):
    nc, fw, NT, B = g.nc, g.fw, g.NT, g.B
    NTT = NT // 128
    NB = moe_nb(NT, B)
    ps = g.ps
    POS4i, G4, EB = keep["POS4i"], keep["G4"], keep["EB"]
    with ExitStack() as es:
        L = sbt(g, es, "rL", [128, NTT, 32], F32)
        RW = sbt(g, es, "rW", [128, 8, 32], F32)
        RB = sbt(g, es, "rB", [128, 32], F32)
        fw.dma("sp", lambda e: e.dma_start(out=RW[:], in_=g.w["router_w"][l].rearrange("(kc p) e -> p kc e", p=128)),
               writes=[K(RW)])
        fw.dma("sp", lambda e: e.dma_start(out=RB[:], in_=g.w["router_b"][l].partition_broadcast(128)),
               writes=[K(RB)])
        xt = [sbt(g, es, f"rx{i}", [128, D], F32) for i in range(2)]
        xb = [sbt(g, es, f"rxb{i}", [128, D], BF16) for i in range(2)]
        xT = [sbt(g, es, f"rxT{i}", [128, 8, 128], F32) for i in range(2)]
        for t in range(NTT):
            i = t % 2
            fw.dma("sp", lambda e: e.dma_start(out=xt[i][:], in_=X1[t * 128:(t + 1) * 128, :]),
                   reads=[K(X1name, t)], writes=[K(xt[i])])
            fw.op("act", lambda: nc.scalar.copy(out=xb[i][:], in_=xt[i][:]), reads=[K(xt[i])], writes=[K(xb[i])])
            fw.dma("sp", lambda e: e.dma_start(out=g.x1b[t * 128:(t + 1) * 128, :], in_=xb[i][:]),
                   reads=[K(xb[i])], writes=[K("x1b", t)])
            for h in range(2):
                pst = ps[6 + h]
                for c in range(4):
                    kc = h * 4 + c
                    fw.op("pe", lambda: nc.tensor.transpose(out=pst[:, c * 128:(c + 1) * 128],
                                                            in_=xt[i][:, kc * 128:(kc + 1) * 128],
                                                            identity=g.ident[:]),
                          reads=[K(xt[i]), K(g.ident)], writes=[K(pst)], inc=(c == 3))
                fw.op("dve", lambda: nc.vector.tensor_copy(out=xT[i][:, h * 4:(h + 1) * 4, :],
                                                           in_=pst[:].rearrange("p (c n) -> p c n", c=4)),
                      reads=[K(pst)], writes=[K(xT[i], h)])
            for kc in range(8):
                fw.op("pe", lambda: nc.tensor.matmul(out=ps[2][:, 0:32], lhsT=xT[i][:, kc, :], rhs=RW[:, kc, :],
                                                     start=(kc == 0), stop=(kc == 7)),
                      reads=[K(xT[i]), K(RW)], writes=[K(ps[2])], inc=(kc == 7))
            fw.op("dve", lambda: nc.vector.tensor_tensor(out=L[:, t, :], in0=ps[2][:, 0:32], in1=RB[:], op=ALU.add),
                  reads=[K(ps[2]), K(RB)], writes=[K(L, t)])
        TOP = sbt(g, es, "rTOP", [128, NTT, 8], F32)
        SEL = sbt(g, es, "rSEL", [128, NTT, 32], F32)
        GT = sbt(g, es, "rGT", [128, NTT, 32], F32)
        CA = sbt(g, es, "rCA", [128, NTT, 32], F32)
        CB = sbt(g, es, "rCB", [128, NTT, 32], F32)
        Z = sbt(g, es, "rZ", [128, NTT], F32)
        SM = sbt(g, es, "rSM", [128, 8, 32], F32)
        SMb = sbt(g, es, "rSMb", [128, 32], BF16)
        for t in range(NTT):
            fw.op("dve", lambda: nc.vector.max(out=TOP[:, t, :], in_=L[:, t, :]), reads=[K(L, t)], writes=[K(TOP, t)])
        bshape = [128, NTT, 32]
        fw.op("dve", lambda: nc.vector.tensor_tensor(out=SEL[:], in0=L[:], in1=TOP[:, :, 3:4].to_broadcast(bshape),
                                                     op=ALU.is_ge), reads=[K(L), K(TOP)], writes=[K(SEL)])
        fw.op("dve", lambda: nc.vector.tensor_tensor(out=GT[:], in0=L[:], in1=TOP[:, :, 0:1].to_broadcast(bshape),
                                                     op=ALU.subtract), reads=[K(L), K(TOP)], writes=[K(GT)])
        fw.op("act", lambda: nc.scalar.activation(out=GT[:], in_=GT[:], func=AF.Exp), reads=[K(GT)], writes=[K(GT)])
        fw.op("dve", lambda: nc.vector.tensor_tensor(out=GT[:], in0=GT[:], in1=SEL[:], op=ALU.mult),
              reads=[K(GT), K(SEL)], writes=[K(GT)])
        fw.op("dve", lambda: nc.vector.tensor_reduce(out=Z[:], in_=GT[:], axis=AX.X, op=ALU.add),
              reads=[K(GT)], writes=[K(Z)])
        fw.op("dve", lambda: nc.vector.reciprocal(out=Z[:], in_=Z[:]), reads=[K(Z)], writes=[K(Z)])
        fw.op("dve", lambda: nc.vector.tensor_tensor(out=GT[:], in0=GT[:],
                                                     in1=Z[:].unsqueeze(2).to_broadcast(bshape), op=ALU.mult),
              reads=[K(GT), K(Z)], writes=[K(GT)])
        src, dst = SEL, CA
        s = 1
        while s < NTT:
            fw.op("dve", lambda: nc.vector.tensor_tensor(out=dst[:, s:, :], in0=src[:, s:, :], in1=src[:, :NTT - s, :],
                                                         op=ALU.add), reads=[K(src)], writes=[K(dst, "hi")])
            fw.op("dve", lambda: nc.vector.tensor_copy(out=dst[:, :s, :], in_=src[:, :s, :]),
                  reads=[K(src)], writes=[K(dst, "lo")])
            src = dst
            dst = CB if dst is CA else CA
            s *= 2
        INC = src
        EXC = dst
        fw.op("dve", lambda: nc.vector.tensor_copy(out=SMb[:], in_=INC[:, NTT - 1, :]), reads=[K(INC)], writes=[K(SMb)])
        fw.op("pe", lambda: nc.tensor.matmul(out=ps[3][:, 0:32], lhsT=g.trib[:], rhs=SMb[:], start=True, stop=True),
              reads=[K(g.trib), K(SMb)], writes=[K(ps[3], 0)])
        fw.op("pe", lambda: nc.tensor.matmul(out=ps[3][:, 32:64], lhsT=g.onesb[:], rhs=SMb[:], start=True, stop=True),
              reads=[K(g.onesb), K(SMb)], writes=[K(ps[3], 1)])
        PP, CNT, PAD, BASE, BEND, TMP = (SM[:, i, :] for i in range(6))
        fw.op("dve", lambda: nc.vector.tensor_copy(out=SM[:, 0:2, :], in_=ps[3][:, 0:64].rearrange("p (a e) -> p a e", a=2)),
              reads=[K(ps[3])], writes=[K(SM, 0), K(SM, 1)])
        fw.op("dve", lambda: nc.vector.tensor_scalar(out=TMP, in0=CNT, scalar1=1.0 / B, scalar2=(B - 1) / (2.0 * B),
                                                     op0=ALU.mult, op1=ALU.add), reads=[K(SM, 1)], writes=[K(SM, 5)])
        fw.op("dve", lambda: nc.vector.tensor_scalar_add(out=TMP, in0=TMP, scalar1=8388608.0),
              reads=[K(SM, 5)], writes=[K(SM, 5)])
        fw.op("dve", lambda: nc.vector.tensor_scalar(out=PAD, in0=TMP, scalar1=-8388608.0, scalar2=float(B),
                                                     op0=ALU.add, op1=ALU.mult), reads=[K(SM, 5)], writes=[K(SM, 2)])
        cur, oth = 2, 4
        s = 1
        while s < 32:
            fw.op("dve", lambda: nc.vector.tensor_tensor(out=SM[:, oth, s:], in0=SM[:, cur, s:], in1=SM[:, cur, :32 - s],
                                                         op=ALU.add), reads=[K(SM, cur)], writes=[K(SM, oth)])
            fw.op("dve", lambda: nc.vector.tensor_copy(out=SM[:, oth, :s], in_=SM[:, cur, :s]),
                  reads=[K(SM, cur)], writes=[K(SM, oth)])
            cur, oth = oth, (5 if oth == 4 else 4)
            s *= 2
        assert cur == 4
        fw.op("dve", lambda: nc.vector.tensor_tensor(out=BASE, in0=SM[:, 4, :], in1=PAD, op=ALU.subtract),
              reads=[K(SM, 4), K(SM, 2)], writes=[K(SM, 3)])
        CMP = sbt(g, es, "rCMP", [128, NB, 32], F32)
        EBf = sbt(g, es, "rEBf", [128, NB], F32)
        fw.op("dve", lambda: nc.vector.tensor_tensor(out=CMP[:], in0=SM[:, 4:5, :].to_broadcast([128, NB, 32]),
                                                     in1=g.iotaB[:, 0:NB].unsqueeze(2).to_broadcast([128, NB, 32]),
                                                     op=ALU.is_le), reads=[K(SM, 4), K(g.iotaB)], writes=[K(CMP)])
        fw.op("dve", lambda: nc.vector.tensor_reduce(out=EBf[:], in_=CMP[:], axis=AX.X, op=ALU.add),
              reads=[K(CMP)], writes=[K(EBf)])
        fw.op("dve", lambda: nc.vector.tensor_scalar_min(out=EBf[:], in0=EBf[:], scalar1=31.0),
              reads=[K(EBf)], writes=[K(EBf)])
        fw.op("dve", lambda: nc.vector.tensor_copy(out=EB[:], in_=EBf[:]), reads=[K(EBf)], writes=[K(EB)])
        IDXG, IDXB = keep["IDXG"], keep["IDXB"]
        IGf = sbt(g, es, "rIGf", [128, NB, 8], F32)
        fw.op("dve", lambda: nc.vector.tensor_scalar(out=EBf[:], in0=EBf[:], scalar1=float(l * NE), scalar2=None,
                                                     op0=ALU.add), reads=[K(EBf)], writes=[K(EBf)])
        fw.op("dve", lambda: nc.vector.tensor_copy(out=IDXB[:], in_=EBf[:]), reads=[K(EBf)], writes=[K(IDXB)])
        fw.op("dve", lambda: nc.vector.scalar_tensor_tensor(out=IGf[:], in0=EBf[:].unsqueeze(2).to_broadcast([128, NB, 8]),
                                                            scalar=1024.0, in1=g.kcp[:].unsqueeze(1).to_broadcast([128, NB, 8]),
                                                            op0=ALU.mult, op1=ALU.add),
              reads=[K(EBf), K(g.kcp)], writes=[K(IGf)])
        SKP = sbt(g, es, "rSKP", [128, NB], F32)
        fw.op("dve", lambda: nc.vector.memset(SKP[:, 0:2], 0.0), writes=[K(SKP, "a")])
        fw.op("dve", lambda: nc.vector.tensor_tensor(out=SKP[:, 2:NB], in0=EBf[:, 2:NB], in1=EBf[:, 0:NB - 2], op=ALU.is_equal),
              reads=[K(EBf)], writes=[K(SKP, "b")])
        fw.op("dve", lambda: nc.vector.scalar_tensor_tensor(out=IGf[:], in0=SKP[:].unsqueeze(2).to_broadcast([128, NB, 8]),
                                                            scalar=4194304.0, in1=IGf[:], op0=ALU.mult, op1=ALU.add),
              reads=[K(SKP), K(IGf)], writes=[K(IGf)])
        fw.op("dve", lambda: nc.vector.tensor_copy(out=IDXG[:], in_=IGf[:]), reads=[K(IGf)], writes=[K(IDXG)])
        fw.op("dve", lambda: nc.vector.tensor_tensor(out=SM[:, 5, :], in0=BASE, in1=PP, op=ALU.add),
              reads=[K(SM, 3), K(SM, 0)], writes=[K(SM, 5)])
        fw.op("dve", lambda: nc.vector.tensor_tensor(out=EXC[:], in0=INC[:], in1=SEL[:], op=ALU.subtract),
              reads=[K(INC), K(SEL)], writes=[K(EXC)])
        fw.op("dve", lambda: nc.vector.tensor_tensor(out=EXC[:], in0=EXC[:], in1=SM[:, 5:6, :].to_broadcast(bshape),
                                                     op=ALU.add), reads=[K(EXC), K(SM, 5)], writes=[K(EXC)])
        fw.op("dve", lambda: nc.vector.scalar_tensor_tensor(out=EXC[:], in0=EXC[:], scalar=1.0, in1=SEL[:],
                                                            op0=ALU.add, op1=ALU.mult),
              reads=[K(EXC), K(SEL)], writes=[K(EXC)])
        fw.op("dve", lambda: nc.vector.tensor_scalar_add(out=EXC[:], in0=EXC[:], scalar1=-1.0),
              reads=[K(EXC)], writes=[K(EXC)])
        POSM = EXC
        P8 = sbt(g, es, "rP8", [128, NTT, 8], F32)
        for t in range(NTT):
            fw.op("dve", lambda: nc.vector.max(out=P8[:, t, :], in_=POSM[:, t, :]), reads=[K(POSM)], writes=[K(P8, t)])
        fw.op("dve", lambda: nc.vector.tensor_copy(out=POS4i[:], in_=P8[:, :, 0:4]), reads=[K(P8)], writes=[K(POS4i)])
        EQ = INC
        for j in range(4):
            fw.op("dve", lambda: nc.vector.tensor_tensor(out=EQ[:], in0=POSM[:], in1=P8[:, :, j:j + 1].to_broadcast(bshape),
                                                         op=ALU.is_equal), reads=[K(POSM), K(P8)], writes=[K(EQ)])
            fw.op("dve", lambda: nc.vector.tensor_tensor(out=EQ[:], in0=EQ[:], in1=GT[:], op=ALU.mult),
                  reads=[K(EQ), K(GT)], writes=[K(EQ)])
            fw.op("dve", lambda: nc.vector.tensor_reduce(out=G4[:, :, j], in_=EQ[:], axis=AX.X, op=ALU.add),
                  reads=[K(EQ)], writes=[K(G4, j)])
        for t in range(NTT):
            i = t % 2
            fw.dma("sp", lambda e: e.dma_start(out=xb[i][:], in_=g.x1b[t * 128:(t + 1) * 128, :]),
                   reads=[K("x1b", t)], writes=[K(xb[i])])
            for j in range(4):
                fw.dma("pool", lambda e: e.indirect_dma_start(
                    out=g.xs[:, :], out_offset=bass.IndirectOffsetOnAxis(ap=POS4i[:, t, j:j + 1], axis=0),
                    in_=xb[i][:], in_offset=None), reads=[K(xb[i]), K(POS4i)], writes=[K("xs", ("s", t, j))])


def phase_experts(g, l, keep):
    nc, fw, NT, B = g.nc, g.fw, g.NT, g.B
    NB = moe_nb(NT, B)
    R = B // 128
    ps = g.ps
    w_gu, w_dn, b_gu, b_dn = g.w["moe_w_gu"], g.w["moe_w_down"], g.w["moe_b_gu"], g.w["moe_b_down"]
    with ExitStack() as es:
        WG = [sbt(g, es, f"eWG{i}", [128, 8, 2048], BF16) for i in range(2)]
        WD = [sbt(g, es, f"eWD{i}", [128, 8, 1024], BF16) for i in range(2)]
        BG = [sbt(g, es, f"eBG{i}", [128, 16], F32) for i in range(2)]
        BD = [sbt(g, es, f"eBD{i}", [128, 1024], F32) for i in range(2)]
        XS = [sbt(g, es, f"eXS{i}", [128, R, D], BF16) for i in range(2)]
        XT = [sbt(g, es, f"eXT{i}", [128, 8, B], BF16) for i in range(2)]
        HT = [sbt(g, es, f"eHT{i}", [128, 8, B], BF16) for i in range(2)]
        GC = [sbt(g, es, f"eGC{i}", [128, B], F32) for i in range(2)]
        SG = [sbt(g, es, f"eSG{i}", [128, B], F32) for i in range(2)]
        UC = [sbt(g, es, f"eUC{i}", [128, B], F32) for i in range(2)]
        Y = [sbt(g, es, f"eY{i}", [128, D], F32) for i in range(2)]
        BGR = sbt(g, es, "eBGR", [2, 2048], F32)
        wgu_rows = w_gu.rearrange("l e k f -> (l e k) f")
        wdn_rows = w_dn.rearrange("l e k f -> (l e k) f")
        bgu_rows = b_gu.rearrange("l e f -> (l e) f")
        bdn_rows = b_dn.rearrange("l e f -> (l e) f")
        IDXG, IDXB = keep["IDXG"], keep["IDXB"]

        def gather(out_ap, rows, idx_ap, reads, writes):
            fw.dma("pool", lambda e: e.indirect_dma_start(
                out=out_ap, out_offset=None, in_=rows[:, :],
                in_offset=bass.IndirectOffsetOnAxis(ap=idx_ap, axis=0)), reads=reads, writes=writes)

        nrows = wgu_rows.shape[0]
        _UID[0] += 1
        bc_reg = es.enter_context(nc.gpsimd.register(f"bc_reg{_UID[0]}"))
        nc.gpsimd.reg_mov(bc_reg, nrows - 1)
        bc_val = nc.gpsimd.snap(bc_reg)

        def wgather(out_ap, rows, idx_ap, writes):
            fw.dma("pool", lambda e: e.indirect_dma_start(
                out=out_ap, out_offset=None, in_=rows[:, :],
                in_offset=bass.IndirectOffsetOnAxis(ap=idx_ap, axis=0),
                bounds_check=bc_val, oob_is_err=False), reads=[K(IDXG)], writes=writes)

        def issue(b, k):
            i = b % 2
            if k < 8:
                wgather(WG[i][:, k, :], wgu_rows, IDXG[:, b, k:k + 1], [K(WG[i], k)])
            else:
                for a in range(2):
                    fc = 2 * (k - 8) + a
                    wgather(WD[i][:, fc, :], wdn_rows, IDXG[:, b, fc:fc + 1], [K(WD[i], k - 8)])

        def prefetch_slot(b, k):
            if b >= NB:
                return
            if k == 0:
                gather(BGR[:], bgu_rows, IDXB[0:2, b:b + 1], [K(IDXB)], [K(BGR)])
                gather(BD[b % 2][:], bdn_rows, IDXB[:, b:b + 1], [K(IDXB)], [K(BD[b % 2])])
            if k < 12:
                issue(b, k)

        def bias_transposes(b):
            i = b % 2
            for m in range(16):
                fw.op("pe", lambda: nc.tensor.transpose(out=ps[7][:, m:m + 1], in_=BGR[0:1, m * 128:(m + 1) * 128],
                                                        identity=g.ident[0:1, 0:1]),
                      reads=[K(BGR), K(g.ident)], writes=[K(ps[7])], inc=(m == 15))
            fw.op("dve", lambda: nc.vector.tensor_copy(out=BG[i][:], in_=ps[7][:, 0:16]),
                  reads=[K(ps[7])], writes=[K(BG[i])])

        def load_x(b):
            fw.dma("sp", lambda e: e.dma_start(out=XS[b % 2][:], in_=g.xs[b * B:(b + 1) * B, :].rearrange("(r p) d -> p r d", p=128)),
                   reads=[K("xs")], writes=[K(XS[b % 2])])

        def x_transposes(b):
            i = b % 2
            for kc in range(8):
                pst = ps[kc % 2]
                pv = pst[:] if kc % 2 == 0 else pst[:].bitcast(BF16)
                for r in range(R):
                    fw.op("pe", lambda: nc.tensor.transpose(out=pv[:, r * 128:(r + 1) * 128],
                                                            in_=XS[i][:, r, kc * 128:(kc + 1) * 128], identity=g.identb[:]),
                          reads=[K(XS[i]), K(g.identb)], writes=[K(pst)], inc=(r == R - 1))
                fw.op("dve", lambda: nc.vector.tensor_copy(out=XT[i][:, kc, :], in_=pv[:, 0:B]),
                      reads=[K(pst)], writes=[K(XT[i], kc)])

        for k in range(13):
            prefetch_slot(0, k)
        load_x(0)
        bias_transposes(0)
        for b in range(NB):
            i = b % 2
            if b + 1 < NB:
                load_x(b + 1)
            x_transposes(b)
            for m in range(8):
                prefetch_slot(b + 1, m)
                j = m % 2
                pg, pu = ps[2 + j], ps[4 + j]
                for (pp, col) in ((pg, m * 128), (pu, 1024 + m * 128)):
                    for kc in range(8):
                        fw.op("pe", lambda: nc.tensor.matmul(out=pp[:, 0:B], lhsT=WG[i][:, kc, col:col + 128],
                                                             rhs=XT[i][:, kc, :], start=(kc == 0), stop=(kc == 7)),
                              reads=[K(WG[i], kc), K(XT[i], kc)], writes=[K(pp)], inc=(kc == 7))
                fw.op("dve", lambda: nc.vector.tensor_scalar(out=GC[j][:], in0=pg[:, 0:B], scalar1=BG[i][:, m:m + 1],
                                                             scalar2=7.0, op0=ALU.add, op1=ALU.min),
                      reads=[K(pg), K(BG[i])], writes=[K(GC[j])])
                fw.op("act", lambda: nc.scalar.activation(out=SG[j][:], in_=GC[j][:], func=AF.Sigmoid, scale=1.702),
                      reads=[K(GC[j])], writes=[K(SG[j])])
                fw.op("act", lambda: nc.scalar.activation(out=UC[j][:], in_=pu[:, 0:B], func=AF.Identity, bias=BG[i][:, 8 + m:9 + m]),
                      reads=[K(pu), K(BG[i])], writes=[K(UC[j])])
                fw.op("dve", lambda: nc.vector.tensor_scalar(out=UC[j][:], in0=UC[j][:], scalar1=7.0, scalar2=-7.0,
                                                             op0=ALU.min, op1=ALU.max),
                      reads=[K(UC[j])], writes=[K(UC[j])])
                fw.op("dve", lambda: nc.vector.tensor_tensor(out=GC[j][:], in0=GC[j][:], in1=SG[j][:], op=ALU.mult),
                      reads=[K(GC[j]), K(SG[j])], writes=[K(GC[j])])
                fw.op("dve", lambda: nc.vector.scalar_tensor_tensor(out=HT[i][:, m, :], in0=UC[j][:], scalar=1.0, in1=GC[j][:],
                                                                    op0=ALU.add, op1=ALU.mult),
                      reads=[K(GC[j]), K(UC[j])], writes=[K(HT[i], m)])
            for r in range(R):
                prefetch_slot(b + 1, 8 + r)
                yb = Y[r % 2]
                for nh in range(2):
                    py = ps[6 + nh]
                    for fc in range(8):
                        fw.op("pe", lambda: nc.tensor.matmul(out=py[:, :], lhsT=HT[i][:, fc, r * 128:(r + 1) * 128],
                                                             rhs=WD[i][:, fc, nh * 512:(nh + 1) * 512],
                                                             start=(fc == 0), stop=(fc == 7)),
                              reads=[K(HT[i], fc), K(WD[i], fc // 2)], writes=[K(py)], inc=(fc == 7))
                    fw.op("dve", lambda: nc.vector.tensor_tensor(out=yb[:, nh * 512:(nh + 1) * 512], in0=py[:, :],
                                                                 in1=BD[i][:, nh * 512:(nh + 1) * 512], op=ALU.add),
                          reads=[K(py), K(BD[i])], writes=[K(yb, nh)])
                row0 = b * B + r * 128
                fw.dma("sp", lambda e: e.dma_start(out=g.ys[row0:row0 + 128, :], in_=yb[:]),
                       reads=[K(yb)], writes=[K("ys", ("b", b, r))])
            for k in range(8 + R, 13):
                prefetch_slot(b + 1, k)
            if b + 1 < NB:
                bias_transposes(b + 1)


def phase_combine(g, l, X1, X1name, XO, XOname, keep):
    nc, fw, NT = g.nc, g.fw, g.NT
    NTT = NT // 128
    POS4i, G4 = keep["POS4i"], keep["G4"]
    with ExitStack() as es:
        lng = sbt(g, es, "cLNG", [128, D], F32)
        lnb = sbt(g, es, "cLNB", [128, D], F32)
        fw.dma("sp", lambda e: e.dma_start(out=lng[:], in_=g.w["ln_g"][l, 1, :].partition_broadcast(128)), writes=[K(lng)])
        fw.dma("sp", lambda e: e.dma_start(out=lnb[:], in_=g.w["ln_b"][l, 1, :].partition_broadcast(128)), writes=[K(lnb)])
        xt = [sbt(g, es, f"cx{i}", [128, D], F32) for i in range(2)]
        yg = [[sbt(g, es, f"cy{i}_{j}", [128, D], F32) for j in range(4)] for i in range(2)]
        ot = [sbt(g, es, f"co{i}", [128, D], F32) for i in range(2)]
        def fetch(t):
            i = t % 2
            fw.dma("sp", lambda e: e.dma_start(out=xt[i][:], in_=X1[t * 128:(t + 1) * 128, :]),
                   reads=[K(X1name, t)], writes=[K(xt[i])])
            for j in range(4):
                fw.dma("pool", lambda e: e.indirect_dma_start(
                    out=yg[i][j][:], out_offset=None, in_=g.ys[:, :],
                    in_offset=bass.IndirectOffsetOnAxis(ap=POS4i[:, t, j:j + 1], axis=0)),
                    reads=[K("ys"), K(POS4i)], writes=[K(yg[i][j])])

        fetch(0)
        for t in range(NTT):
            i = t % 2
            if t + 1 < NTT:
                fetch(t + 1)
            fw.op("act", lambda: nc.scalar.mul(out=xt[i][:], in_=xt[i][:], mul=ALPHA), reads=[K(xt[i])], writes=[K(xt[i])])
            for j in range(4):
                fw.op("dve", lambda: nc.vector.scalar_tensor_tensor(out=xt[i][:], in0=yg[i][j][:], scalar=G4[:, t, j:j + 1],
                                                                    in1=xt[i][:], op0=ALU.mult, op1=ALU.add),
                      reads=[K(yg[i][j]), K(G4), K(xt[i])], writes=[K(xt[i])])
            layer_norm_tile(g, xt[i], lng, lnb, ot[i], None, es)
            fw.dma("sp", lambda e: e.dma_start(out=XO[t * 128:(t + 1) * 128, :], in_=ot[i][:]),
                   reads=[K(ot[i])], writes=[K(XOname, t)])


WEIGHT_SHAPES = {
    "ln_g": [4, 2, 1024], "ln_b": [4, 2, 1024],
    "nsa_w_in": [2, 1024, 2608], "nsa_cmp_pos": [2, 2, 32, 64], "nsa_cmp_w1": [2, 2, 2048, 256],
    "nsa_cmp_b1": [2, 2, 256], "nsa_cmp_w2": [2, 2, 256, 64], "nsa_cmp_b2": [2, 2, 64],
    "nsa_gate_b": [2, 48], "nsa_w_out": [2, 1024, 1024],
    "ml_w_in": [1, 1024, 3080], "ml_conv_w": [1, 4, 1024], "ml_conv_b": [1, 1024], "ml_gate_b": [1, 8],
    "ml_norm_g": [1, 1024], "ml_w_out": [1, 1024, 1024],
    "hg_w_in": [1, 1024, 4096], "hg_lower": [4, 1024], "hg_norm_g": [1, 1024], "hg_w_out": [1, 1024, 1024],
    "router_w": [4, 1024, 32], "router_b": [4, 32],
    "moe_w_gu": [4, 32, 1024, 2048], "moe_b_gu": [4, 32, 2048], "moe_w_down": [4, 32, 1024, 1024],
    "moe_b_down": [4, 32, 1024],
}
NB_MAX = 128


def make_consts(B):
    k_ = np.arange(128)
    c = np.zeros((128, 3 * 128 + NB_MAX + 8 + 128), np.float32)
    c[:, 3 * 128 + NB_MAX + 8:] = (k_[:, None] > k_[None, :]) & ((k_[:, None] // 64) == (k_[None, :] // 64))
    c[:, 0:128] = np.eye(128, dtype=np.float32)
    k = np.arange(128)
    c[:, 128:256] = (k[:, None] < k[None, :]).astype(np.float32)
    c[:, 256:384] = 1.0
    c[:, 384:384 + NB_MAX] = (np.arange(NB_MAX) * B)[None, :]
    c[:, 384 + NB_MAX:384 + NB_MAX + 8] = np.arange(8)[None, :] * 128 + k[:, None]
    return c


def build(NT, B, plan, wnames, wshapes=None):
    nc = bass.Bass("TRN2", target_bir_lowering=False)
    g = G()
    g.nc, g.NT, g.B = nc, NT, B
    NB = moe_nb(NT, B)
    wshapes = wshapes or WEIGHT_SHAPES
    g.w = {n: nc.dram_tensor(n, list(wshapes[n]), F32, kind="ExternalInput").ap() for n in wnames}
    xin = nc.dram_tensor("x", [NT, D], F32, kind="ExternalInput").ap()
    cst = nc.dram_tensor("cst", [128, 3 * 128 + NB_MAX + 8 + 128], F32, kind="ExternalInput").ap()
    out = nc.dram_tensor("out", [NT, D], F32, kind="ExternalOutput").ap()
    xa = nc.dram_tensor("xa", [NT, D], F32).ap()
    xbuf = nc.dram_tensor("xbuf", [NT, D], F32).ap()
    g.x1b = nc.dram_tensor("x1b", [NT, D], BF16).ap()
    g.xs = nc.dram_tensor("xs", [NB * B, D], BF16).ap()
    g.ys = nc.dram_tensor("ys", [NB * B, D], F32).ap()
    kinds = {ph[1] % 3 for ph in plan if ph[0] == "mixer"}
    if kinds:
        g.nsac = nc.dram_tensor("nsac", [128, NSAC_COLS], F32, kind="ExternalInput").ap()
        g.v2_scr = nc.dram_tensor("v2_scr", [T, 1024], BF16).ap()
        g.sg_scr = nc.dram_tensor("sg_scr", [T, 1024], BF16).ap()
        g.qT_scr = nc.dram_tensor("qT_scr", [1024, T], BF16).ap()
        g.kT_scr = nc.dram_tensor("kT_scr", [4, 256, T], BF16).ap()
        g.v_scr = nc.dram_tensor("v_scr", [T, 512], BF16).ap()
    g.o_scr = (nc.dram_tensor("o_scr", [T, 1024], BF16, kind="ExternalOutput") if DEBUG else nc.dram_tensor("o_scr", [T, 1024], BF16)).ap()
    with ExitStack() as es:
        fw = FW(nc, es)
        g.fw = fw
        g.ps = [es.enter_context(nc.psum_tensor("ps0", [128, 1024], BF16))]
        g.ps += [es.enter_context(nc.psum_tensor(f"ps{i}", [128, 512], F32)) for i in range(1, 8)]
        C = sbt(g, es, "cstf", [128, 3 * 128 + NB_MAX + 8 + 128], F32)
        g.C = C
        g.kcp = sbt(g, es, "kcp", [128, 8], F32)
        g.ident = sbt(g, es, "ident", [128, 128], F32)
        g.identb = sbt(g, es, "identb", [128, 128], BF16)
        g.trib = sbt(g, es, "trib", [128, 128], BF16)
        g.onesb = sbt(g, es, "onesb", [128, 128], BF16)
        g.iotaB = sbt(g, es, "iotaB", [128, NB_MAX], F32)
        g.ln_st = sbt(g, es, "ln_st", [128, 2, 6], F32)
        g.ln_mv = sbt(g, es, "ln_mv", [128, 4], F32)
        fw.dma("sp", lambda e: e.dma_start(out=C[:], in_=cst[:, :]), writes=[K(C)])
        fw.op("dve", lambda: nc.vector.tensor_copy(out=g.ident[:], in_=C[:, 0:128]), reads=[K(C)], writes=[K(g.ident)])
        fw.op("dve", lambda: nc.vector.tensor_copy(out=g.identb[:], in_=C[:, 0:128]), reads=[K(C)], writes=[K(g.identb)])
        fw.op("dve", lambda: nc.vector.tensor_copy(out=g.trib[:], in_=C[:, 128:256]), reads=[K(C)], writes=[K(g.trib)])
        fw.op("dve", lambda: nc.vector.tensor_copy(out=g.onesb[:], in_=C[:, 256:384]), reads=[K(C)], writes=[K(g.onesb)])
        fw.op("dve", lambda: nc.vector.tensor_copy(out=g.iotaB[:], in_=C[:, 384:384 + NB_MAX]), reads=[K(C)],
              writes=[K(g.iotaB)])
        fw.op("dve", lambda: nc.vector.tensor_copy(out=g.kcp[:], in_=C[:, 384 + NB_MAX:384 + NB_MAX + 8]), reads=[K(C)], writes=[K(g.kcp)])
        NTT = NT // 128
        keep = {"IDXG": sbt(g, es, "kIDXG", [128, NB, 8], I32), "IDXB": sbt(g, es, "kIDXB", [128, NB], I32),
                "POS4i": sbt(g, es, "kPOS", [128, NTT, 4], I32), "G4": sbt(g, es, "kG4", [128, NTT, 4], F32),
                "EB": sbt(g, es, "kEB", [128, NB], I32)}
        bufs = {"x": xin, "xa": xa, "xb": xbuf, "out": out}
        for ph in plan:
            kind = ph[0]
            if kind == "moe":
                _, l, src, dst = ph
                phase_route(g, l, bufs[src], src, keep)
                fw.barrier()
                phase_experts(g, l, keep)
                fw.barrier()
                phase_combine(g, l, bufs[src], src, bufs[dst], dst, keep)
                fw.barrier()
            elif kind == "mixer":
                _, l, src, dst = ph
                MIXERS[l % 3](g, l, bufs[src], src, bufs[dst], dst)
                fw.barrier()
        fw.finish(["out"])
        g.ninst = fw.ninst
    return nc, g


MIXERS = {}
DEBUG = False


T = 2048
TT = T // 128


def build_xT(g, es, X, Xname, row0, XT, xstage):
    nc, fw, ps = g.nc, g.fw, g.ps
    for t in range(TT):
        xt = xstage[t % 2]
        fw.dma("sp", lambda e: e.dma_start(out=xt[:], in_=X[row0 + t * 128:row0 + (t + 1) * 128, :]),
               reads=[K(Xname, (row0 // 128) + t)], writes=[K(xt)])
        for h in range(2):
            pst = ps[6 + h]
            for c in range(4):
                kc = h * 4 + c
                fw.op("pe", lambda: nc.tensor.transpose(out=pst[:, c * 128:(c + 1) * 128],
                                                        in_=xt[:, kc * 128:(kc + 1) * 128], identity=g.ident[:]),
                      reads=[K(xt), K(g.ident)], writes=[K(pst)], inc=(c == 3))
            eng = "dve" if h == 0 else "act"
            if h == 0:
                fw.op("dve", lambda: nc.vector.tensor_copy(out=XT[:, 0:4, t * 128:(t + 1) * 128],
                                                           in_=pst[:].rearrange("p (c n) -> p c n", c=4)),
                      reads=[K(pst)], writes=[K(XT, (t, 0))])
            else:
                fw.op("act", lambda: nc.scalar.copy(out=XT[:, 4:8, t * 128:(t + 1) * 128],
                                                    in_=pst[:].rearrange("p (c n) -> p c n", c=4)),
                      reads=[K(pst)], writes=[K(XT, (t, 1))])


def load_w_bf16(g, W, src, ncols, stage, col_ops):
    fw = g.fw
    for kc in range(8):
        st = stage[kc % 2]
        fw.dma("sp", lambda e: e.dma_start(out=st[:, 0:ncols], in_=src[kc * 128:(kc + 1) * 128, :]), writes=[K(st)])
        col_ops(kc, st)


def tail_load_x(g, X, Xname, row0, t, bufs, rows=128):
    xt = bufs["xt"][t % 2]
    r0 = row0 + t * rows
    g.fw.dma("sp", lambda e: e.dma_start(out=xt[0:rows, :], in_=X[r0:r0 + rows, :]), reads=[K(Xname, r0 // 128)], writes=[K(xt)])


def mixer_tail(g, es, l, X, Xname, XO, XOname, row0, t, OTOKt, WO, lng, lnb, bufs, okey, rows=128, xloaded=False):
    nc, fw, ps = g.nc, g.fw, g.ps
    OTt, xt, ot = bufs["OTt"][t % 2], bufs["xt"][t % 2], bufs["ot"][t % 2]
    pst = ps[0]
    r0 = row0 + t * rows
    xkey = K(Xname, r0 // 128)
    for kc in range(8):
        fw.op("pe", lambda: nc.tensor.transpose(out=pst[:, kc * 128:kc * 128 + rows], in_=OTOKt[:, kc * 128:(kc + 1) * 128],
                                                identity=g.identb[0:rows, 0:rows]),
              reads=[okey, K(g.identb)], writes=[K(pst)], inc=(kc == 7))
    fw.op("dve", lambda: nc.vector.tensor_copy(out=OTt[:, :, 0:rows], in_=pst[:].rearrange("p (c n) -> p c n", c=8)[:, :, 0:rows]),
          reads=[K(pst)], writes=[K(OTt)])
    if not xloaded:
        fw.dma("sp", lambda e: e.dma_start(out=xt[0:rows, :], in_=X[r0:r0 + rows, :]), reads=[xkey], writes=[K(xt)])
    for nh in range(2):
        py = ps[6 + nh]
        for kc in range(8):
            fw.op("pe", lambda: nc.tensor.matmul(out=py[0:rows, :], lhsT=OTt[:, kc, 0:rows], rhs=WO[:, kc, nh * 512:(nh + 1) * 512],
                                                 start=(kc == 0), stop=(kc == 7)),
                  reads=[K(OTt), K(WO)], writes=[K(py)], inc=(kc == 7))
        fw.op("dve", lambda: nc.vector.scalar_tensor_tensor(out=xt[0:rows, nh * 512:(nh + 1) * 512],
                                                            in0=xt[0:rows, nh * 512:(nh + 1) * 512], scalar=ALPHA, in1=py[0:rows, :],
                                                            op0=ALU.mult, op1=ALU.add),
              reads=[K(xt), K(py)], writes=[K(xt)])
    layer_norm_tile(g, xt, lng, lnb, ot, None, es, rows)
    fw.dma("sp", lambda e: e.dma_start(out=XO[r0:r0 + rows, :], in_=ot[0:rows, :]),
           reads=[K(ot)], writes=[K(XOname, r0 // 128)])


def load_tail_weights(g, es, l, w_out_ap, stage):
    nc, fw = g.nc, g.fw
    WO = sbt(g, es, "mWO", [128, 8, 1024], BF16)
    lng = sbt(g, es, "mLNG", [128, D], F32)
    lnb = sbt(g, es, "mLNB", [128, D], F32)
    fw.dma("sp", lambda e: e.dma_start(out=lng[:], in_=g.w["ln_g"][l, 0, :].partition_broadcast(128)), writes=[K(lng)])
    fw.dma("sp", lambda e: e.dma_start(out=lnb[:], in_=g.w["ln_b"][l, 0, :].partition_broadcast(128)), writes=[K(lnb)])
    load_w_bf16(g, WO, w_out_ap, 1024, stage,
                lambda kc, st: fw.op("act", lambda: nc.scalar.copy(out=WO[:, kc, :], in_=st[:, 0:1024]),
                                     reads=[K(st)], writes=[K(WO, kc)]))
    bufs = {"OTt": [sbt(g, es, f"mOTt{i}", [128, 8, 128], BF16) for i in range(2)],
            "xt": [sbt(g, es, f"mxt{i}", [128, D], F32) for i in range(2)],
            "ot": [sbt(g, es, "mot", [128, D], F32)] * 2}
    return WO, lng, lnb, bufs


NSAC_COLS = 2048 + 4096 + 2048 + 32 + 512 + 512


def make_nsa_consts():
    c = np.zeros((128, NSAC_COLS), np.float32)
    t = np.arange(T)
    cc = np.arange(128)
    c[:, 0:2048] = ((16 * cc[:, None] + 31) <= t[None, :]) & (cc[:, None] < 127)
    sl = np.arange(128)[:, None]
    tl = np.arange(512)[None, :]
    for di in range(8):
        delta = di * 128 - 384
        lag = delta + tl - sl
        c[:, 2048 + di * 512:2048 + (di + 1) * 512] = (lag >= 0) & (lag < 512)
    blk = np.arange(32)
    c[0:32, 6144:8192] = (t[None, :] // 64 == blk[:, None])
    cmp_start = np.arange(127) * 16
    sel_start = np.arange(32) * 64
    overlap = (np.minimum(cmp_start[:, None] + 32, sel_start[None, :] + 64) - np.maximum(cmp_start[:, None], sel_start[None, :]))
    c[0:127, 8192:8224] = np.clip(overlap, 0, None) / 16.0
    cur = (t // 64)[:, None]
    forced = (blk[None, :] == 0) | (blk[None, :] == cur) | (blk[None, :] == cur - 1)
    valid = blk[None, :] <= cur
    mul = (valid & ~forced).astype(np.float32)
    add = np.where(forced, 1e30, np.where(valid, 0.0, -1e30)).astype(np.float32)
    c[:, 8224:8736] = mul.reshape(16, 128, 32).transpose(1, 0, 2).reshape(128, 512)
    c[:, 8736:9248] = add.reshape(16, 128, 32).transpose(1, 0, 2).reshape(128, 512)
    return c


def phase_nsa(g, l, X, Xname, XO, XOname):
    nc, fw, ps = g.nc, g.fw, g.ps
    sl = l // 3
    nseq = g.NT // T
    w_in = g.w["nsa_w_in"][sl]
    qS, kS, vS, oS = g.qT_scr, g.kT_scr, g.v_scr, g.o_scr
    with ExitStack() as es:
        st1 = sbt(g, es, "nST", [128, 2608], F32)
        CMPM = sbt(g, es, "nCMPM", [128, 2048], BF16)
        WINM = sbt(g, es, "nWINM", [128, 8, 512], BF16)
        EXPB = sbt(g, es, "nEXPB", [32, 2048], BF16)
        MULA = sbt(g, es, "nMULA", [128, 2, 16, 32], F32)
        VCA = sbt(g, es, "nVCA", [128, 97], BF16)
        for (dst, key, c0, rows) in ((CMPM[:], K(CMPM), 0, 128), (WINM[:, 0:4, :], K(WINM, 0), 2048, 128),
                                     (WINM[:, 4:8, :], K(WINM, 1), 4096, 128), (EXPB[:], K(EXPB), 6144, 32)):
            fw.dma("sp", lambda e: e.dma_start(out=st1[0:rows, 0:2048], in_=g.nsac[0:rows, c0:c0 + 2048]), writes=[K(st1)])
            src = st1[0:rows, 0:2048] if len(dst.shape) == 2 else st1[:, 0:2048].rearrange("p (a b) -> p a b", a=4)
            fw.op("dve", lambda: nc.vector.tensor_copy(out=dst, in_=src), reads=[K(st1)], writes=[key])
        fw.dma("sp", lambda e: e.dma_start(out=st1[:, 0:1056], in_=g.nsac[:, 8192:9248]), writes=[K(st1)])
        fw.op("dve", lambda: nc.vector.tensor_copy(out=VCA[:, 65:97], in_=st1[:, 0:32]), reads=[K(st1)], writes=[K(VCA, "c")])
        fw.op("dve", lambda: nc.vector.tensor_copy(out=MULA[:].rearrange("p a t b -> p (a t b)"), in_=st1[:, 32:1056]),
              reads=[K(st1)], writes=[K(MULA)])
        fw.op("dve", lambda: nc.vector.memset(VCA[:, 64:65], 1.0), writes=[K(VCA, "o")])
        W2 = sbt(g, es, "nW2", [128, 2, 2, 64], BF16)
        B1 = sbt(g, es, "nB1", [128, 2, 2], F32)
        B2K = sbt(g, es, "nB2K", [64, 1], F32)
        B2V = sbt(g, es, "nB2V", [128, 64], F32)
        POST = sbt(g, es, "nPOST", [64, 2, 32], BF16)
        CONSTH = sbt(g, es, "nCONSTH", [128, 2, 2], F32)
        GB = sbt(g, es, "nGB", [128, 48], F32)
        SGT = sbt(g, es, "nSGT", [128, TT, 48], F32)
        w1 = g.w["nsa_cmp_w1"][sl]
        w2 = g.w["nsa_cmp_w2"][sl]
        fw.dma("sp", lambda e: e.dma_start(out=st1[:, 0:256].rearrange("p (j c d) -> p j c d", j=2, c=2),
                                           in_=w2.rearrange("j (c p) d -> p j c d", p=128)), writes=[K(st1)])
        fw.op("dve", lambda: nc.vector.tensor_copy(out=W2[:], in_=st1[:, 0:256].rearrange("p (j c d) -> p j c d", j=2, c=2)),
              reads=[K(st1)], writes=[K(W2)])
        fw.dma("sp", lambda e: e.dma_start(out=B1[:], in_=g.w["nsa_cmp_b1"][sl].rearrange("j (c p) -> p j c", p=128),
                                           allow_slow_non_contiguous=True), writes=[K(B1)])
        fw.dma("sp", lambda e: e.dma_start(out=B2K[:, :], in_=g.w["nsa_cmp_b2"][sl, 0, :].rearrange("(d o) -> d o", o=1),
                                           allow_slow_non_contiguous=True), writes=[K(B2K)])
        fw.dma("sp", lambda e: e.dma_start(out=B2V[:], in_=g.w["nsa_cmp_b2"][sl, 1, :].partition_broadcast(128)), writes=[K(B2V)])
        fw.dma("sp", lambda e: e.dma_start(out=GB[:], in_=g.w["nsa_gate_b"][sl, :].partition_broadcast(128)), writes=[K(GB)])
        fw.dma("sp", lambda e: e.dma_start(out=st1[0:64, 0:64].rearrange("d (j p) -> d j p", j=2),
                                           in_=g.w["nsa_cmp_pos"][sl].rearrange("j p d -> d j p"),
                                           allow_slow_non_contiguous=True), writes=[K(st1)])
        fw.op("dve", lambda: nc.vector.tensor_copy(out=POST[:], in_=st1[0:64, 0:64].rearrange("d (j p) -> d j p", j=2)),
              reads=[K(st1)], writes=[K(POST)])
        WO, lng, lnb, tb_bufs = load_tail_weights(g, es, l, g.w["nsa_w_out"][sl], [st1, st1])
        first = [True]

        for s in range(nseq):
            row0 = s * T
            with ExitStack() as esA:
                WQ = sbt(g, esA, "nWQ", [128, 8, 1024], BF16)
                WK = sbt(g, esA, "nWK", [128, 8, 4, 256], BF16)
                WVg = sbt(g, esA, "nWVg", [128, 8, 4, 128], BF16)
                WG = sbt(g, esA, "nWG", [128, 8, 48], BF16)
                XT = sbt(g, esA, "nXT", [128, 8, T], BF16)
                EV = [sbt(g, esA, f"nEV{i}", [128, 512], BF16) for i in range(3)]
                kcols = (1024, 1280, 1536, 2048)

                def cast_in(kc, st):
                    fw.op("act", lambda: nc.scalar.mul(out=WQ[:, kc, :], in_=st[:, 0:1024], mul=0.125), reads=[K(st)], writes=[K(WQ, kc)])
                    for ty, c0 in enumerate(kcols):
                        if ty % 2 == 0:
                            fw.op("dve", lambda: nc.vector.tensor_copy(out=WK[:, kc, ty, :], in_=st[:, c0:c0 + 256]),
                                  reads=[K(st)], writes=[K(WK, (kc, ty))])
                        else:
                            fw.op("pool", lambda: nc.gpsimd.tensor_copy(out=WK[:, kc, ty, :], in_=st[:, c0:c0 + 256]),
                                  reads=[K(st)], writes=[K(WK, (kc, ty))])
                    fw.op("dve", lambda: nc.vector.tensor_copy(out=WVg[:, kc, :, 0:64], in_=st[:, 1792:2048].rearrange("p (g d) -> p g d", g=4)),
                          reads=[K(st)], writes=[K(WVg, (kc, 0))])
                    fw.op("pool", lambda: nc.gpsimd.tensor_copy(out=WVg[:, kc, :, 64:128], in_=st[:, 2304:2560].rearrange("p (g d) -> p g d", g=4)),
                          reads=[K(st)], writes=[K(WVg, (kc, 1))])
                    fw.op("dve", lambda: nc.vector.tensor_copy(out=WG[:, kc, :], in_=st[:, 2560:2608]), reads=[K(st)], writes=[K(WG, kc)])

                load_w_bf16(g, None, w_in, 2608, [st1, st1], cast_in)
                build_xT(g, esA, X, Xname, row0, XT, tb_bufs["xt"])
                nev = [0]

                def evac_store(pS, ncol, dst_ap, dkey):
                    E = EV[nev[0] % 3]
                    if nev[0] % 2 == 0:
                        fw.op("act", lambda: nc.scalar.copy(out=E[:, 0:ncol], in_=pS[:, 0:ncol]), reads=[K(pS)], writes=[K(E)])
                    else:
                        fw.op("dve", lambda: nc.vector.tensor_copy(out=E[:, 0:ncol], in_=pS[:, 0:ncol]), reads=[K(pS)], writes=[K(E)])
                    nev[0] += 1
                    fw.dma("sp", lambda e: e.dma_start(out=dst_ap, in_=E[:, 0:ncol]), reads=[K(E)], writes=[dkey])

                for t in range(TT):
                    for kc in range(8):
                        fw.op("pe", lambda: nc.tensor.matmul(out=ps[4][:, 0:48], lhsT=XT[:, kc, t * 128:(t + 1) * 128], rhs=WG[:, kc, :],
                                                             start=(kc == 0), stop=(kc == 7)),
                              reads=[K(XT), K(WG)], writes=[K(ps[4])], inc=(kc == 7))
                    fw.op("dve", lambda: nc.vector.tensor_tensor(out=SGT[:, t, :], in0=ps[4][:, 0:48], in1=GB[:], op=ALU.add),
                          reads=[K(ps[4]), K(GB)], writes=[K(SGT, t)])
                fw.op("act", lambda: nc.scalar.activation(out=SGT[:], in_=SGT[:], func=AF.Sigmoid), reads=[K(SGT)], writes=[K(SGT)])
                np_ = [0]
                for c in range(8):
                    for tb in range(4):
                        pS = ps[2 + np_[0] % 2]
                        np_[0] += 1
                        for kc in range(8):
                            fw.op("pe", lambda: nc.tensor.matmul(out=pS[:, :], lhsT=WQ[:, kc, c * 128:(c + 1) * 128],
                                                                 rhs=XT[:, kc, tb * 512:(tb + 1) * 512], start=(kc == 0), stop=(kc == 7)),
                                  reads=[K(WQ), K(XT)], writes=[K(pS)], inc=(kc == 7))
                        evac_store(pS, 512, qS[c * 128:(c + 1) * 128, tb * 512:(tb + 1) * 512], K("qS", (c, tb)))
                for ty in range(4):
                    for c in range(2):
                        for tb in range(4):
                            pS = ps[2 + np_[0] % 2]
                            np_[0] += 1
                            for kc in range(8):
                                fw.op("pe", lambda: nc.tensor.matmul(out=pS[:, :], lhsT=WK[:, kc, ty, c * 128:(c + 1) * 128],
                                                                     rhs=XT[:, kc, tb * 512:(tb + 1) * 512], start=(kc == 0), stop=(kc == 7)),
                                      reads=[K(WK), K(XT)], writes=[K(pS)], inc=(kc == 7))
                            evac_store(pS, 512, kS[ty, c * 128:(c + 1) * 128, tb * 512:(tb + 1) * 512], K("kS", (ty, c, tb)))
                for t in range(TT):
                    pS = ps[2 + np_[0] % 2]
                    np_[0] += 1
                    for kc in range(8):
                        fw.op("pe", lambda: nc.tensor.matmul(out=pS[:, :], lhsT=XT[:, kc, t * 128:(t + 1) * 128],
                                                             rhs=WVg[:, kc, :, :].rearrange("p g d -> p (g d)"), start=(kc == 0), stop=(kc == 7)),
                              reads=[K(XT), K(WVg)], writes=[K(pS)], inc=(kc == 7))
                    evac_store(pS, 512, vS[t * 128:(t + 1) * 128, :], K("vS", t))
            fw.barrier()
            with ExitStack() as esB:
                W1 = sbt(g, esB, "nW1", [64, 2, 32, 256], BF16)
                QTg = sbt(g, esB, "nQTg", [64, 4, T], BF16)
                KT4 = sbt(g, esB, "nKT4", [64, 4, T], BF16)
                VA = sbt(g, esB, "nVA", [128, TT, 2, 65], BF16)
                HID = sbt(g, esB, "nHID", [128, 2, 2, 128], BF16)
                HU = [sbt(g, esB, f"nHU{i}", [128, 128], F32) for i in range(3)]
                KCMPT = sbt(g, esB, "nKCMPT", [64, 128], BF16)
                OACC = sbt(g, esB, "nOACC", [128, 4, 4, 64], F32)
                OB = sbt(g, esB, "nOB", [128, 4, 256], BF16)
                EB_ = [sbt(g, esB, f"nE{i}", [128, 512], BF16) for i in range(4)]
                PT = [sbt(g, esB, f"nPT{i}", [128, 512], BF16) for i in range(4)]
                PTC = [sbt(g, esB, f"nPTC{i}", [128, 512], BF16) for i in range(4)]
                SELXM = [sbt(g, esB, f"nSELXM{i}", [128, 512], BF16) for i in range(3)]
                SELT = sbt(g, esB, "nSELT", [32, 512], BF16)
                RC = sbt(g, esB, "nRC", [128, 4, 97], F32)
                AC = sbt(g, esB, "nAC", [128, 16, 65], F32)
                ZR = sbt(g, esB, "nZR", [128, 16], F32)
                CO = sbt(g, esB, "nCO", [128, 16], F32)
                TMPA = sbt(g, esB, "nTMPA", [128, 16, 64], F32)
                IMP = sbt(g, esB, "nIMP", [128, 32], F32)
                TOP8 = sbt(g, esB, "nTOP8", [128, 8], F32)
                SELM = sbt(g, esB, "nSELM", [128, 32], BF16)
                OTK = [sbt(g, esB, f"nOTK{i}", [128, D], BF16) for i in range(2)]
                fw.op("pool", lambda: nc.gpsimd.memset(VA[:, :, :, 64:65], 1.0), writes=[K(VA, "ones")])
                for j in range(2):
                    for pq in range(4):
                        fw.dma("sp", lambda e: e.dma_start(
                            out=st1[0:64, 0:2048].rearrange("d (p h) -> d p h", p=8),
                            in_=w1[j, pq * 512:(pq + 1) * 512, :].rearrange("(p d) h -> d p h", d=64)), writes=[K(st1)])
                        fw.op("act", lambda: nc.scalar.copy(out=W1[:, j, pq * 8:(pq + 1) * 8, :],
                                                            in_=st1[0:64, 0:2048].rearrange("d (p h) -> d p h", p=8)),
                              reads=[K(st1)], writes=[K(W1, (j, pq))])
                if first[0]:
                    first[0] = False
                    for j in range(2):
                        for hc in range(2):
                            for p in range(32):
                                fw.op("pe", lambda: nc.tensor.matmul(out=ps[4][:, (j * 2 + hc):(j * 2 + hc) + 1],
                                                                     lhsT=W1[:, j, p, hc * 128:(hc + 1) * 128],
                                                                     rhs=POST[:, j, p:p + 1], start=(p == 0), stop=(p == 31)),
                                      reads=[K(W1), K(POST)], writes=[K(ps[4])], inc=(p == 31))
                    fw.op("dve", lambda: nc.vector.tensor_tensor(out=CONSTH[:].rearrange("p j c -> p (j c)"), in0=ps[4][:, 0:4],
                                                                 in1=B1[:].rearrange("p j c -> p (j c)"), op=ALU.add),
                          reads=[K(ps[4]), K(B1)], writes=[K(CONSTH)])
                cnt = {"s": 0, "s3": 0, "e": 0, "p": 0, "x": 0}

                def accum_evac(tb, gi, which):
                    for bk in range(3):
                        n = 6 if bk < 2 else 4
                        fw.op("dve", lambda: nc.vector.tensor_copy(out=AC[:, bk * 6:bk * 6 + n, :],
                                                                   in_=ps[5 + bk][:, 0:n * 65].rearrange("p (a c) -> p a c", c=65)),
                              reads=[K(ps[5 + bk])], writes=[K(AC, bk)])
                    fw.op("dve", lambda: nc.vector.tensor_scalar_max(out=ZR[:], in0=AC[:, :, 64], scalar1=1e-30), reads=[K(AC)], writes=[K(ZR)])
                    fw.op("dve", lambda: nc.vector.reciprocal(out=ZR[:], in_=ZR[:]), reads=[K(ZR)], writes=[K(ZR)])
                    gview = SGT[:, tb * 4:(tb + 1) * 4, gi * 12 + which:gi * 12 + 12:3]
                    fw.op("dve", lambda: nc.vector.tensor_tensor(out=CO[:].rearrange("p (t h) -> p t h", t=4),
                                                                 in0=ZR[:].rearrange("p (t h) -> p t h", t=4), in1=gview, op=ALU.mult),
                          reads=[K(ZR), K(SGT)], writes=[K(CO)])
                    fw.op("dve", lambda: nc.vector.tensor_tensor(out=TMPA[:], in0=AC[:, :, 0:64],
                                                                 in1=CO[:].unsqueeze(2).to_broadcast([128, 16, 64]), op=ALU.mult),
                          reads=[K(AC), K(CO)], writes=[K(TMPA)])
                    fw.op("pool", lambda: nc.gpsimd.tensor_tensor(out=OACC[:].rearrange("p t h d -> p (t h) d"),
                                                                  in0=OACC[:].rearrange("p t h d -> p (t h) d"), in1=TMPA[:], op=ALU.add),
                          reads=[K(OACC), K(TMPA)], writes=[K(OACC)])

                def acc_ap(h, tt):
                    a = tt * 4 + h
                    return ps[5 + a // 6], (a % 6) * 65

                def attend(tb, kts, kty, vslot, maskfn, which, gi, span):
                    for bk in range(3):
                        fw.op("dve", lambda: nc.vector.memset(ps[5 + bk][:, :], 0.0), writes=[K(ps[5 + bk])])
                    units = [(kt, h) for kt in kts for h in range(4)]

                    def cols(kt):
                        lo, hi = span(kt)
                        return lo * 128, (hi + 1) * 128

                    def emit_S(u):
                        kt, h = u
                        c_lo, c_hi = cols(kt)
                        pS = ps[1 + cnt["s3"] % 3]
                        cnt["s3"] += 1
                        fw.op("pe", lambda: nc.tensor.matmul(out=pS[:, c_lo:c_hi], lhsT=KT4[:, kty, kt * 128:(kt + 1) * 128],
                                                             rhs=QTg[:, h, tb * 512 + c_lo:tb * 512 + c_hi], start=True, stop=True),
                              reads=[K(KT4), K(QTg)], writes=[K(pS)])
                        return pS

                    pend = [emit_S(units[0])]
                    if len(units) > 1:
                        pend.append(emit_S(units[1]))
                    mk = None
                    masks = {}
                    for i, (kt, h) in enumerate(units):
                        pS = pend.pop(0)
                        if i + 2 < len(units):
                            pend.append(emit_S(units[i + 2]))
                        c_lo, c_hi = cols(kt)
                        if h == 0:
                            if kt not in masks:
                                masks[kt] = maskfn(kt, c_lo, c_hi)
                            mk = masks.pop(kt)
                        E = EB_[cnt["e"] % 4]
                        cnt["e"] += 1
                        fw.op("act", lambda: nc.scalar.activation(out=E[:, c_lo:c_hi], in_=pS[:, c_lo:c_hi], func=AF.Exp),
                              reads=[K(pS)], writes=[K(E)])
                        P = PT[cnt["p"] % 4]
                        cnt["p"] += 1
                        fw.op("dve", lambda: nc.vector.tensor_tensor(out=P[:, c_lo:c_hi], in0=E[:, c_lo:c_hi], in1=mk[0], op=ALU.mult),
                              reads=[K(E), mk[1]], writes=[K(P)])
                        if h == 1:
                            nk = kts.index(kt) + 1
                            if nk < len(kts):
                                masks[kts[nk]] = maskfn(kts[nk], *cols(kts[nk]))
                        lo, hi = span(kt)
                        for tt in range(lo, hi + 1):
                            pa, c0 = acc_ap(h, tt)
                            last = max(k2 for k2 in kts if span(k2)[0] <= tt <= span(k2)[1])
                            fw.op("pe", lambda: nc.tensor.matmul(out=pa[:, c0:c0 + 65], lhsT=P[:, tt * 128:(tt + 1) * 128],
                                                                 rhs=VA[:, kt, vslot, :], start=False, stop=(kt == last),
                                                                 skip_group_check=True),
                                  reads=[K(P), K(VA)], writes=[K(pa, c0)], inc=(kt == last))
                    accum_evac(tb, gi, which)

                for gi in range(4):
                    for h in range(4):
                        hg = gi * 4 + h
                        fw.dma("sp", lambda e: e.dma_start(out=QTg[:, h, :], in_=qS[hg * 64:(hg + 1) * 64, :]),
                               reads=[K("qS")], writes=[K(QTg, h)])
                    for ty in range(4):
                        fw.dma("sp", lambda e: e.dma_start(out=KT4[:, ty, :], in_=kS[ty, gi * 64:(gi + 1) * 64, :]),
                               reads=[K("kS")], writes=[K(KT4, ty)])
                    for a in range(2):
                        fw.dma("sp", lambda e: e.dma_start(
                            out=VA[:, :, a, 0:64],
                            in_=vS[:, gi * 128 + a * 64:gi * 128 + (a + 1) * 64].rearrange("(t p) d -> p t d", p=128)),
                            reads=[K("vS")], writes=[K(VA, ("v", a))])
                    for j in range(2):
                        for hc in range(2):
                            pS = ps[2 + (j * 2 + hc) % 2]
                            for p in range(32):
                                fw.op("pe", lambda: nc.tensor.matmul(out=pS[:, 0:127], lhsT=W1[:, j, p, hc * 128:(hc + 1) * 128],
                                                                     rhs=KT4[:, j, p:p + 16 * 126 + 1:16], start=(p == 0), stop=(p == 31)),
                                      reads=[K(W1), K(KT4, j)], writes=[K(pS)], inc=(p == 31))
                            u, u2, sg = HU
                            fw.op("act", lambda: nc.scalar.activation(out=u[:, 0:127], in_=pS[:, 0:127], func=AF.Identity,
                                                                      bias=CONSTH[:, j, hc:hc + 1]), reads=[K(pS), K(CONSTH)], writes=[K(u)])
                            fw.op("dve", lambda: nc.vector.tensor_tensor(out=u2[:, 0:127], in0=u[:, 0:127], in1=u[:, 0:127], op=ALU.mult),
                                  reads=[K(u)], writes=[K(u2)])
                            fw.op("dve", lambda: nc.vector.tensor_scalar(out=u2[:, 0:127], in0=u2[:, 0:127], scalar1=0.044715, scalar2=1.0,
                                                                         op0=ALU.mult, op1=ALU.add), reads=[K(u2)], writes=[K(u2)])
                            fw.op("dve", lambda: nc.vector.tensor_tensor(out=u2[:, 0:127], in0=u2[:, 0:127], in1=u[:, 0:127], op=ALU.mult),
                                  reads=[K(u2), K(u)], writes=[K(u2)])
                            fw.op("act", lambda: nc.scalar.activation(out=sg[:, 0:127], in_=u2[:, 0:127], func=AF.Sigmoid, scale=1.5957691216),
                                  reads=[K(u2)], writes=[K(sg)])
                            fw.op("dve", lambda: nc.vector.tensor_tensor(out=HID[:, j, hc, 0:127], in0=u[:, 0:127], in1=sg[:, 0:127], op=ALU.mult),
                                  reads=[K(u), K(sg)], writes=[K(HID, (j, hc))])
                    pS = ps[2]
                    for hc in range(2):
                        fw.op("pe", lambda: nc.tensor.matmul(out=pS[0:64, 0:127], lhsT=W2[:, 0, hc, :], rhs=HID[:, 0, hc, 0:127],
                                                             start=(hc == 0), stop=(hc == 1)),
                              reads=[K(W2), K(HID)], writes=[K(pS)], inc=(hc == 1))
                    fw.op("act", lambda: nc.scalar.activation(out=KCMPT[:, 0:127], in_=pS[0:64, 0:127], func=AF.Identity, bias=B2K[:, 0:1]),
                          reads=[K(pS), K(B2K)], writes=[K(KCMPT)])
                    pS = ps[3]
                    for hc in range(2):
                        fw.op("pe", lambda: nc.tensor.matmul(out=pS[0:127, 0:64], lhsT=HID[:, 1, hc, 0:127], rhs=W2[:, 1, hc, :],
                                                             start=(hc == 0), stop=(hc == 1)),
                              reads=[K(W2), K(HID)], writes=[K(pS)], inc=(hc == 1))
                    fw.op("dve", lambda: nc.vector.tensor_tensor(out=VCA[0:127, 0:64], in0=pS[0:127, 0:64], in1=B2V[0:127, :], op=ALU.add),
                          reads=[K(pS), K(B2V)], writes=[K(VCA, "v")])
                    for tb in range(4):
                        for h in range(4):
                            pS = ps[2 + cnt["s"] % 2]
                            cnt["s"] += 1
                            fw.op("pe", lambda: nc.tensor.matmul(out=pS[0:127, :], lhsT=KCMPT[:, 0:127],
                                                                 rhs=QTg[:, h, tb * 512:(tb + 1) * 512], start=True, stop=True),
                                  reads=[K(KCMPT), K(QTg)], writes=[K(pS)])
                            E = EB_[cnt["e"] % 3]
                            cnt["e"] += 1
                            fw.op("act", lambda: nc.scalar.activation(out=E[0:127, :], in_=pS[0:127, :], func=AF.Exp), reads=[K(pS)], writes=[K(E)])
                            fw.op("dve", lambda: nc.vector.tensor_tensor(out=PTC[h][0:127, :], in0=E[0:127, :],
                                                                         in1=CMPM[0:127, tb * 512:(tb + 1) * 512], op=ALU.mult),
                                  reads=[K(E), K(CMPM)], writes=[K(PTC[h])])
                        for tt in range(4):
                            tile = tb * 4 + tt
                            for h in range(4):
                                fw.op("pe", lambda: nc.tensor.matmul(out=ps[4][:, h * 97:(h + 1) * 97], lhsT=PTC[h][0:127, tt * 128:(tt + 1) * 128],
                                                                     rhs=VCA[0:127, :], start=True, stop=True),
                                      reads=[K(PTC[h]), K(VCA)], writes=[K(ps[4])], inc=(h == 3))
                            fw.op("dve", lambda: nc.vector.tensor_copy(out=RC[:], in_=ps[4][:, 0:388].rearrange("p (h c) -> p h c", h=4)),
                                  reads=[K(ps[4])], writes=[K(RC)])
                            fw.op("dve", lambda: nc.vector.tensor_scalar_max(out=ZR[:, 0:4], in0=RC[:, :, 64], scalar1=1e-30),
                                  reads=[K(RC)], writes=[K(ZR)])
                            fw.op("dve", lambda: nc.vector.reciprocal(out=ZR[:, 0:4], in_=ZR[:, 0:4]), reads=[K(ZR)], writes=[K(ZR)])
                            fw.op("dve", lambda: nc.vector.tensor_scalar(out=IMP[:], in0=RC[:, 0, 65:97], scalar1=ZR[:, 0:1], scalar2=None,
                                                                         op0=ALU.mult), reads=[K(RC), K(ZR)], writes=[K(IMP)])
                            for h in range(1, 4):
                                fw.op("dve", lambda: nc.vector.scalar_tensor_tensor(out=IMP[:], in0=RC[:, h, 65:97], scalar=ZR[:, h:h + 1],
                                                                                    in1=IMP[:], op0=ALU.mult, op1=ALU.add),
                                      reads=[K(RC), K(ZR), K(IMP)], writes=[K(IMP)])
                            fw.op("dve", lambda: nc.vector.tensor_tensor(out=IMP[:], in0=IMP[:], in1=MULA[:, 0, tile, :], op=ALU.mult),
                                  reads=[K(IMP), K(MULA)], writes=[K(IMP)])
                            fw.op("dve", lambda: nc.vector.tensor_tensor(out=IMP[:], in0=IMP[:], in1=MULA[:, 1, tile, :], op=ALU.add),
                                  reads=[K(IMP), K(MULA)], writes=[K(IMP)])
                            fw.op("dve", lambda: nc.vector.max(out=TOP8[:], in_=IMP[:]), reads=[K(IMP)], writes=[K(TOP8)])
                            fw.op("dve", lambda: nc.vector.tensor_scalar(out=SELM[:], in0=IMP[:], scalar1=TOP8[:, 7:8], scalar2=None,
                                                                         op0=ALU.is_ge), reads=[K(IMP), K(TOP8)], writes=[K(SELM)])
                            fw.op("pe", lambda: nc.tensor.transpose(out=ps[0][0:32, tt * 128:(tt + 1) * 128], in_=SELM[:], identity=g.identb[:]),
                                  reads=[K(SELM), K(g.identb)], writes=[K(ps[0])])
                            fw.op("dve", lambda: nc.vector.tensor_tensor(out=CO[:, 0:4], in0=ZR[:, 0:4], in1=SGT[:, tile, gi * 12:gi * 12 + 12:3],
                                                                         op=ALU.mult), reads=[K(ZR), K(SGT)], writes=[K(CO)])
                            fw.op("dve", lambda: nc.vector.tensor_tensor(out=OACC[:, tt, :, :], in0=RC[:, :, 0:64],
                                                                         in1=CO[:, 0:4].unsqueeze(2).to_broadcast([128, 4, 64]), op=ALU.mult),
                                  reads=[K(RC), K(CO)], writes=[K(OACC)])
                        fw.op("act", lambda: nc.scalar.copy(out=SELT[:], in_=ps[0][0:32, 0:512]), reads=[K(ps[0])], writes=[K(SELT)])

                        def sel_mask(kt, c_lo, c_hi, tb=tb):
                            delta = tb * 512 - kt * 128
                            M = SELXM[cnt["x"] % 3]
                            cnt["x"] += 1
                            fw.op("pe", lambda: nc.tensor.matmul(out=ps[4][:, c_lo:c_hi], lhsT=EXPB[:, kt * 128:(kt + 1) * 128], rhs=SELT[:, c_lo:c_hi],
                                                                 start=True, stop=True), reads=[K(EXPB), K(SELT)], writes=[K(ps[4])])
                            if delta <= 0:
                                di = (delta + 384) // 128
                                fw.op("dve", lambda: nc.vector.tensor_tensor(out=M[:, c_lo:c_hi], in0=ps[4][:, c_lo:c_hi], in1=WINM[:, di, c_lo:c_hi], op=ALU.mult),
                                      reads=[K(ps[4]), K(WINM)], writes=[K(M)])
                            else:
                                fw.op("act", lambda: nc.scalar.copy(out=M[:, c_lo:c_hi], in_=ps[4][:, c_lo:c_hi]), reads=[K(ps[4])], writes=[K(M)])
                            return (M[:, c_lo:c_hi], K(M))

                        def win_mask(kt, c_lo, c_hi, tb=tb):
                            di = (tb * 512 - kt * 128 + 384) // 128
                            return (WINM[:, di, c_lo:c_hi], K(WINM))

                        attend(tb, list(range(max(0, tb * 4 - 4), tb * 4 + 4)), 3, 1, win_mask, 2, gi,
                               lambda kt, tb=tb: (max(0, kt - tb * 4), min(3, kt + 4 - tb * 4)))
                        attend(tb, list(range(0, tb * 4 + 4)), 2, 0, sel_mask, 1, gi,
                               lambda kt, tb=tb: (max(0, kt - tb * 4), 3))
                        fw.op("act", lambda: nc.scalar.copy(out=OB[:], in_=OACC[:].rearrange("p t h d -> p t (h d)")),
                              reads=[K(OACC)], writes=[K(OB)])
                        fw.dma("sp", lambda e: e.dma_start(
                            out=oS[tb * 512:(tb + 1) * 512, gi * 256:(gi + 1) * 256].rearrange("(t p) c -> p t c", p=128), in_=OB[:]),
                            reads=[K(OB)], writes=[K("oS", (tb, gi))])
                def pre(t):
                    fw.dma("sp", lambda e: e.dma_start(out=OTK[t % 2][:], in_=oS[t * 128:(t + 1) * 128, :]), reads=[K("oS")], writes=[K(OTK[t % 2])])
                    tail_load_x(g, X, Xname, row0, t, tb_bufs)

                pre(0)
                for t in range(TT):
                    ok = OTK[t % 2]
                    if t + 1 < TT:
                        pre(t + 1)
                    mixer_tail(g, esB, l, X, Xname, XO, XOname, row0, t, ok[:], WO, lng, lnb, tb_bufs, K(ok), xloaded=True)
            fw.barrier()


MIXERS[0] = phase_nsa


class ColView:
    def __init__(self, tile, n):
        self.tile, self.n, self.name = tile, n, tile.name

    def __getitem__(self, key):
        return self.tile[:, 0:self.n][key]


class RowView:
    def __init__(self, tile, n):
        self.tile, self.n, self.name = tile, n, tile.name

    def __getitem__(self, key):
        return self.tile[0:4, 0:self.n][key]


def row_scan(g, a, b, n, op):
    nc, fw = g.nc, g.fw
    s = 1
    src, dst = a, b
    while s < n:
        fw.op("dve", lambda: nc.vector.tensor_tensor(out=dst[:, s:n], in0=src[:, s:n], in1=src[:, 0:n - s], op=op),
              reads=[K(src)], writes=[K(dst, "hi")])
        fw.op("dve", lambda: nc.vector.tensor_copy(out=dst[:, 0:s], in_=src[:, 0:s]), reads=[K(src)], writes=[K(dst, "lo")])
        src, dst = dst, src
        s *= 2
    return src, dst


def phase_mlstm(g, l, X, Xname, XO, XOname):
    nc, fw, ps = g.nc, g.fw, g.ps
    sl = l // 3
    nseq = g.NT // T
    w_in = g.w["ml_w_in"][sl]
    qkS, vS, sgS = g.qT_scr, g.v2_scr, g.sg_scr
    LNS = float(np.log(128.0 ** -0.5))
    with ExitStack() as es:
        st1 = sbt(g, es, "mST", [128, 3080], F32)
        CAUS = sbt(g, es, "mCAUS", [128, 4, 512], BF16)
        fw.dma("sp", lambda e: e.dma_start(out=st1[:, 0:2048], in_=g.nsac[:, 2048:4096]), writes=[K(st1)])
        fw.op("dve", lambda: nc.vector.tensor_copy(out=CAUS[:], in_=st1[:, 0:2048].rearrange("p (a b) -> p a b", a=4)),
              reads=[K(st1)], writes=[K(CAUS)])
        CW = sbt(g, es, "mCW", [128, 8, 4], F32)
        CB = sbt(g, es, "mCB", [128, 8], F32)
        GBI = sbt(g, es, "mGBI", [4, 1], F32)
        GBF = sbt(g, es, "mGBF", [4, 1], F32)
        NG = sbt(g, es, "mNG", [128, D], F32)
        ONESB = sbt(g, es, "mONES", [128, 1], BF16)
        SELH = sbt(g, es, "mSELH", [4, 4, 128], F32)
        cT = sbt(g, es, "mcT", [128, TT, 4], F32)
        emmT = sbt(g, es, "memmT", [128, TT, 4], F32)
        NA = sbt(g, es, "mNA", [4, T], F32)
        for j in range(4):
            fw.dma("sp", lambda e: e.dma_start(out=CW[:, :, j], in_=g.w["ml_conv_w"][sl, j, :].rearrange("(c p) -> p c", p=128),
                                               allow_slow_non_contiguous=True), writes=[K(CW, j)])
        fw.dma("sp", lambda e: e.dma_start(out=CB[:], in_=g.w["ml_conv_b"][sl].rearrange("(c p) -> p c", p=128),
                                           allow_slow_non_contiguous=True), writes=[K(CB)])
        fw.dma("sp", lambda e: e.dma_start(out=GBI[:], in_=g.w["ml_gate_b"][sl, 0:4].rearrange("(h o) -> h o", o=1),
                                           allow_slow_non_contiguous=True), writes=[K(GBI)])
        fw.dma("sp", lambda e: e.dma_start(out=GBF[:], in_=g.w["ml_gate_b"][sl, 4:8].rearrange("(h o) -> h o", o=1),
                                           allow_slow_non_contiguous=True), writes=[K(GBF)])
        fw.dma("sp", lambda e: e.dma_start(out=NG[:], in_=g.w["ml_norm_g"][sl, :].partition_broadcast(128)), writes=[K(NG)])
        fw.op("dve", lambda: nc.vector.memset(ONESB[:], 1.0), writes=[K(ONESB)])
        fw.op("dve", lambda: nc.vector.tensor_copy(out=SELH[:], in_=g.ident[0:4, 0:4].unsqueeze(2).to_broadcast([4, 4, 128])),
              reads=[K(g.ident)], writes=[K(SELH)])
        fw.op("dve", lambda: nc.vector.tensor_scalar_mul(out=GBF[:], in0=GBF[:], scalar1=-1.0), reads=[K(GBF)], writes=[K(GBF)])
        WO, lng, lnb, tb_bufs = load_tail_weights(g, es, l, g.w["ml_w_out"][sl], [st1, st1])

        for s in range(nseq):
            row0 = s * T
            with ExitStack() as esA:
                WI = sbt(g, esA, "mWI", [128, 8, 3080], BF16)
                XT = sbt(g, esA, "mXT", [128, 8, T], BF16)
                PRE = sbt(g, esA, "mPRE", [128, T + 3], F32)
                ACC = sbt(g, esA, "mACC", [128, T], F32)
                QKB = sbt(g, esA, "mQKB", [128, T], BF16)
                EV = [sbt(g, esA, f"mEV{i}", [128, 512], BF16) for i in range(3)]
                R0 = sbt(g, esA, "mR0", [4, T], F32)
                R1 = sbt(g, esA, "mR1", [4, T], F32)
                R2 = RowView(ACC, T)
                R3 = RowView(PRE, T)
                load_w_bf16(g, None, w_in, 3080, [st1, st1],
                            lambda kc, st: (fw.op("act", lambda: nc.scalar.copy(out=WI[:, kc, 0:1540], in_=st[:, 0:1540]),
                                                  reads=[K(st)], writes=[K(WI, (kc, 0))]),
                                            fw.op("dve", lambda: nc.vector.tensor_copy(out=WI[:, kc, 1540:3080], in_=st[:, 1540:3080]),
                                                  reads=[K(st)], writes=[K(WI, (kc, 1))])))
                build_xT(g, esA, X, Xname, row0, XT, tb_bufs["xt"])
                fw.op("dve", lambda: nc.vector.memset(PRE[:, 0:3], 0.0), writes=[K(PRE, "pad")])
                np_ = [0]
                for c in range(8):
                    for tb in range(4):
                        pS = ps[2 + np_[0] % 2]
                        np_[0] += 1
                        for kc in range(8):
                            fw.op("pe", lambda: nc.tensor.matmul(out=pS[:, :], lhsT=WI[:, kc, c * 128:(c + 1) * 128],
                                                                 rhs=XT[:, kc, tb * 512:(tb + 1) * 512], start=(kc == 0), stop=(kc == 7)),
                                  reads=[K(WI), K(XT)], writes=[K(pS)], inc=(kc == 7))
                        fw.op("act", lambda: nc.scalar.copy(out=PRE[:, 3 + tb * 512:3 + (tb + 1) * 512], in_=pS[:, :]),
                              reads=[K(pS)], writes=[K(PRE, tb)])
                    fw.op("dve", lambda: nc.vector.tensor_scalar(out=ACC[:], in0=PRE[:, 3:T + 3], scalar1=CW[:, c, 3:4], scalar2=None,
                                                                 op0=ALU.mult), reads=[K(PRE), K(CW)], writes=[K(ACC)])
                    for j in range(3):
                        eng = ("dve", nc.vector)
                        fw.op(eng[0], lambda: eng[1].scalar_tensor_tensor(out=ACC[:], in0=PRE[:, j:T + j], scalar=CW[:, c, j:j + 1],
                                                                          in1=ACC[:], op0=ALU.mult, op1=ALU.add),
                              reads=[K(PRE), K(CW), K(ACC)], writes=[K(ACC)])
                    fw.op("act", lambda: nc.scalar.activation(out=QKB[:], in_=ACC[:], func=AF.Silu, bias=CB[:, c:c + 1]),
                          reads=[K(ACC), K(CB)], writes=[K(QKB)])
                    fw.dma("sp", lambda e: e.dma_start(out=qkS[c * 128:(c + 1) * 128, :], in_=QKB[:]), reads=[K(QKB)], writes=[K("qkS", c)])
                nev = [0]

                def evac_store(pS, dst_ap, dkey, sig):
                    E = EV[nev[0] % 3]
                    nev[0] += 1
                    if sig:
                        fw.op("act", lambda: nc.scalar.activation(out=E[:], in_=pS[:, :], func=AF.Sigmoid), reads=[K(pS)], writes=[K(E)])
                    else:
                        fw.op("dve", lambda: nc.vector.tensor_copy(out=E[:], in_=pS[:, :]), reads=[K(pS)], writes=[K(E)])
                    fw.dma("sp", lambda e: e.dma_start(out=dst_ap, in_=E[:]), reads=[K(E)], writes=[dkey])

                for t in range(TT):
                    for (c0, dstS, nm, sig) in ((1024, vS, "vS2", False), (2048, sgS, "sgS", True)):
                        for nh in range(2):
                            pS = ps[2 + np_[0] % 2]
                            np_[0] += 1
                            for kc in range(8):
                                fw.op("pe", lambda: nc.tensor.matmul(out=pS[:, :], lhsT=XT[:, kc, t * 128:(t + 1) * 128],
                                                                     rhs=WI[:, kc, c0 + nh * 512:c0 + (nh + 1) * 512],
                                                                     start=(kc == 0), stop=(kc == 7)),
                                      reads=[K(XT), K(WI)], writes=[K(pS)], inc=(kc == 7))
                            evac_store(pS, dstS[t * 128:(t + 1) * 128, nh * 512:(nh + 1) * 512], K(nm, (t, nh)), sig)
                for (c0, R) in ((3072, R0), (3076, R1)):
                    for tb in range(4):
                        pS = ps[2 + np_[0] % 2]
                        np_[0] += 1
                        for kc in range(8):
                            fw.op("pe", lambda: nc.tensor.matmul(out=pS[0:4, :], lhsT=WI[:, kc, c0:c0 + 4],
                                                                 rhs=XT[:, kc, tb * 512:(tb + 1) * 512], start=(kc == 0), stop=(kc == 7)),
                                  reads=[K(WI), K(XT)], writes=[K(pS)], inc=(kc == 7))
                        if c0 == 3072:
                            fw.op("act", lambda: nc.scalar.activation(out=R[:, tb * 512:(tb + 1) * 512], in_=pS[0:4, :], func=AF.Identity,
                                                                      bias=GBI[:, 0:1]), reads=[K(pS), K(GBI)], writes=[K(R, tb)])
                        else:
                            fw.op("act", lambda: nc.scalar.activation(out=R[:, tb * 512:(tb + 1) * 512], in_=pS[0:4, :], func=AF.Exp,
                                                                      bias=GBF[:, 0:1], scale=-1.0), reads=[K(pS), K(GBF)], writes=[K(R, tb)])
                fw.op("act", lambda: nc.scalar.activation(out=R1[:], in_=R1[:], func=AF.Ln, bias=1.0), reads=[K(R1)], writes=[K(R1)])
                fw.op("dve", lambda: nc.vector.tensor_scalar_mul(out=R1[:], in0=R1[:], scalar1=-1.0), reads=[K(R1)], writes=[K(R1)])
                Bt, free = row_scan(g, R1, R2, T, ALU.add)
                other = R3
                fw.op("dve", lambda: nc.vector.tensor_tensor(out=other[:], in0=R0[:], in1=Bt[:], op=ALU.subtract),
                      reads=[K(R0), K(Bt)], writes=[K(other)])
                Cm, free2 = row_scan(g, other, free, T, ALU.max)
                fw.op("dve", lambda: nc.vector.tensor_scalar(out=NA[:], in0=Cm[:], scalar1=0.0, scalar2=-1.0, op0=ALU.max, op1=ALU.mult),
                      reads=[K(Cm)], writes=[K(NA)])
                fw.op("dve", lambda: nc.vector.tensor_tensor(out=free2[:], in0=NA[:], in1=Bt[:], op=ALU.subtract),
                      reads=[K(NA), K(Bt)], writes=[K(free2)])
                fw.op("act", lambda: nc.scalar.activation(out=free2[:], in_=free2[:], func=AF.Exp), reads=[K(free2)], writes=[K(free2)])
                fw.op("dve", lambda: nc.vector.tensor_tensor(out=R0[:], in0=R0[:], in1=Bt[:], op=ALU.subtract),
                      reads=[K(R0), K(Bt)], writes=[K(R0)])
                fw.op("dve", lambda: nc.vector.tensor_scalar_add(out=R0[:], in0=R0[:], scalar1=LNS), reads=[K(R0)], writes=[K(R0)])
                for (src, dstT) in ((R0, cT), (free2, emmT)):
                    for t in range(TT):
                        fw.op("pe", lambda: nc.tensor.transpose(out=ps[4][:, t * 4:(t + 1) * 4], in_=src[0:4, t * 128:(t + 1) * 128],
                                                                identity=g.ident[0:4, 0:4]),
                              reads=[K(src), K(g.ident)], writes=[K(ps[4])], inc=(t == TT - 1))
                    fw.op("dve", lambda: nc.vector.tensor_copy(out=dstT[:].rearrange("p t h -> p (t h)"), in_=ps[4][:, 0:TT * 4]),
                          reads=[K(ps[4])], writes=[K(dstT)])
            fw.barrier()
            with ExitStack() as esB:
                QT = sbt(g, esB, "mQT", [128, 4, T], BF16)
                KT = sbt(g, esB, "mKT", [128, 4, T], BF16)
                V = sbt(g, esB, "mV", [128, TT, D], BF16)
                EF = [sbt(g, esB, f"mEF{i}", [128, 512], F32) for i in range(3)]
                PT = [sbt(g, esB, f"mPT{i}", [128, 512], BF16) for i in range(4)]
                HN = sbt(g, esB, "mHN", [128, 4, D], F32)
                DEN = sbt(g, esB, "mDEN", [128, 4], F32)
                SQ = sbt(g, esB, "mSQ", [128, D], F32)
                MS = sbt(g, esB, "mMS", [128, 4], F32)
                SGt = [sbt(g, esB, f"mSGt{i}", [128, D], BF16) for i in range(2)]
                OTK = [sbt(g, esB, f"mOTK{i}", [128, D], BF16) for i in range(2)]
                for h in range(4):
                    fw.dma("sp", lambda e: e.dma_start(out=QT[:, h, :], in_=qkS[h * 128:(h + 1) * 128, :]), reads=[K("qkS")], writes=[K(QT, h)])
                    fw.dma("sp", lambda e: e.dma_start(out=KT[:, h, :], in_=qkS[512 + h * 128:512 + (h + 1) * 128, :]),
                           reads=[K("qkS")], writes=[K(KT, h)])
                for t4 in range(4):
                    fw.dma("sp", lambda e: e.dma_start(out=V[:, t4 * 4:(t4 + 1) * 4, :],
                                                       in_=vS[t4 * 512:(t4 + 1) * 512, :].rearrange("(t p) d -> p t d", p=128)),
                           reads=[K("vS2")], writes=[K(V, t4)])
                cnt = {"s": 0, "e": 0, "p": 0}
                for tb in range(4):
                    for h in range(4):
                        fw.op("pe", lambda: nc.tensor.matmul(out=ps[4][:, :], lhsT=SELH[:, h, :], rhs=NA[:, tb * 512:(tb + 1) * 512],
                                                             start=True, stop=True), reads=[K(SELH), K(NA)], writes=[K(ps[4])])
                        for bk in (5, 6, 7):
                            fw.op("dve", lambda: nc.vector.memset(ps[bk][:, :], 0.0), writes=[K(ps[bk])])
                        def emit_S(kt, h=h, tb=tb):
                            pS_ = ps[1 + cnt["s"] % 3]
                            cnt["s"] += 1
                            cl = max(0, kt - tb * 4) * 128
                            fw.op("pe", lambda: nc.tensor.matmul(out=pS_[:, cl:512], lhsT=KT[:, h, kt * 128:(kt + 1) * 128],
                                                                 rhs=QT[:, h, tb * 512 + cl:(tb + 1) * 512], start=True, stop=True),
                                  reads=[K(KT), K(QT)], writes=[K(pS_)])
                            return pS_

                        pend = [emit_S(0), emit_S(1)]
                        for kt in range(tb * 4 + 4):
                            pS = pend.pop(0)
                            if kt + 2 < tb * 4 + 4:
                                pend.append(emit_S(kt + 2))
                            E = EF[cnt["e"] % 3]
                            cnt["e"] += 1
                            cl = max(0, kt - tb * 4) * 128
                            fw.op("act", lambda: nc.scalar.activation(out=E[:, cl:512], in_=ps[4][:, cl:512], func=AF.Exp, bias=cT[:, kt, h:h + 1]),
                                  reads=[K(ps[4]), K(cT)], writes=[K(E)])
                            P = PT[cnt["p"] % 4]
                            cnt["p"] += 1
                            fw.op("dve", lambda: nc.vector.tensor_tensor(out=P[:, cl:512], in0=pS[:, cl:512], in1=E[:, cl:512], op=ALU.mult),
                                  reads=[K(pS), K(E)], writes=[K(P)])
                            if kt >= tb * 4:
                                di = (tb * 512 - kt * 128 + 384) // 128
                                fw.op("pool", lambda: nc.gpsimd.tensor_tensor(out=P[:, cl:512], in0=P[:, cl:512], in1=CAUS[:, di, cl:512], op=ALU.mult),
                                      reads=[K(P), K(CAUS)], writes=[K(P)])
                            for tt in range(4):
                                if kt > tb * 4 + tt:
                                    continue
                                last = (kt == tb * 4 + tt)
                                pn = ps[5 + tt // 2]
                                c0 = (tt % 2) * 256
                                fw.op("pe", lambda: nc.tensor.matmul(out=pn[:, c0:c0 + 256], lhsT=P[:, tt * 128:(tt + 1) * 128],
                                                                     rhs=V[:, kt, h * 256:(h + 1) * 256], start=False, stop=last,
                                                                     skip_group_check=True),
                                      reads=[K(P), K(V)], writes=[K(pn, c0)])
                                fw.op("pe", lambda: nc.tensor.matmul(out=ps[7][:, tt:tt + 1], lhsT=P[:, tt * 128:(tt + 1) * 128],
                                                                     rhs=ONESB[:, 0:1], start=False, stop=last, skip_group_check=True),
                                      reads=[K(P), K(ONESB)], writes=[K(ps[7], tt)], inc=last)
                        fw.op("dve", lambda: nc.vector.tensor_scalar_mul(out=MS[:], in0=ps[7][:, 0:4], scalar1=-1.0),
                              reads=[K(ps[7])], writes=[K(MS)])
                        fw.op("dve", lambda: nc.vector.tensor_tensor(out=DEN[:], in0=ps[7][:, 0:4], in1=MS[:], op=ALU.max),
                              reads=[K(ps[7]), K(MS)], writes=[K(DEN)])
                        fw.op("dve", lambda: nc.vector.tensor_tensor(out=DEN[:], in0=DEN[:], in1=emmT[:, tb * 4:(tb + 1) * 4, h],
                                                                     op=ALU.max), reads=[K(DEN), K(emmT)], writes=[K(DEN)])
                        fw.op("dve", lambda: nc.vector.reciprocal(out=DEN[:], in_=DEN[:]), reads=[K(DEN)], writes=[K(DEN)])
                        for tt in range(4):
                            pn = ps[5 + tt // 2]
                            c0 = (tt % 2) * 256
                            fw.op("act", lambda: nc.scalar.activation(out=HN[:, tt, h * 256:(h + 1) * 256], in_=pn[:, c0:c0 + 256],
                                                                      func=AF.Copy, scale=DEN[:, tt:tt + 1]),
                                  reads=[K(pn), K(DEN)], writes=[K(HN, (tt, h))])
                    def pre(t):
                        fw.dma("sp", lambda e: e.dma_start(out=SGt[t % 2][:], in_=sgS[t * 128:(t + 1) * 128, :]), reads=[K("sgS")], writes=[K(SGt[t % 2])])
                        tail_load_x(g, X, Xname, row0, t, tb_bufs)

                    pre(tb * 4)
                    for tt in range(4):
                        t = tb * 4 + tt
                        sgt, ok = SGt[t % 2], OTK[t % 2]
                        if tt < 3:
                            pre(t + 1)
                        fw.op("pool", lambda: nc.gpsimd.tensor_tensor(out=SQ[:], in0=HN[:, tt, :], in1=HN[:, tt, :], op=ALU.mult),
                              reads=[K(HN)], writes=[K(SQ)])
                        fw.op("dve", lambda: nc.vector.tensor_reduce(out=MS[:], in_=SQ[:].rearrange("p (h d) -> p h d", h=4), axis=AX.X, op=ALU.add),
                              reads=[K(SQ)], writes=[K(MS)])
                        fw.op("dve", lambda: nc.vector.tensor_scalar(out=MS[:], in0=MS[:], scalar1=1.0 / 256, scalar2=1e-6, op0=ALU.mult, op1=ALU.add),
                              reads=[K(MS)], writes=[K(MS)])
                        fw.op("act", lambda: nc.scalar.sqrt(out=MS[:], in_=MS[:]), reads=[K(MS)], writes=[K(MS)])
                        fw.op("dve", lambda: nc.vector.reciprocal(out=MS[:], in_=MS[:]), reads=[K(MS)], writes=[K(MS)])
                        fw.op("dve", lambda: nc.vector.tensor_tensor(out=SQ[:].rearrange("p (h d) -> p h d", h=4),
                                                                     in0=HN[:, tt, :].rearrange("p (h d) -> p h d", h=4),
                                                                     in1=MS[:].unsqueeze(2).to_broadcast([128, 4, 256]), op=ALU.mult),
                              reads=[K(HN), K(MS)], writes=[K(SQ)])
                        fw.op("pool", lambda: nc.gpsimd.tensor_tensor(out=SQ[:], in0=SQ[:], in1=NG[:], op=ALU.mult),
                              reads=[K(SQ), K(NG)], writes=[K(SQ)])
                        fw.op("dve", lambda: nc.vector.tensor_tensor(out=ok[:], in0=SQ[:], in1=sgt[:], op=ALU.mult),
                              reads=[K(SQ), K(sgt)], writes=[K(ok)])
                        mixer_tail(g, esB, l, X, Xname, XO, XOname, row0, t, ok[:], WO, lng, lnb, tb_bufs, K(ok), xloaded=True)
            fw.barrier()


MIXERS[1] = phase_mlstm


HL = 64


def phase_hgrn(g, l, X, Xname, XO, XOname):
    nc, fw, ps = g.nc, g.fw, g.ps
    sl = l // 3
    nseq = g.NT // T
    w_in = g.w["hg_w_in"][sl]
    qdS = g.qT_scr
    kdS = g.kT_scr.rearrange("a c t -> (a c) t")
    vS, sgS, kkS = g.v2_scr, g.sg_scr, g.o_scr
    NCH = T // HL
    with ExitStack() as es:
        st1 = sbt(g, es, "hST", [128, 4096], F32)
        NG = sbt(g, es, "hNG", [128, D], F32)
        LBb = sbt(g, es, "hLBb", [128, D], F32)
        OMb = sbt(g, es, "hOMb", [128, D], F32)
        LBp = sbt(g, es, "hLBp", [128, 8], F32)
        OMp = sbt(g, es, "hOMp", [128, 8], F32)
        HLp = sbt(g, es, "hHLp", [128, 8, 4], F32)
        UM = sbt(g, es, "hUM", [128, 128], BF16)
        SUFU = sbt(g, es, "hSUFU", [128, 128], F32)
        EGE = sbt(g, es, "hEGE", [128, 8, NCH], F32)
        fw.dma("sp", lambda e: e.dma_start(out=NG[:], in_=g.w["hg_norm_g"][sl, :].partition_broadcast(128)), writes=[K(NG)])
        fw.op("dve", lambda: nc.vector.tensor_tensor(out=UM[:], in0=g.C[:, 0:128], in1=g.C[:, 128:256], op=ALU.add),
              reads=[K(g.C)], writes=[K(UM)])
        fw.op("dve", lambda: nc.vector.tensor_copy(out=SUFU[:], in_=g.C[:, 3 * 128 + NB_MAX + 8:3 * 128 + NB_MAX + 8 + 128]),
              reads=[K(g.C)], writes=[K(SUFU)])
        for r in range(4):
            fw.dma("sp", lambda e: e.dma_start(out=st1[:, r * 1024:(r + 1) * 1024], in_=g.w["hg_lower"][r, :].partition_broadcast(128)),
                   writes=[K(st1, r)])
            fw.dma("sp", lambda e: e.dma_start(out=HLp[:, :, r], in_=g.w["hg_lower"][r, :].rearrange("(c p) -> p c", p=128),
                                               allow_slow_non_contiguous=True), writes=[K(HLp, r)])
        fw.op("act", lambda: nc.scalar.activation(out=st1[:], in_=st1[:], func=AF.Exp), reads=[K(st1)], writes=[K(st1)])
        fw.op("act", lambda: nc.scalar.activation(out=HLp[:], in_=HLp[:], func=AF.Exp), reads=[K(HLp)], writes=[K(HLp)])
        e4 = st1[:].rearrange("p (r d) -> p r d", r=4)
        fw.op("dve", lambda: nc.vector.tensor_tensor(out=OMb[:], in0=e4[:, 0, :], in1=e4[:, 1, :], op=ALU.add), reads=[K(st1)], writes=[K(OMb)])
        fw.op("dve", lambda: nc.vector.tensor_tensor(out=OMb[:], in0=OMb[:], in1=e4[:, 2, :], op=ALU.add), reads=[K(st1), K(OMb)], writes=[K(OMb)])
        fw.op("dve", lambda: nc.vector.tensor_tensor(out=OMb[:], in0=OMb[:], in1=e4[:, 3, :], op=ALU.add), reads=[K(st1), K(OMb)], writes=[K(OMb)])
        fw.op("dve", lambda: nc.vector.reciprocal(out=OMb[:], in_=OMb[:]), reads=[K(OMb)], writes=[K(OMb)])
        fw.op("dve", lambda: nc.vector.tensor_copy(out=LBb[:], in_=e4[:, 1, :]), reads=[K(st1)], writes=[K(LBb)])
        for i in range(2, l + 1):
            fw.op("dve", lambda: nc.vector.tensor_tensor(out=LBb[:], in0=LBb[:], in1=e4[:, i, :], op=ALU.add), reads=[K(st1), K(LBb)], writes=[K(LBb)])
        fw.op("dve", lambda: nc.vector.tensor_tensor(out=LBb[:], in0=LBb[:], in1=OMb[:], op=ALU.mult), reads=[K(LBb), K(OMb)], writes=[K(LBb)])
        fw.op("dve", lambda: nc.vector.tensor_scalar(out=OMb[:], in0=LBb[:], scalar1=-1.0, scalar2=1.0, op0=ALU.mult, op1=ALU.add),
              reads=[K(LBb)], writes=[K(OMb)])
        fw.op("dve", lambda: nc.vector.tensor_reduce(out=OMp[:], in_=HLp[:], axis=AX.X, op=ALU.add), reads=[K(HLp)], writes=[K(OMp)])
        fw.op("dve", lambda: nc.vector.reciprocal(out=OMp[:], in_=OMp[:]), reads=[K(OMp)], writes=[K(OMp)])
        fw.op("dve", lambda: nc.vector.tensor_reduce(out=LBp[:], in_=HLp[:, :, 1:l + 1], axis=AX.X, op=ALU.add), reads=[K(HLp)], writes=[K(LBp)])
        fw.op("dve", lambda: nc.vector.tensor_tensor(out=LBp[:], in0=LBp[:], in1=OMp[:], op=ALU.mult), reads=[K(LBp), K(OMp)], writes=[K(LBp)])
        fw.op("dve", lambda: nc.vector.tensor_scalar(out=OMp[:], in0=LBp[:], scalar1=-1.0, scalar2=1.0, op0=ALU.mult, op1=ALU.add),
              reads=[K(LBp)], writes=[K(OMp)])
        WO, lng, lnb, tb_bufs = load_tail_weights(g, es, l, g.w["hg_w_out"][sl], [st1, st1])

        for s in range(nseq):
            row0 = s * T
            with ExitStack() as esA:
                WI = sbt(g, esA, "hWI", [128, 8, 4096], BF16)
                XT = sbt(g, esA, "hXT", [128, 8, T], BF16)
                FA = sbt(g, esA, "hFA", [128, T], F32)
                FB = sbt(g, esA, "hFB", [128, T], F32)
                FC = sbt(g, esA, "hFC", [128, T], F32)
                QB = sbt(g, esA, "hQB", [128, T], BF16)
                EV = [sbt(g, esA, f"hEV{i}", [128, 512], BF16) for i in range(3)]
                TM = [ColView(FA, D), ColView(FB, D), ColView(FC, D)]
                TMb = ColView(QB, D)
                load_w_bf16(g, None, w_in, 4096, [st1, st1],
                            lambda kc, st: (fw.op("act", lambda: nc.scalar.copy(out=WI[:, kc, 0:2048], in_=st[:, 0:2048]),
                                                  reads=[K(st)], writes=[K(WI, (kc, 0))]),
                                            fw.op("dve", lambda: nc.vector.tensor_copy(out=WI[:, kc, 2048:4096], in_=st[:, 2048:4096]),
                                                  reads=[K(st)], writes=[K(WI, (kc, 1))])))
                build_xT(g, esA, X, Xname, row0, XT, tb_bufs["xt"])
                np_ = [0]

                def proj_fm(c0, dst):
                    for tb in range(4):
                        pS = ps[2 + np_[0] % 2]
                        np_[0] += 1
                        for kc in range(8):
                            fw.op("pe", lambda: nc.tensor.matmul(out=pS[:, :], lhsT=WI[:, kc, c0:c0 + 128],
                                                                 rhs=XT[:, kc, tb * 512:(tb + 1) * 512], start=(kc == 0), stop=(kc == 7)),
                                  reads=[K(WI), K(XT)], writes=[K(pS)], inc=(kc == 7))
                        fw.op("act", lambda: nc.scalar.copy(out=dst[:, tb * 512:(tb + 1) * 512], in_=pS[:, :]),
                              reads=[K(pS)], writes=[K(dst, tb)])

                for h in range(8):
                    proj_fm(1024 + h * 128, FA)
                    fw.op("act", lambda: nc.scalar.activation(out=FA[:], in_=FA[:], func=AF.Sigmoid), reads=[K(FA)], writes=[K(FA)])
                    fw.op("dve", lambda: nc.vector.tensor_scalar(out=FA[:], in0=FA[:], scalar1=OMp[:, h:h + 1], scalar2=LBp[:, h:h + 1],
                                                                 op0=ALU.mult, op1=ALU.add), reads=[K(FA), K(OMp), K(LBp)], writes=[K(FA)])
                    fw.op("act", lambda: nc.scalar.activation(out=FB[:], in_=FA[:], func=AF.Ln), reads=[K(FA)], writes=[K(FB)])
                    src, dst = FB, FC
                    sft = 1
                    while sft < HL:
                        s3, d3 = (x_[:].rearrange("p (c j) -> p c j", j=HL) for x_ in (src, dst))
                        fw.op("dve", lambda: nc.vector.tensor_tensor(out=d3[:, :, sft:], in0=s3[:, :, sft:], in1=s3[:, :, :HL - sft], op=ALU.add),
                              reads=[K(src)], writes=[K(dst, "hi")])
                        fw.op("act", lambda: nc.scalar.copy(out=d3[:, :, :sft], in_=s3[:, :, :sft]), reads=[K(src)], writes=[K(dst, "lo")])
                        src, dst = dst, src
                        sft *= 2
                    Gt, tmp = src, dst
                    fw.op("act", lambda: nc.scalar.activation(out=EGE[:, h, :], in_=Gt[:].rearrange("p (c j) -> p c j", j=HL)[:, :, HL - 1],
                                                              func=AF.Exp), reads=[K(Gt)], writes=[K(EGE, h)])
                    fw.op("act", lambda: nc.scalar.activation(out=tmp[:], in_=Gt[:], func=AF.Exp, scale=-1.0), reads=[K(Gt)], writes=[K(tmp)])
                    fw.op("dve", lambda: nc.vector.tensor_scalar(out=FA[:], in0=FA[:], scalar1=-1.0, scalar2=1.0, op0=ALU.mult, op1=ALU.add),
                          reads=[K(FA)], writes=[K(FA)])
                    fw.op("dve", lambda: nc.vector.tensor_tensor(out=QB[:], in0=FA[:], in1=tmp[:], op=ALU.mult), reads=[K(FA), K(tmp)], writes=[K(QB)])
                    fw.dma("sp", lambda e: e.dma_start(out=kdS[h * 128:(h + 1) * 128, :], in_=QB[:]), reads=[K(QB)], writes=[K("kdS", h)])
                    fw.op("act", lambda: nc.scalar.activation(out=tmp[:], in_=Gt[:], func=AF.Exp), reads=[K(Gt)], writes=[K(tmp)])
                    proj_fm(h * 128, FA)
                    fw.op("act", lambda: nc.scalar.activation(out=FA[:], in_=FA[:], func=AF.Silu), reads=[K(FA)], writes=[K(FA)])
                    fw.op("dve", lambda: nc.vector.tensor_tensor(out=QB[:], in0=FA[:], in1=tmp[:], op=ALU.mult), reads=[K(FA), K(tmp)], writes=[K(QB)])
                    fw.dma("sp", lambda e: e.dma_start(out=qdS[h * 128:(h + 1) * 128, :], in_=QB[:]), reads=[K(QB)], writes=[K("qdS", h)])
                for t in range(TT):
                    for (c0, dstS, nm, sig) in ((2048, vS, "vS2", False), (3072, sgS, "sgS", True)):
                        for nh in range(2):
                            pS = ps[2 + np_[0] % 2]
                            np_[0] += 1
                            for kc in range(8):
                                fw.op("pe", lambda: nc.tensor.matmul(out=pS[:, :], lhsT=XT[:, kc, t * 128:(t + 1) * 128],
                                                                     rhs=WI[:, kc, c0 + nh * 512:c0 + (nh + 1) * 512],
                                                                     start=(kc == 0), stop=(kc == 7)),
                                      reads=[K(XT), K(WI)], writes=[K(pS)], inc=(kc == 7))
                            E = EV[np_[0] % 3]
                            if sig:
                                fw.op("act", lambda: nc.scalar.activation(out=E[:], in_=pS[:, :], func=AF.Sigmoid), reads=[K(pS)], writes=[K(E)])
                            else:
                                fw.op("dve", lambda: nc.vector.tensor_copy(out=E[:], in_=pS[:, :]), reads=[K(pS)], writes=[K(E)])
                            fw.dma("sp", lambda e: e.dma_start(out=dstS[t * 128:(t + 1) * 128, nh * 512:(nh + 1) * 512], in_=E[:]),
                                   reads=[K(E)], writes=[K(nm, (t, nh))])
                    fT, lfT, sfx = TM
                    for nh in range(2):
                        pS = ps[2 + np_[0] % 2]
                        np_[0] += 1
                        for kc in range(8):
                            fw.op("pe", lambda: nc.tensor.matmul(out=pS[:, :], lhsT=XT[:, kc, t * 128:(t + 1) * 128],
                                                                 rhs=WI[:, kc, 1024 + nh * 512:1024 + (nh + 1) * 512],
                                                                 start=(kc == 0), stop=(kc == 7)),
                                  reads=[K(XT), K(WI)], writes=[K(pS)], inc=(kc == 7))
                        fw.op("act", lambda: nc.scalar.activation(out=fT[:, nh * 512:(nh + 1) * 512], in_=pS[:, :], func=AF.Sigmoid),
                              reads=[K(pS)], writes=[K(fT, nh)])
                    fw.op("dve", lambda: nc.vector.tensor_tensor(out=fT[:], in0=fT[:], in1=OMb[:], op=ALU.mult), reads=[K(fT), K(OMb)], writes=[K(fT)])
                    fw.op("pool", lambda: nc.gpsimd.tensor_tensor(out=fT[:], in0=fT[:], in1=LBb[:], op=ALU.add), reads=[K(fT), K(LBb)], writes=[K(fT)])
                    fw.op("act", lambda: nc.scalar.activation(out=lfT[:], in_=fT[:], func=AF.Ln), reads=[K(fT)], writes=[K(lfT)])
                    for nh in range(2):
                        pS = ps[4 + nh]
                        fw.op("pe", lambda: nc.tensor.matmul(out=pS[:, :], lhsT=SUFU[:], rhs=lfT[:, nh * 512:(nh + 1) * 512], start=True, stop=True),
                              reads=[K(SUFU), K(lfT)], writes=[K(pS)])
                        fw.op("act", lambda: nc.scalar.activation(out=sfx[:, nh * 512:(nh + 1) * 512], in_=pS[:, :], func=AF.Exp),
                              reads=[K(pS)], writes=[K(sfx, nh)])
                    fw.op("dve", lambda: nc.vector.tensor_scalar(out=fT[:], in0=fT[:], scalar1=-1.0, scalar2=1.0, op0=ALU.mult, op1=ALU.add),
                          reads=[K(fT)], writes=[K(fT)])
                    fw.op("dve", lambda: nc.vector.tensor_tensor(out=TMb[:], in0=fT[:], in1=sfx[:], op=ALU.mult), reads=[K(fT), K(sfx)], writes=[K(TMb)])
                    fw.dma("sp", lambda e: e.dma_start(out=kkS[t * 128:(t + 1) * 128, :], in_=TMb[:]), reads=[K(TMb)], writes=[K("kkS", t)])
            fw.barrier()
            with ExitStack() as esB:
                S = sbt(g, esB, "hS", [128, 8, 128], F32)
                Sb = sbt(g, esB, "hSb", [128, 8, 128], BF16)
                QD = sbt(g, esB, "hQD", [128, 8, 512], BF16)
                KD = sbt(g, esB, "hKD", [128, 8, 512], BF16)
                V64 = sbt(g, esB, "hV64", [64, 8, D], BF16)
                KK64 = sbt(g, esB, "hKK64", [64, 8, D], BF16)
                AT = sbt(g, esB, "hAT", [64, 8, 64], BF16)
                HN = sbt(g, esB, "hHN", [64, D], F32)
                SQ = sbt(g, esB, "hSQ", [64, D], F32)
                MS = sbt(g, esB, "hMS", [64, 8], F32)
                SGt = [sbt(g, esB, f"hSGt{i}", [64, D], BF16) for i in range(2)]
                OTK = [sbt(g, esB, f"hOTK{i}", [64, D], BF16) for i in range(2)]
                fw.op("dve", lambda: nc.vector.memset(S[:], 0.0), writes=[K(S)])
                fw.op("dve", lambda: nc.vector.memset(Sb[:], 0.0), writes=[K(Sb)])
                for tb in range(4):
                    fw.dma("sp", lambda e: e.dma_start(out=QD[:], in_=qdS[:, tb * 512:(tb + 1) * 512].rearrange("(h k) t -> k h t", k=128)),
                           reads=[K("qdS")], writes=[K(QD)])
                    fw.dma("sp", lambda e: e.dma_start(out=KD[:], in_=kdS[:, tb * 512:(tb + 1) * 512].rearrange("(h k) t -> k h t", k=128)),
                           reads=[K("kdS")], writes=[K(KD)])
                    fw.dma("sp", lambda e: e.dma_start(out=V64[:], in_=vS[tb * 512:(tb + 1) * 512, :].rearrange("(c s) d -> s c d", s=HL)),
                           reads=[K("vS2")], writes=[K(V64)])
                    fw.dma("sp", lambda e: e.dma_start(out=KK64[:], in_=kkS[tb * 512:(tb + 1) * 512, :].rearrange("(c s) d -> s c d", s=HL)),
                           reads=[K("kkS")], writes=[K(KK64)])
                    for ci in range(8):
                        cg = tb * 8 + ci
                        cs = slice(ci * HL, (ci + 1) * HL)
                        fw.dma("sp", lambda e: e.dma_start(out=SGt[cg % 2][:], in_=sgS[cg * HL:(cg + 1) * HL, :]), reads=[K("sgS")], writes=[K(SGt[cg % 2])])
                        tail_load_x(g, X, Xname, row0, cg, tb_bufs, rows=HL)
                        for h in range(8):
                            fw.op("pe", lambda: nc.tensor.matmul(out=ps[2][0:64, h * 64:(h + 1) * 64], lhsT=KD[:, h, cs], rhs=QD[:, h, cs],
                                                                 start=True, stop=True), reads=[K(KD), K(QD)], writes=[K(ps[2])], inc=(h == 7))
                        fw.op("dve", lambda: nc.vector.tensor_tensor(out=AT[:], in0=ps[2][0:64, :].rearrange("p (h t) -> p h t", h=8),
                                                                     in1=UM[0:64, 0:64].unsqueeze(1).to_broadcast([64, 8, 64]), op=ALU.mult),
                              reads=[K(ps[2]), K(UM)], writes=[K(AT)])
                        for h in range(8):
                            po = ps[3 + h // 4]
                            c0 = (h % 4) * 128
                            fw.op("pe", lambda: nc.tensor.matmul(out=po[0:64, c0:c0 + 128], lhsT=AT[:, h, :], rhs=V64[:, ci, h * 128:(h + 1) * 128],
                                                                 start=True, stop=False), reads=[K(AT), K(V64)], writes=[K(po)], inc=False)
                            fw.op("pe", lambda: nc.tensor.matmul(out=po[0:64, c0:c0 + 128], lhsT=QD[:, h, cs], rhs=Sb[:, h, :],
                                                                 start=False, stop=True), reads=[K(QD), K(Sb)], writes=[K(po)], inc=(h % 4 == 3))
                        for hh in range(2):
                            fw.op("act", lambda: nc.scalar.copy(out=HN[:, hh * 512:(hh + 1) * 512], in_=ps[3 + hh][0:64, :]),
                                  reads=[K(ps[3 + hh])], writes=[K(HN, hh)])
                        for hh in range(2):
                            for h4 in range(4):
                                h = hh * 4 + h4
                                fw.op("pe", lambda: nc.tensor.matmul(out=ps[5][:, h4 * 128:(h4 + 1) * 128], lhsT=KK64[:, ci, h * 128:(h + 1) * 128],
                                                                     rhs=V64[:, ci, h * 128:(h + 1) * 128], start=True, stop=True),
                                      reads=[K(KK64), K(V64)], writes=[K(ps[5])], inc=(h4 == 3))
                            hs = slice(hh * 4, (hh + 1) * 4)
                            fw.op("dve", lambda: nc.vector.tensor_tensor(out=S[:, hs, :], in0=S[:, hs, :],
                                                                         in1=EGE[:, hs, cg:cg + 1].to_broadcast([128, 4, 128]), op=ALU.mult),
                                  reads=[K(S, hh), K(EGE)], writes=[K(S, hh)])
                            fw.op("dve", lambda: nc.vector.tensor_tensor(out=S[:, hs, :], in0=S[:, hs, :],
                                                                         in1=ps[5][:, :].rearrange("p (h v) -> p h v", h=4), op=ALU.add),
                                  reads=[K(S, hh), K(ps[5])], writes=[K(S, hh)])
                            fw.op("act", lambda: nc.scalar.copy(out=Sb[:, hs, :], in_=S[:, hs, :]), reads=[K(S, hh)], writes=[K(Sb, hh)])
                        sgt, ok = SGt[cg % 2], OTK[cg % 2]
                        fw.op("pool", lambda: nc.gpsimd.tensor_tensor(out=SQ[:], in0=HN[:], in1=HN[:], op=ALU.mult), reads=[K(HN)], writes=[K(SQ)])
                        fw.op("dve", lambda: nc.vector.tensor_reduce(out=MS[:], in_=SQ[:].rearrange("p (h d) -> p h d", h=8), axis=AX.X, op=ALU.add),
                              reads=[K(SQ)], writes=[K(MS)])
                        fw.op("dve", lambda: nc.vector.tensor_scalar(out=MS[:], in0=MS[:], scalar1=1.0 / 128, scalar2=1e-6, op0=ALU.mult, op1=ALU.add),
                              reads=[K(MS)], writes=[K(MS)])
                        fw.op("act", lambda: nc.scalar.sqrt(out=MS[:], in_=MS[:]), reads=[K(MS)], writes=[K(MS)])
                        fw.op("dve", lambda: nc.vector.reciprocal(out=MS[:], in_=MS[:]), reads=[K(MS)], writes=[K(MS)])
                        fw.op("dve", lambda: nc.vector.tensor_tensor(out=SQ[:].rearrange("p (h d) -> p h d", h=8),
                                                                     in0=HN[:].rearrange("p (h d) -> p h d", h=8),
                                                                     in1=MS[:].unsqueeze(2).to_broadcast([64, 8, 128]), op=ALU.mult),
                              reads=[K(HN), K(MS)], writes=[K(SQ)])
                        fw.op("pool", lambda: nc.gpsimd.tensor_tensor(out=SQ[:], in0=SQ[:], in1=NG[0:64, :], op=ALU.mult),
                              reads=[K(SQ), K(NG)], writes=[K(SQ)])
                        fw.op("dve", lambda: nc.vector.tensor_tensor(out=ok[:], in0=SQ[:], in1=sgt[:], op=ALU.mult),
                              reads=[K(SQ), K(sgt)], writes=[K(ok)])
                        mixer_tail(g, esB, l, X, Xname, XO, XOname, row0, cg, ok[:], WO, lng, lnb, tb_bufs, K(ok), rows=HL, xloaded=True)
            fw.barrier()


MIXERS[2] = phase_hgrn


N_CORES = 8
MOE_B = 512
W_NAMES = [n for n in WEIGHT_SHAPES]


def full_plan():
    plan = []
    cur = "x"
    for l in range(4):
        plan.append(("mixer", l, cur, "xa"))
        dst = "out" if l == 3 else "xb"
        plan.append(("moe", l, "xa", dst))
        cur = dst
    return plan


def run_forward(inputs, n_cores=N_CORES, seq_per_core=4):
    x = np.ascontiguousarray(np.asarray(inputs["x"], dtype=np.float32))
    NT = seq_per_core * T
    nc, g = build(NT, MOE_B, full_plan(), W_NAMES)
    cst = make_consts(MOE_B)
    nsac = make_nsa_consts()
    weights = {n: np.ascontiguousarray(np.asarray(inputs[n], dtype=np.float32)) for n in W_NAMES}
    in_maps = []
    for c in range(n_cores):
        m = dict(weights)
        m["x"] = x[c * seq_per_core:(c + 1) * seq_per_core].reshape(NT, D)
        m["cst"] = cst
        m["nsac"] = nsac
        in_maps.append(m)
    res = run_bass_kernel_spmd(nc, in_maps, core_ids=list(range(n_cores)))
    outs = [np.asarray(r["out"]).reshape(seq_per_core, T, D) for r in res.results]
    return np.concatenate(outs, axis=0).astype(np.float32)


def kernel(**inputs):
    return run_forward(inputs)
```

```python
from contextlib import ExitStack
import numpy as np
import concourse.bass as bass
import concourse.mybir as mybir
from concourse.bass_utils import run_bass_kernel_spmd

F32 = mybir.dt.float32
BF16 = mybir.dt.bfloat16
I32 = mybir.dt.int32
AF = mybir.ActivationFunctionType
ALU = mybir.AluOpType
AX = mybir.AxisListType

COMPUTE = ("pe", "dve", "act", "pool")
EPOCH = 24000
DMA_SLOTS = 12


class FW:
    def __init__(self, nc, es):
        self.nc = nc
        self.es = es
        self.e = {"pe": nc.tensor, "dve": nc.vector, "act": nc.scalar, "pool": nc.gpsimd, "sp": nc.sync}
        self.csem = {k: [] for k in COMPUTE}
        self.ccnt = {k: 0 for k in COMPUTE}
        self.dsem = {}
        self.dn = {}
        self.seen = {k: {} for k in self.e}
        self.state = {}
        self.nsem = 0
        self.ninst = 0
        self.persistent = set()
        for k in COMPUTE:
            self._new_epoch(k)

    def _sem(self, name):
        self.nsem += 1
        return self.es.enter_context(self.nc.semaphore(name))

    def _new_epoch(self, k):
        self.csem[k].append(self._sem(f"c_{k}_{len(self.csem[k])}"))
        self.ccnt[k] = 0

    def _deps(self, reads, writes):
        deps = []
        for key in reads:
            name, tag = key
            st = self.state.get(name)
            if not st:
                continue
            tags = list(st.keys()) if tag is None else [tag, None]
            for t in tags:
                s = st.get(t)
                if s and s[0] is not None:
                    deps.append(s[0])
        for key in writes:
            name, tag = key
            st = self.state.get(name)
            if not st:
                continue
            tags = list(st.keys()) if tag is None else [tag, None]
            for t in tags:
                s = st.get(t)
                if s:
                    if s[0] is not None:
                        deps.append(s[0])
                    deps.extend(s[1].values())
        return deps

    def _record(self, reads, writes, tok):
        for name, tag in reads:
            st = self.state.setdefault(name, {})
            s = st.setdefault(tag, [None, {}])
            s[1][tok[0]] = tok
        for name, tag in writes:
            st = self.state.setdefault(name, {})
            if tag is None:
                st.clear()
            st[tag] = [tok, {}]

    def _wait(self, stream, deps, skip_self=None):
        seen = self.seen[stream]
        need = {}
        for (semkey, val, sem) in deps:
            if skip_self is not None and semkey[0] == "c" and semkey[1] == skip_self:
                continue
            if seen.get(semkey, 0) >= val:
                continue
            if semkey[0] == "c":
                if any(k2[0] == "c" and k2[1] == semkey[1] and k2[2] > semkey[2] for k2 in seen):
                    continue
            if need.get(semkey, (0, None))[0] < val:
                need[semkey] = (val, sem)
        for semkey, (val, sem) in need.items():
            self.e[stream].wait_ge(sem, val)
            seen[semkey] = val
            self.ninst += 1

    def op(self, eng, emit, reads=(), writes=(), inc=True):
        deps = self._deps(reads, writes)
        self._wait(eng, deps, skip_self="pe" if eng == "pe" else None)
        ins = emit()
        self.ninst += 1
        ep = len(self.csem[eng]) - 1
        if inc:
            self.ccnt[eng] += 1
            ins.then_inc(self.csem[eng][ep], 1)
            tok = (("c", eng, ep), self.ccnt[eng], self.csem[eng][ep])
            if self.ccnt[eng] >= EPOCH:
                self._new_epoch(eng)
        else:
            tok = (("c", eng, ep), self.ccnt[eng] + 1, self.csem[eng][ep])
        self._record(reads, writes, tok)
        return ins

    def dma(self, q, emit, reads=(), writes=()):
        if q not in self.dsem:
            self.dsem[q] = [self._sem(f"d_{q}_{i}") for i in range(DMA_SLOTS)]
            self.dn[q] = 0
        i = self.dn[q]
        slot = i % DMA_SLOTS
        sem = self.dsem[q][slot]
        deps = self._deps(reads, writes)
        if i >= DMA_SLOTS:
            deps.append((("d", q, slot), 16 * (i // DMA_SLOTS), sem))
        self._wait(q, deps)
        ins = emit(self.e[q])
        ins.then_inc(sem, 16)
        self.ninst += 1
        self.dn[q] = i + 1
        tok = (("d", q, slot), 16 * (i // DMA_SLOTS + 1), sem)
        self._record(reads, writes, tok)
        return ins

    def barrier(self):
        deps = []
        for k in COMPUTE:
            ep = len(self.csem[k]) - 1
            if self.ccnt[k] > 0:
                deps.append((("c", k, ep), self.ccnt[k], self.csem[k][ep]))
            elif ep > 0:
                deps.append((("c", k, ep - 1), EPOCH, self.csem[k][ep - 1]))
        for q, n in self.dn.items():
            for slot in range(min(n, DMA_SLOTS)):
                last = ((n - 1 - slot) // DMA_SLOTS) * DMA_SLOTS + slot
                deps.append((("d", q, slot), 16 * (last // DMA_SLOTS + 1), self.dsem[q][slot]))
        for stream in self.e:
            self._wait(stream, deps)
        self.state = {k: v for k, v in self.state.items() if k in self.persistent}

    def finish(self, out_names):
        deps = []
        for name in out_names:
            st = self.state.get(name, {})
            for s in st.values():
                if s[0] is not None:
                    deps.append(s[0])
        self._wait("sp", deps)


def K(t, tag=None):
    return (t if isinstance(t, str) else t.name, tag)


D = 1024
NE = 32
ALPHA = 8 ** 0.25
LN_EPS = 1e-5


def moe_nb(NT, B):
    return (4 * NT + NE * (B - 1) + B - 1) // B


class G:
    pass


_UID = [0]


def sbt(g, es, name, shape, dt):
    _UID[0] += 1
    return es.enter_context(g.nc.sbuf_tensor(f"{name}_{_UID[0]}", shape, dt))


def layer_norm_tile(g, xt, lng, lnb, out, tmpname, es, rows=128):
    nc, fw = g.nc, g.fw
    st = g.ln_st
    mv = g.ln_mv
    R = slice(0, rows)
    for c in range(2):
        fw.op("dve", lambda: nc.vector.bn_stats(out=st[R, c, :], in_=xt[R, c * 512:(c + 1) * 512]),
              reads=[K(xt)], writes=[K(st, c)])
    fw.op("dve", lambda: nc.vector.bn_aggr(out=mv[R, 0:2], in_=st[R]), reads=[K(st)], writes=[K(mv, 0)])
    fw.op("dve", lambda: nc.vector.tensor_scalar_add(out=mv[R, 3:4], in0=mv[R, 1:2], scalar1=LN_EPS),
          reads=[K(mv, 0)], writes=[K(mv, 2)])
    fw.op("act", lambda: nc.scalar.sqrt(out=mv[R, 3:4], in_=mv[R, 3:4]), reads=[K(mv, 2)], writes=[K(mv, 2)])
    fw.op("dve", lambda: nc.vector.reciprocal(out=mv[R, 2:3], in_=mv[R, 3:4]), reads=[K(mv, 2)], writes=[K(mv, 1)])
    fw.op("dve", lambda: nc.vector.tensor_scalar(out=xt[R], in0=xt[R], scalar1=mv[R, 0:1], scalar2=mv[R, 2:3],
                                                 op0=ALU.subtract, op1=ALU.mult),
          reads=[K(xt), K(mv, 0), K(mv, 1)], writes=[K(xt)])
    fw.op("pool", lambda: nc.gpsimd.tensor_tensor(out=xt[R], in0=xt[R], in1=lng[R], op=ALU.mult),
          reads=[K(xt), K(lng)], writes=[K(xt)])
    fw.op("pool", lambda: nc.gpsimd.tensor_tensor(out=out[R], in0=xt[R], in1=lnb[R], op=ALU.add),
          reads=[K(xt), K(lnb)], writes=[K(out)])


def phase_route(g, l, X1, X1name, keep):
    nc, fw, NT, B = g.nc, g.fw, g.NT, g.B
    NTT = NT // 128
    NB = moe_nb(NT, B)
    ps = g.ps
    POS4i, G4, EB = keep["POS4i"], keep["G4"], keep["EB"]
    with ExitStack() as es:
        L = sbt(g, es, "rL", [128, NTT, 32], F32)
        RW = sbt(g, es, "rW", [128, 8, 32], F32)
        RB = sbt(g, es, "rB", [128, 32], F32)
        fw.dma("sp", lambda e: e.dma_start(out=RW[:], in_=g.w["router_w"][l].rearrange("(kc p) e -> p kc e", p=128)),
               writes=[K(RW)])
        fw.dma("sp", lambda e: e.dma_start(out=RB[:], in_=g.w["router_b"][l].partition_broadcast(128)),
               writes=[K(RB)])
        xt = [sbt(g, es, f"rx{i}", [128, D], F32) for i in range(2)]
        xb = [sbt(g, es, f"rxb{i}", [128, D], BF16) for i in range(2)]
        xT = [sbt(g, es, f"rxT{i}", [128, 8, 128], F32) for i in range(2)]
        for t in range(NTT):
            i = t % 2
            fw.dma("sp", lambda e: e.dma_start(out=xt[i][:], in_=X1[t * 128:(t + 1) * 128, :]),
                   reads=[K(X1name, t)], writes=[K(xt[i])])
            fw.op("act", lambda: nc.scalar.copy(out=xb[i][:], in_=xt[i][:]), reads=[K(xt[i])], writes=[K(xb[i])])
            fw.dma("sp", lambda e: e.dma_start(out=g.x1b[t * 128:(t + 1) * 128, :], in_=xb[i][:]),
                   reads=[K(xb[i])], writes=[K("x1b", t)])
            for h in range(2):
                pst = ps[6 + h]
                for c in range(4):
                    kc = h * 4 + c
                    fw.op("pe", lambda: nc.tensor.transpose(out=pst[:, c * 128:(c + 1) * 128],
                                                            in_=xt[i][:, kc * 128:(kc + 1) * 128],
                                                            identity=g.ident[:]),
                          reads=[K(xt[i]), K(g.ident)], writes=[K(pst)], inc=(c == 3))
                fw.op("dve", lambda: nc.vector.tensor_copy(out=xT[i][:, h * 4:(h + 1) * 4, :],
                                                           in_=pst[:].rearrange("p (c n) -> p c n", c=4)),
                      reads=[K(pst)], writes=[K(xT[i], h)])
            for kc in range(8):
                fw.op("pe", lambda: nc.tensor.matmul(out=ps[2][:, 0:32], lhsT=xT[i][:, kc, :], rhs=RW[:, kc, :],
                                                     start=(kc == 0), stop=(kc == 7)),
                      reads=[K(xT[i]), K(RW)], writes=[K(ps[2])], inc=(kc == 7))
            fw.op("dve", lambda: nc.vector.tensor_tensor(out=L[:, t, :], in0=ps[2][:, 0:32], in1=RB[:], op=ALU.add),
                  reads=[K(ps[2]), K(RB)], writes=[K(L, t)])
        TOP = sbt(g, es, "rTOP", [128, NTT, 8], F32)
        SEL = sbt(g, es, "rSEL", [128, NTT, 32], F32)
        GT = sbt(g, es, "rGT", [128, NTT, 32], F32)
        CA = sbt(g, es, "rCA", [128, NTT, 32], F32)
        CB = sbt(g, es, "rCB", [128, NTT, 32], F32)
        Z = sbt(g, es, "rZ", [128, NTT], F32)
        SM = sbt(g, es, "rSM", [128, 8, 32], F32)
        SMb = sbt(g, es, "rSMb", [128, 32], BF16)
        for t in range(NTT):
            fw.op("dve", lambda: nc.vector.max(out=TOP[:, t, :], in_=L[:, t, :]), reads=[K(L, t)], writes=[K(TOP, t)])
        bshape = [128, NTT, 32]
        fw.op("dve", lambda: nc.vector.tensor_tensor(out=SEL[:], in0=L[:], in1=TOP[:, :, 3:4].to_broadcast(bshape),
                                                     op=ALU.is_ge), reads=[K(L), K(TOP)], writes=[K(SEL)])
        fw.op("dve", lambda: nc.vector.tensor_tensor(out=GT[:], in0=L[:], in1=TOP[:, :, 0:1].to_broadcast(bshape),
                                                     op=ALU.subtract), reads=[K(L), K(TOP)], writes=[K(GT)])
        fw.op("act", lambda: nc.scalar.activation(out=GT[:], in_=GT[:], func=AF.Exp), reads=[K(GT)], writes=[K(GT)])
        fw.op("dve", lambda: nc.vector.tensor_tensor(out=GT[:], in0=GT[:], in1=SEL[:], op=ALU.mult),
              reads=[K(GT), K(SEL)], writes=[K(GT)])
        fw.op("dve", lambda: nc.vector.tensor_reduce(out=Z[:], in_=GT[:], axis=AX.X, op=ALU.add),
              reads=[K(GT)], writes=[K(Z)])
        fw.op("dve", lambda: nc.vector.reciprocal(out=Z[:], in_=Z[:]), reads=[K(Z)], writes=[K(Z)])
        fw.op("dve", lambda: nc.vector.tensor_tensor(out=GT[:], in0=GT[:],
                                                     in1=Z[:].unsqueeze(2).to_broadcast(bshape), op=ALU.mult),
              reads=[K(GT), K(Z)], writes=[K(GT)])
        src, dst = SEL, CA
        s = 1
        while s < NTT:
            fw.op("dve", lambda: nc.vector.tensor_tensor(out=dst[:, s:, :], in0=src[:, s:, :], in1=src[:, :NTT - s, :],
                                                         op=ALU.add), reads=[K(src)], writes=[K(dst, "hi")])
            fw.op("dve", lambda: nc.vector.tensor_copy(out=dst[:, :s, :], in_=src[:, :s, :]),
                  reads=[K(src)], writes=[K(dst, "lo")])
            src = dst
            dst = CB if dst is CA else CA
            s *= 2
        INC = src
        EXC = dst
        fw.op("dve", lambda: nc.vector.tensor_copy(out=SMb[:], in_=INC[:, NTT - 1, :]), reads=[K(INC)], writes=[K(SMb)])
        fw.op("pe", lambda: nc.tensor.matmul(out=ps[3][:, 0:32], lhsT=g.trib[:], rhs=SMb[:], start=True, stop=True),
              reads=[K(g.trib), K(SMb)], writes=[K(ps[3], 0)])
        fw.op("pe", lambda: nc.tensor.matmul(out=ps[3][:, 32:64], lhsT=g.onesb[:], rhs=SMb[:], start=True, stop=True),
              reads=[K(g.onesb), K(SMb)], writes=[K(ps[3], 1)])
        PP, CNT, PAD, BASE, BEND, TMP = (SM[:, i, :] for i in range(6))
        fw.op("dve", lambda: nc.vector.tensor_copy(out=SM[:, 0:2, :], in_=ps[3][:, 0:64].rearrange("p (a e) -> p a e", a=2)),
              reads=[K(ps[3])], writes=[K(SM, 0), K(SM, 1)])
        fw.op("dve", lambda: nc.vector.tensor_scalar(out=TMP, in0=CNT, scalar1=1.0 / B, scalar2=(B - 1) / (2.0 * B),
                                                     op0=ALU.mult, op1=ALU.add), reads=[K(SM, 1)], writes=[K(SM, 5)])
        fw.op("dve", lambda: nc.vector.tensor_scalar_add(out=TMP, in0=TMP, scalar1=8388608.0),
              reads=[K(SM, 5)], writes=[K(SM, 5)])
        fw.op("dve", lambda: nc.vector.tensor_scalar(out=PAD, in0=TMP, scalar1=-8388608.0, scalar2=float(B),
                                                     op0=ALU.add, op1=ALU.mult), reads=[K(SM, 5)], writes=[K(SM, 2)])
        cur, oth = 2, 4
        s = 1
        while s < 32:
            fw.op("dve", lambda: nc.vector.tensor_tensor(out=SM[:, oth, s:], in0=SM[:, cur, s:], in1=SM[:, cur, :32 - s],
                                                         op=ALU.add), reads=[K(SM, cur)], writes=[K(SM, oth)])
            fw.op("dve", lambda: nc.vector.tensor_copy(out=SM[:, oth, :s], in_=SM[:, cur, :s]),
                  reads=[K(SM, cur)], writes=[K(SM, oth)])
            cur, oth = oth, (5 if oth == 4 else 4)
            s *= 2
        assert cur == 4
        fw.op("dve", lambda: nc.vector.tensor_tensor(out=BASE, in0=SM[:, 4, :], in1=PAD, op=ALU.subtract),
              reads=[K(SM, 4), K(SM, 2)], writes=[K(SM, 3)])
        CMP = sbt(g, es, "rCMP", [128, NB, 32], F32)
        EBf = sbt(g, es, "rEBf", [128, NB], F32)
        fw.op("dve", lambda: nc.vector.tensor_tensor(out=CMP[:], in0=SM[:, 4:5, :].to_broadcast([128, NB, 32]),
                                                     in1=g.iotaB[:, 0:NB].unsqueeze(2).to_broadcast([128, NB, 32]),
                                                     op=ALU.is_le), reads=[K(SM, 4), K(g.iotaB)], writes=[K(CMP)])
        fw.op("dve", lambda: nc.vector.tensor_reduce(out=EBf[:], in_=CMP[:], axis=AX.X, op=ALU.add),
              reads=[K(CMP)], writes=[K(EBf)])
        fw.op("dve", lambda: nc.vector.tensor_scalar_min(out=EBf[:], in0=EBf[:], scalar1=31.0),
              reads=[K(EBf)], writes=[K(EBf)])
        fw.op("dve", lambda: nc.vector.tensor_copy(out=EB[:], in_=EBf[:]), reads=[K(EBf)], writes=[K(EB)])
        IDXG, IDXB = keep["IDXG"], keep["IDXB"]
        IGf = sbt(g, es, "rIGf", [128, NB, 8], F32)
        fw.op("dve", lambda: nc.vector.tensor_scalar(out=EBf[:], in0=EBf[:], scalar1=float(l * NE), scalar2=None,
                                                     op0=ALU.add), reads=[K(EBf)], writes=[K(EBf)])
        fw.op("dve", lambda: nc.vector.tensor_copy(out=IDXB[:], in_=EBf[:]), reads=[K(EBf)], writes=[K(IDXB)])
        fw.op("dve", lambda: nc.vector.scalar_tensor_tensor(out=IGf[:], in0=EBf[:].unsqueeze(2).to_broadcast([128, NB, 8]),
                                                            scalar=1024.0, in1=g.kcp[:].unsqueeze(1).to_broadcast([128, NB, 8]),
                                                            op0=ALU.mult, op1=ALU.add),
              reads=[K(EBf), K(g.kcp)], writes=[K(IGf)])
        SKP = sbt(g, es, "rSKP", [128, NB], F32)
        fw.op("dve", lambda: nc.vector.memset(SKP[:, 0:2], 0.0), writes=[K(SKP, "a")])
        fw.op("dve", lambda: nc.vector.tensor_tensor(out=SKP[:, 2:NB], in0=EBf[:, 2:NB], in1=EBf[:, 0:NB - 2], op=ALU.is_equal),
              reads=[K(EBf)], writes=[K(SKP, "b")])
        fw.op("dve", lambda: nc.vector.scalar_tensor_tensor(out=IGf[:], in0=SKP[:].unsqueeze(2).to_broadcast([128, NB, 8]),
                                                            scalar=4194304.0, in1=IGf[:], op0=ALU.mult, op1=ALU.add),
              reads=[K(SKP), K(IGf)], writes=[K(IGf)])
        fw.op("dve", lambda: nc.vector.tensor_copy(out=IDXG[:], in_=IGf[:]), reads=[K(IGf)], writes=[K(IDXG)])
        fw.op("dve", lambda: nc.vector.tensor_tensor(out=SM[:, 5, :], in0=BASE, in1=PP, op=ALU.add),
              reads=[K(SM, 3), K(SM, 0)], writes=[K(SM, 5)])
        fw.op("dve", lambda: nc.vector.tensor_tensor(out=EXC[:], in0=INC[:], in1=SEL[:], op=ALU.subtract),
              reads=[K(INC), K(SEL)], writes=[K(EXC)])
        fw.op("dve", lambda: nc.vector.tensor_tensor(out=EXC[:], in0=EXC[:], in1=SM[:, 5:6, :].to_broadcast(bshape),
                                                     op=ALU.add), reads=[K(EXC), K(SM, 5)], writes=[K(EXC)])
        fw.op("dve", lambda: nc.vector.scalar_tensor_tensor(out=EXC[:], in0=EXC[:], scalar=1.0, in1=SEL[:],
                                                            op0=ALU.add, op1=ALU.mult),
              reads=[K(EXC), K(SEL)], writes=[K(EXC)])
        fw.op("dve", lambda: nc.vector.tensor_scalar_add(out=EXC[:], in0=EXC[:], scalar1=-1.0),
              reads=[K(EXC)], writes=[K(EXC)])
        POSM = EXC
        P8 = sbt(g, es, "rP8", [128, NTT, 8], F32)
        for t in range(NTT):
            fw.op("dve", lambda: nc.vector.max(out=P8[:, t, :], in_=POSM[:, t, :]), reads=[K(POSM)], writes=[K(P8, t)])
        fw.op("dve", lambda: nc.vector.tensor_copy(out=POS4i[:], in_=P8[:, :, 0:4]), reads=[K(P8)], writes=[K(POS4i)])
        EQ = INC
        for j in range(4):
            fw.op("dve", lambda: nc.vector.tensor_tensor(out=EQ[:], in0=POSM[:], in1=P8[:, :, j:j + 1].to_broadcast(bshape),
                                                         op=ALU.is_equal), reads=[K(POSM), K(P8)], writes=[K(EQ)])
            fw.op("dve", lambda: nc.vector.tensor_tensor(out=EQ[:], in0=EQ[:], in1=GT[:], op=ALU.mult),
                  reads=[K(EQ), K(GT)], writes=[K(EQ)])
            fw.op("dve", lambda: nc.vector.tensor_reduce(out=G4[:, :, j], in_=EQ[:], axis=AX.X, op=ALU.add),
                  reads=[K(EQ)], writes=[K(G4, j)])
        for t in range(NTT):
            i = t % 2
            fw.dma("sp", lambda e: e.dma_start(out=xb[i][:], in_=g.x1b[t * 128:(t + 1) * 128, :]),
                   reads=[K("x1b", t)], writes=[K(xb[i])])
            for j in range(4):
                fw.dma("pool", lambda e: e.indirect_dma_start(
                    out=g.xs[:, :], out_offset=bass.IndirectOffsetOnAxis(ap=POS4i[:, t, j:j + 1], axis=0),
                    in_=xb[i][:], in_offset=None), reads=[K(xb[i]), K(POS4i)], writes=[K("xs", ("s", t, j))])


def phase_experts(g, l, keep):
    nc, fw, NT, B = g.nc, g.fw, g.NT, g.B
    NB = moe_nb(NT, B)
    R = B // 128
    ps = g.ps
    w_gu, w_dn, b_gu, b_dn = g.w["moe_w_gu"], g.w["moe_w_down"], g.w["moe_b_gu"], g.w["moe_b_down"]
    with ExitStack() as es:
        WG = [sbt(g, es, f"eWG{i}", [128, 8, 2048], BF16) for i in range(2)]
        WD = [sbt(g, es, f"eWD{i}", [128, 8, 1024], BF16) for i in range(2)]
        BG = [sbt(g, es, f"eBG{i}", [128, 16], F32) for i in range(2)]
        BD = [sbt(g, es, f"eBD{i}", [128, 1024], F32) for i in range(2)]
        XS = [sbt(g, es, f"eXS{i}", [128, R, D], BF16) for i in range(2)]
        XT = [sbt(g, es, f"eXT{i}", [128, 8, B], BF16) for i in range(2)]
        HT = [sbt(g, es, f"eHT{i}", [128, 8, B], BF16) for i in range(2)]
        GC = [sbt(g, es, f"eGC{i}", [128, B], F32) for i in range(2)]
        SG = [sbt(g, es, f"eSG{i}", [128, B], F32) for i in range(2)]
        UC = [sbt(g, es, f"eUC{i}", [128, B], F32) for i in range(2)]
        Y = [sbt(g, es, f"eY{i}", [128, D], F32) for i in range(2)]
        BGR = sbt(g, es, "eBGR", [2, 2048], F32)
        wgu_rows = w_gu.rearrange("l e k f -> (l e k) f")
        wdn_rows = w_dn.rearrange("l e k f -> (l e k) f")
        bgu_rows = b_gu.rearrange("l e f -> (l e) f")
        bdn_rows = b_dn.rearrange("l e f -> (l e) f")
        IDXG, IDXB = keep["IDXG"], keep["IDXB"]

        def gather(out_ap, rows, idx_ap, reads, writes):
            fw.dma("pool", lambda e: e.indirect_dma_start(
                out=out_ap, out_offset=None, in_=rows[:, :],
                in_offset=bass.IndirectOffsetOnAxis(ap=idx_ap, axis=0)), reads=reads, writes=writes)

        nrows = wgu_rows.shape[0]
        _UID[0] += 1
        bc_reg = es.enter_context(nc.gpsimd.register(f"bc_reg{_UID[0]}"))
        nc.gpsimd.reg_mov(bc_reg, nrows - 1)
        bc_val = nc.gpsimd.snap(bc_reg)

        def wgather(out_ap, rows, idx_ap, writes):
            fw.dma("pool", lambda e: e.indirect_dma_start(
                out=out_ap, out_offset=None, in_=rows[:, :],
                in_offset=bass.IndirectOffsetOnAxis(ap=idx_ap, axis=0),
                bounds_check=bc_val, oob_is_err=False), reads=[K(IDXG)], writes=writes)

        def issue(b, k):
            i = b % 2
            if k < 8:
                wgather(WG[i][:, k, :], wgu_rows, IDXG[:, b, k:k + 1], [K(WG[i], k)])
            else:
                for a in range(2):
                    fc = 2 * (k - 8) + a
                    wgather(WD[i][:, fc, :], wdn_rows, IDXG[:, b, fc:fc + 1], [K(WD[i], k - 8)])

        def prefetch_slot(b, k):
            if b >= NB:
                return
            if k == 0:
                gather(BGR[:], bgu_rows, IDXB[0:2, b:b + 1], [K(IDXB)], [K(BGR)])
                gather(BD[b % 2][:], bdn_rows, IDXB[:, b:b + 1], [K(IDXB)], [K(BD[b % 2])])
            if k < 12:
                issue(b, k)

        def bias_transposes(b):
            i = b % 2
            for m in range(16):
                fw.op("pe", lambda: nc.tensor.transpose(out=ps[7][:, m:m + 1], in_=BGR[0:1, m * 128:(m + 1) * 128],
                                                        identity=g.ident[0:1, 0:1]),
                      reads=[K(BGR), K(g.ident)], writes=[K(ps[7])], inc=(m == 15))
            fw.op("dve", lambda: nc.vector.tensor_copy(out=BG[i][:], in_=ps[7][:, 0:16]),
                  reads=[K(ps[7])], writes=[K(BG[i])])

        def load_x(b):
            fw.dma("sp", lambda e: e.dma_start(out=XS[b % 2][:], in_=g.xs[b * B:(b + 1) * B, :].rearrange("(r p) d -> p r d", p=128)),
                   reads=[K("xs")], writes=[K(XS[b % 2])])

        def x_transposes(b):
            i = b % 2
            for kc in range(8):
                pst = ps[kc % 2]
                pv = pst[:] if kc % 2 == 0 else pst[:].bitcast(BF16)
                for r in range(R):
                    fw.op("pe", lambda: nc.tensor.transpose(out=pv[:, r * 128:(r + 1) * 128],
                                                            in_=XS[i][:, r, kc * 128:(kc + 1) * 128], identity=g.identb[:]),
                          reads=[K(XS[i]), K(g.identb)], writes=[K(pst)], inc=(r == R - 1))
                fw.op("act", lambda: nc.scalar.copy(out=XT[i][:, kc, :], in_=pv[:, 0:B]),
                      reads=[K(pst)], writes=[K(XT[i], kc)])

        for k in range(13):
            prefetch_slot(0, k)
        load_x(0)
        bias_transposes(0)
        for b in range(NB):
            i = b % 2
            if b + 1 < NB:
                load_x(b + 1)
            x_transposes(b)
            for m in range(8):
                prefetch_slot(b + 1, m)
                j = m % 2
                pg, pu = ps[2 + j], ps[4 + j]
                for (pp, col) in ((pg, m * 128), (pu, 1024 + m * 128)):
                    for kc in range(8):
                        fw.op("pe", lambda: nc.tensor.matmul(out=pp[:, 0:B], lhsT=WG[i][:, kc, col:col + 128],
                                                             rhs=XT[i][:, kc, :], start=(kc == 0), stop=(kc == 7)),
                              reads=[K(WG[i], kc), K(XT[i], kc)], writes=[K(pp)], inc=(kc == 7))
                fw.op("dve", lambda: nc.vector.tensor_scalar(out=GC[j][:], in0=pg[:, 0:B], scalar1=BG[i][:, m:m + 1],
                                                             scalar2=7.0, op0=ALU.add, op1=ALU.min),
                      reads=[K(pg), K(BG[i])], writes=[K(GC[j])])
                fw.op("act", lambda: nc.scalar.activation(out=SG[j][:], in_=GC[j][:], func=AF.Sigmoid, scale=1.702),
                      reads=[K(GC[j])], writes=[K(SG[j])])
                fw.op("act", lambda: nc.scalar.activation(out=UC[j][:], in_=pu[:, 0:B], func=AF.Identity, bias=BG[i][:, 8 + m:9 + m]),
                      reads=[K(pu), K(BG[i])], writes=[K(UC[j])])
                fw.op("dve", lambda: nc.vector.tensor_scalar(out=UC[j][:], in0=UC[j][:], scalar1=7.0, scalar2=-7.0,
                                                             op0=ALU.min, op1=ALU.max),
                      reads=[K(UC[j])], writes=[K(UC[j])])
                fw.op("dve", lambda: nc.vector.tensor_tensor(out=GC[j][:], in0=GC[j][:], in1=SG[j][:], op=ALU.mult),
                      reads=[K(GC[j]), K(SG[j])], writes=[K(GC[j])])
                fw.op("dve", lambda: nc.vector.scalar_tensor_tensor(out=HT[i][:, m, :], in0=UC[j][:], scalar=1.0, in1=GC[j][:],
                                                                    op0=ALU.add, op1=ALU.mult),
                      reads=[K(GC[j]), K(UC[j])], writes=[K(HT[i], m)])
            for r in range(R):
                prefetch_slot(b + 1, 8 + r)
                yb = Y[r % 2]
                for nh in range(2):
                    py = ps[6 + nh]
                    for fc in range(8):
                        fw.op("pe", lambda: nc.tensor.matmul(out=py[:, :], lhsT=HT[i][:, fc, r * 128:(r + 1) * 128],
                                                             rhs=WD[i][:, fc, nh * 512:(nh + 1) * 512],
                                                             start=(fc == 0), stop=(fc == 7)),
                              reads=[K(HT[i], fc), K(WD[i], fc // 2)], writes=[K(py)], inc=(fc == 7))
                    fw.op("dve", lambda: nc.vector.tensor_tensor(out=yb[:, nh * 512:(nh + 1) * 512], in0=py[:, :],
                                                                 in1=BD[i][:, nh * 512:(nh + 1) * 512], op=ALU.add),
                          reads=[K(py), K(BD[i])], writes=[K(yb, nh)])
                row0 = b * B + r * 128
                fw.dma("sp", lambda e: e.dma_start(out=g.ys[row0:row0 + 128, :], in_=yb[:]),
                       reads=[K(yb)], writes=[K("ys", ("b", b, r))])
            for k in range(8 + R, 13):
                prefetch_slot(b + 1, k)
            if b + 1 < NB:
                bias_transposes(b + 1)


def phase_combine(g, l, X1, X1name, XO, XOname, keep):
    nc, fw, NT = g.nc, g.fw, g.NT
    NTT = NT // 128
    POS4i, G4 = keep["POS4i"], keep["G4"]
    with ExitStack() as es:
        lng = sbt(g, es, "cLNG", [128, D], F32)
        lnb = sbt(g, es, "cLNB", [128, D], F32)
        fw.dma("sp", lambda e: e.dma_start(out=lng[:], in_=g.w["ln_g"][l, 1, :].partition_broadcast(128)), writes=[K(lng)])
        fw.dma("sp", lambda e: e.dma_start(out=lnb[:], in_=g.w["ln_b"][l, 1, :].partition_broadcast(128)), writes=[K(lnb)])
        xt = [sbt(g, es, f"cx{i}", [128, D], F32) for i in range(2)]
        yg = [[sbt(g, es, f"cy{i}_{j}", [128, D], F32) for j in range(4)] for i in range(2)]
        ot = [sbt(g, es, f"co{i}", [128, D], F32) for i in range(2)]
        def fetch(t):
            i = t % 2
            fw.dma("sp", lambda e: e.dma_start(out=xt[i][:], in_=X1[t * 128:(t + 1) * 128, :]),
                   reads=[K(X1name, t)], writes=[K(xt[i])])
            for j in range(4):
                fw.dma("pool", lambda e: e.indirect_dma_start(
                    out=yg[i][j][:], out_offset=None, in_=g.ys[:, :],
                    in_offset=bass.IndirectOffsetOnAxis(ap=POS4i[:, t, j:j + 1], axis=0)),
                    reads=[K("ys"), K(POS4i)], writes=[K(yg[i][j])])

        fetch(0)
        for t in range(NTT):
            i = t % 2
            if t + 1 < NTT:
                fetch(t + 1)
            fw.op("act", lambda: nc.scalar.mul(out=xt[i][:], in_=xt[i][:], mul=ALPHA), reads=[K(xt[i])], writes=[K(xt[i])])
            for j in range(4):
                fw.op("dve", lambda: nc.vector.scalar_tensor_tensor(out=xt[i][:], in0=yg[i][j][:], scalar=G4[:, t, j:j + 1],
                                                                    in1=xt[i][:], op0=ALU.mult, op1=ALU.add),
                      reads=[K(yg[i][j]), K(G4), K(xt[i])], writes=[K(xt[i])])
            layer_norm_tile(g, xt[i], lng, lnb, ot[i], None, es)
            fw.dma("sp", lambda e: e.dma_start(out=XO[t * 128:(t + 1) * 128, :], in_=ot[i][:]),
                   reads=[K(ot[i])], writes=[K(XOname, t)])


WEIGHT_SHAPES = {
    "ln_g": [4, 2, 1024], "ln_b": [4, 2, 1024],
    "nsa_w_in": [2, 1024, 2608], "nsa_cmp_pos": [2, 2, 32, 64], "nsa_cmp_w1": [2, 2, 2048, 256],
    "nsa_cmp_b1": [2, 2, 256], "nsa_cmp_w2": [2, 2, 256, 64], "nsa_cmp_b2": [2, 2, 64],
    "nsa_gate_b": [2, 48], "nsa_w_out": [2, 1024, 1024],
    "ml_w_in": [1, 1024, 3080], "ml_conv_w": [1, 4, 1024], "ml_conv_b": [1, 1024], "ml_gate_b": [1, 8],
    "ml_norm_g": [1, 1024], "ml_w_out": [1, 1024, 1024],
    "hg_w_in": [1, 1024, 4096], "hg_lower": [4, 1024], "hg_norm_g": [1, 1024], "hg_w_out": [1, 1024, 1024],
    "router_w": [4, 1024, 32], "router_b": [4, 32],
    "moe_w_gu": [4, 32, 1024, 2048], "moe_b_gu": [4, 32, 2048], "moe_w_down": [4, 32, 1024, 1024],
    "moe_b_down": [4, 32, 1024],
}
NB_MAX = 128


def make_consts(B):
    k_ = np.arange(128)
    c = np.zeros((128, 3 * 128 + NB_MAX + 8 + 128), np.float32)
    c[:, 3 * 128 + NB_MAX + 8:] = (k_[:, None] > k_[None, :]) & ((k_[:, None] // 64) == (k_[None, :] // 64))
    c[:, 0:128] = np.eye(128, dtype=np.float32)
    k = np.arange(128)
    c[:, 128:256] = (k[:, None] < k[None, :]).astype(np.float32)
    c[:, 256:384] = 1.0
    c[:, 384:384 + NB_MAX] = (np.arange(NB_MAX) * B)[None, :]
    c[:, 384 + NB_MAX:384 + NB_MAX + 8] = np.arange(8)[None, :] * 128 + k[:, None]
    return c


def build(NT, B, plan, wnames, wshapes=None):
    nc = bass.Bass("TRN2", target_bir_lowering=False)
    g = G()
    g.nc, g.NT, g.B = nc, NT, B
    NB = moe_nb(NT, B)
    wshapes = wshapes or WEIGHT_SHAPES
    g.w = {n: nc.dram_tensor(n, list(wshapes[n]), F32, kind="ExternalInput").ap() for n in wnames}
    xin = nc.dram_tensor("x", [NT, D], F32, kind="ExternalInput").ap()
    cst = nc.dram_tensor("cst", [128, 3 * 128 + NB_MAX + 8 + 128], F32, kind="ExternalInput").ap()
    out = nc.dram_tensor("out", [NT, D], F32, kind="ExternalOutput").ap()
    xa = nc.dram_tensor("xa", [NT, D], F32).ap()
    xbuf = nc.dram_tensor("xbuf", [NT, D], F32).ap()
    g.x1b = nc.dram_tensor("x1b", [NT, D], BF16).ap()
    g.xs = nc.dram_tensor("xs", [NB * B, D], BF16).ap()
    g.ys = nc.dram_tensor("ys", [NB * B, D], F32).ap()
    kinds = {ph[1] % 3 for ph in plan if ph[0] == "mixer"}
    if kinds:
        g.nsac = nc.dram_tensor("nsac", [128, NSAC_COLS], F32, kind="ExternalInput").ap()
        g.v2_scr = nc.dram_tensor("v2_scr", [T, 1024], BF16).ap()
        g.sg_scr = nc.dram_tensor("sg_scr", [T, 1024], BF16).ap()
        g.qT_scr = nc.dram_tensor("qT_scr", [1024, T], BF16).ap()
        g.kT_scr = nc.dram_tensor("kT_scr", [4, 256, T], BF16).ap()
        g.v_scr = nc.dram_tensor("v_scr", [T, 512], BF16).ap()
    g.o_scr = (nc.dram_tensor("o_scr", [T, 1024], BF16, kind="ExternalOutput") if DEBUG else nc.dram_tensor("o_scr", [T, 1024], BF16)).ap()
    with ExitStack() as es:
        fw = FW(nc, es)
        g.fw = fw
        g.ps = [es.enter_context(nc.psum_tensor("ps0", [128, 1024], BF16))]
        g.ps += [es.enter_context(nc.psum_tensor(f"ps{i}", [128, 512], F32)) for i in range(1, 8)]
        C = sbt(g, es, "cstf", [128, 3 * 128 + NB_MAX + 8 + 128], F32)
        g.C = C
        g.kcp = sbt(g, es, "kcp", [128, 8], F32)
        g.ident = sbt(g, es, "ident", [128, 128], F32)
        g.identb = sbt(g, es, "identb", [128, 128], BF16)
        g.trib = sbt(g, es, "trib", [128, 128], BF16)
        g.onesb = sbt(g, es, "onesb", [128, 128], BF16)
        g.iotaB = sbt(g, es, "iotaB", [128, NB_MAX], F32)
        g.ln_st = sbt(g, es, "ln_st", [128, 2, 6], F32)
        g.ln_mv = sbt(g, es, "ln_mv", [128, 4], F32)
        fw.dma("sp", lambda e: e.dma_start(out=C[:], in_=cst[:, :]), writes=[K(C)])
        fw.op("dve", lambda: nc.vector.tensor_copy(out=g.ident[:], in_=C[:, 0:128]), reads=[K(C)], writes=[K(g.ident)])
        fw.op("dve", lambda: nc.vector.tensor_copy(out=g.identb[:], in_=C[:, 0:128]), reads=[K(C)], writes=[K(g.identb)])
        fw.op("dve", lambda: nc.vector.tensor_copy(out=g.trib[:], in_=C[:, 128:256]), reads=[K(C)], writes=[K(g.trib)])
        fw.op("dve", lambda: nc.vector.tensor_copy(out=g.onesb[:], in_=C[:, 256:384]), reads=[K(C)], writes=[K(g.onesb)])
        fw.op("dve", lambda: nc.vector.tensor_copy(out=g.iotaB[:], in_=C[:, 384:384 + NB_MAX]), reads=[K(C)],
              writes=[K(g.iotaB)])
        fw.op("dve", lambda: nc.vector.tensor_copy(out=g.kcp[:], in_=C[:, 384 + NB_MAX:384 + NB_MAX + 8]), reads=[K(C)], writes=[K(g.kcp)])
        NTT = NT // 128
        keep = {"IDXG": sbt(g, es, "kIDXG", [128, NB, 8], I32), "IDXB": sbt(g, es, "kIDXB", [128, NB], I32),
                "POS4i": sbt(g, es, "kPOS", [128, NTT, 4], I32), "G4": sbt(g, es, "kG4", [128, NTT, 4], F32),
                "EB": sbt(g, es, "kEB", [128, NB], I32)}
        bufs = {"x": xin, "xa": xa, "xb": xbuf, "out": out}
        for ph in plan:
            kind = ph[0]
            if kind == "moe":
                _, l, src, dst = ph
                phase_route(g, l, bufs[src], src, keep)
                fw.barrier()
                phase_experts(g, l, keep)
                fw.barrier()
                phase_combine(g, l, bufs[src], src, bufs[dst], dst, keep)
                fw.barrier()
            elif kind == "mixer":
                _, l, src, dst = ph
                MIXERS[l % 3](g, l, bufs[src], src, bufs[dst], dst)
                fw.barrier()
        fw.finish(["out"])
        g.ninst = fw.ninst
    return nc, g


MIXERS = {}
DEBUG = False


T = 2048
TT = T // 128


def build_xT(g, es, X, Xname, row0, XT, xstage):
    nc, fw, ps = g.nc, g.fw, g.ps
    for t in range(TT):
        xt = xstage[t % 2]
        fw.dma("sp", lambda e: e.dma_start(out=xt[:], in_=X[row0 + t * 128:row0 + (t + 1) * 128, :]),
               reads=[K(Xname, (row0 // 128) + t)], writes=[K(xt)])
        for h in range(2):
            pst = ps[6 + h]
            for c in range(4):
                kc = h * 4 + c
                fw.op("pe", lambda: nc.tensor.transpose(out=pst[:, c * 128:(c + 1) * 128],
                                                        in_=xt[:, kc * 128:(kc + 1) * 128], identity=g.ident[:]),
                      reads=[K(xt), K(g.ident)], writes=[K(pst)], inc=(c == 3))
            eng = "dve" if h == 0 else "act"
            if h == 0:
                fw.op("dve", lambda: nc.vector.tensor_copy(out=XT[:, 0:4, t * 128:(t + 1) * 128],
                                                           in_=pst[:].rearrange("p (c n) -> p c n", c=4)),
                      reads=[K(pst)], writes=[K(XT, (t, 0))])
            else:
                fw.op("act", lambda: nc.scalar.copy(out=XT[:, 4:8, t * 128:(t + 1) * 128],
                                                    in_=pst[:].rearrange("p (c n) -> p c n", c=4)),
                      reads=[K(pst)], writes=[K(XT, (t, 1))])


def load_w_bf16(g, W, src, ncols, stage, col_ops):
    fw = g.fw
    for kc in range(8):
        st = stage[kc % 2]
        fw.dma("sp", lambda e: e.dma_start(out=st[:, 0:ncols], in_=src[kc * 128:(kc + 1) * 128, :]), writes=[K(st)])
        col_ops(kc, st)


def tail_load_x(g, X, Xname, row0, t, bufs, rows=128):
    xt = bufs["xt"][t % 2]
    r0 = row0 + t * rows
    g.fw.dma("sp", lambda e: e.dma_start(out=xt[0:rows, :], in_=X[r0:r0 + rows, :]), reads=[K(Xname, r0 // 128)], writes=[K(xt)])


def mixer_tail(g, es, l, X, Xname, XO, XOname, row0, t, OTOKt, WO, lng, lnb, bufs, okey, rows=128, xloaded=False):
    nc, fw, ps = g.nc, g.fw, g.ps
    OTt, xt, ot = bufs["OTt"][t % 2], bufs["xt"][t % 2], bufs["ot"][t % 2]
    pst = ps[0]
    r0 = row0 + t * rows
    xkey = K(Xname, r0 // 128)
    for kc in range(8):
        fw.op("pe", lambda: nc.tensor.transpose(out=pst[:, kc * 128:kc * 128 + rows], in_=OTOKt[:, kc * 128:(kc + 1) * 128],
                                                identity=g.identb[0:rows, 0:rows]),
              reads=[okey, K(g.identb)], writes=[K(pst)], inc=(kc == 7))
    fw.op("dve", lambda: nc.vector.tensor_copy(out=OTt[:, :, 0:rows], in_=pst[:].rearrange("p (c n) -> p c n", c=8)[:, :, 0:rows]),
          reads=[K(pst)], writes=[K(OTt)])
    if not xloaded:
        fw.dma("sp", lambda e: e.dma_start(out=xt[0:rows, :], in_=X[r0:r0 + rows, :]), reads=[xkey], writes=[K(xt)])
    for nh in range(2):
        py = ps[6 + nh]
        for kc in range(8):
            fw.op("pe", lambda: nc.tensor.matmul(out=py[0:rows, :], lhsT=OTt[:, kc, 0:rows], rhs=WO[:, kc, nh * 512:(nh + 1) * 512],
                                                 start=(kc == 0), stop=(kc == 7)),
                  reads=[K(OTt), K(WO)], writes=[K(py)], inc=(kc == 7))
        fw.op("dve", lambda: nc.vector.scalar_tensor_tensor(out=xt[0:rows, nh * 512:(nh + 1) * 512],
                                                            in0=xt[0:rows, nh * 512:(nh + 1) * 512], scalar=ALPHA, in1=py[0:rows, :],
                                                            op0=ALU.mult, op1=ALU.add),
              reads=[K(xt), K(py)], writes=[K(xt)])
    layer_norm_tile(g, xt, lng, lnb, ot, None, es, rows)
    fw.dma("sp", lambda e: e.dma_start(out=XO[r0:r0 + rows, :], in_=ot[0:rows, :]),
           reads=[K(ot)], writes=[K(XOname, r0 // 128)])


def load_tail_weights(g, es, l, w_out_ap, stage):
    nc, fw = g.nc, g.fw
    WO = sbt(g, es, "mWO", [128, 8, 1024], BF16)
    lng = sbt(g, es, "mLNG", [128, D], F32)
    lnb = sbt(g, es, "mLNB", [128, D], F32)
    fw.dma("sp", lambda e: e.dma_start(out=lng[:], in_=g.w["ln_g"][l, 0, :].partition_broadcast(128)), writes=[K(lng)])
    fw.dma("sp", lambda e: e.dma_start(out=lnb[:], in_=g.w["ln_b"][l, 0, :].partition_broadcast(128)), writes=[K(lnb)])
    load_w_bf16(g, WO, w_out_ap, 1024, stage,
                lambda kc, st: fw.op("act", lambda: nc.scalar.copy(out=WO[:, kc, :], in_=st[:, 0:1024]),
                                     reads=[K(st)], writes=[K(WO, kc)]))
    bufs = {"OTt": [sbt(g, es, f"mOTt{i}", [128, 8, 128], BF16) for i in range(2)],
            "xt": [sbt(g, es, f"mxt{i}", [128, D], F32) for i in range(2)],
            "ot": [sbt(g, es, "mot", [128, D], F32)] * 2}
    return WO, lng, lnb, bufs


NSAC_COLS = 2048 + 4096 + 2048 + 32 + 512 + 512


def make_nsa_consts():
    c = np.zeros((128, NSAC_COLS), np.float32)
    t = np.arange(T)
    cc = np.arange(128)
    c[:, 0:2048] = ((16 * cc[:, None] + 31) <= t[None, :]) & (cc[:, None] < 127)
    sl = np.arange(128)[:, None]
    tl = np.arange(512)[None, :]
    for di in range(8):
        delta = di * 128 - 384
        lag = delta + tl - sl
        c[:, 2048 + di * 512:2048 + (di + 1) * 512] = (lag >= 0) & (lag < 512)
    blk = np.arange(32)
    c[0:32, 6144:8192] = (t[None, :] // 64 == blk[:, None])
    cmp_start = np.arange(127) * 16
    sel_start = np.arange(32) * 64
    overlap = (np.minimum(cmp_start[:, None] + 32, sel_start[None, :] + 64) - np.maximum(cmp_start[:, None], sel_start[None, :]))
    c[0:127, 8192:8224] = np.clip(overlap, 0, None) / 16.0
    cur = (t // 64)[:, None]
    forced = (blk[None, :] == 0) | (blk[None, :] == cur) | (blk[None, :] == cur - 1)
    valid = blk[None, :] <= cur
    mul = (valid & ~forced).astype(np.float32)
    add = np.where(forced, 1e30, np.where(valid, 0.0, -1e30)).astype(np.float32)
    c[:, 8224:8736] = mul.reshape(16, 128, 32).transpose(1, 0, 2).reshape(128, 512)
    c[:, 8736:9248] = add.reshape(16, 128, 32).transpose(1, 0, 2).reshape(128, 512)
    return c


def phase_nsa(g, l, X, Xname, XO, XOname):
    nc, fw, ps = g.nc, g.fw, g.ps
    sl = l // 3
    nseq = g.NT // T
    w_in = g.w["nsa_w_in"][sl]
    qS, kS, vS, oS = g.qT_scr, g.kT_scr, g.v_scr, g.o_scr
    with ExitStack() as es:
        st1 = sbt(g, es, "nST", [128, 2608], F32)
        CMPM = sbt(g, es, "nCMPM", [128, 2048], BF16)
        WINM = sbt(g, es, "nWINM", [128, 8, 512], BF16)
        EXPB = sbt(g, es, "nEXPB", [32, 2048], BF16)
        MULA = sbt(g, es, "nMULA", [128, 2, 16, 32], F32)
        VCA = sbt(g, es, "nVCA", [128, 97], BF16)
        for (dst, key, c0, rows) in ((CMPM[:], K(CMPM), 0, 128), (WINM[:, 0:4, :], K(WINM, 0), 2048, 128),
                                     (WINM[:, 4:8, :], K(WINM, 1), 4096, 128), (EXPB[:], K(EXPB), 6144, 32)):
            fw.dma("sp", lambda e: e.dma_start(out=st1[0:rows, 0:2048], in_=g.nsac[0:rows, c0:c0 + 2048]), writes=[K(st1)])
            src = st1[0:rows, 0:2048] if len(dst.shape) == 2 else st1[:, 0:2048].rearrange("p (a b) -> p a b", a=4)
            fw.op("dve", lambda: nc.vector.tensor_copy(out=dst, in_=src), reads=[K(st1)], writes=[key])
        fw.dma("sp", lambda e: e.dma_start(out=st1[:, 0:1056], in_=g.nsac[:, 8192:9248]), writes=[K(st1)])
        fw.op("dve", lambda: nc.vector.tensor_copy(out=VCA[:, 65:97], in_=st1[:, 0:32]), reads=[K(st1)], writes=[K(VCA, "c")])
        fw.op("dve", lambda: nc.vector.tensor_copy(out=MULA[:].rearrange("p a t b -> p (a t b)"), in_=st1[:, 32:1056]),
              reads=[K(st1)], writes=[K(MULA)])
        fw.op("dve", lambda: nc.vector.memset(VCA[:, 64:65], 1.0), writes=[K(VCA, "o")])
        W2 = sbt(g, es, "nW2", [128, 2, 2, 64], BF16)
        B1 = sbt(g, es, "nB1", [128, 2, 2], F32)
        B2K = sbt(g, es, "nB2K", [64, 1], F32)
        B2V = sbt(g, es, "nB2V", [128, 64], F32)
        POST = sbt(g, es, "nPOST", [64, 2, 32], BF16)
        CONSTH = sbt(g, es, "nCONSTH", [128, 2, 2], F32)
        GB = sbt(g, es, "nGB", [128, 48], F32)
        SGT = sbt(g, es, "nSGT", [128, TT, 48], F32)
        w1 = g.w["nsa_cmp_w1"][sl]
        w2 = g.w["nsa_cmp_w2"][sl]
        fw.dma("sp", lambda e: e.dma_start(out=st1[:, 0:256].rearrange("p (j c d) -> p j c d", j=2, c=2),
                                           in_=w2.rearrange("j (c p) d -> p j c d", p=128)), writes=[K(st1)])
        fw.op("dve", lambda: nc.vector.tensor_copy(out=W2[:], in_=st1[:, 0:256].rearrange("p (j c d) -> p j c d", j=2, c=2)),
              reads=[K(st1)], writes=[K(W2)])
        fw.dma("sp", lambda e: e.dma_start(out=B1[:], in_=g.w["nsa_cmp_b1"][sl].rearrange("j (c p) -> p j c", p=128),
                                           allow_slow_non_contiguous=True), writes=[K(B1)])
        fw.dma("sp", lambda e: e.dma_start(out=B2K[:, :], in_=g.w["nsa_cmp_b2"][sl, 0, :].rearrange("(d o) -> d o", o=1),
                                           allow_slow_non_contiguous=True), writes=[K(B2K)])
        fw.dma("sp", lambda e: e.dma_start(out=B2V[:], in_=g.w["nsa_cmp_b2"][sl, 1, :].partition_broadcast(128)), writes=[K(B2V)])
        fw.dma("sp", lambda e: e.dma_start(out=GB[:], in_=g.w["nsa_gate_b"][sl, :].partition_broadcast(128)), writes=[K(GB)])
        fw.dma("sp", lambda e: e.dma_start(out=st1[0:64, 0:64].rearrange("d (j p) -> d j p", j=2),
                                           in_=g.w["nsa_cmp_pos"][sl].rearrange("j p d -> d j p"),
                                           allow_slow_non_contiguous=True), writes=[K(st1)])
        fw.op("dve", lambda: nc.vector.tensor_copy(out=POST[:], in_=st1[0:64, 0:64].rearrange("d (j p) -> d j p", j=2)),
              reads=[K(st1)], writes=[K(POST)])
        WO, lng, lnb, tb_bufs = load_tail_weights(g, es, l, g.w["nsa_w_out"][sl], [st1, st1])
        first = [True]

        for s in range(nseq):
            row0 = s * T
            with ExitStack() as esA:
                WQ = sbt(g, esA, "nWQ", [128, 8, 1024], BF16)
                WK = sbt(g, esA, "nWK", [128, 8, 4, 256], BF16)
                WVg = sbt(g, esA, "nWVg", [128, 8, 4, 128], BF16)
                WG = sbt(g, esA, "nWG", [128, 8, 48], BF16)
                XT = sbt(g, esA, "nXT", [128, 8, T], BF16)
                EV = [sbt(g, esA, f"nEV{i}", [128, 512], BF16) for i in range(3)]
                kcols = (1024, 1280, 1536, 2048)

                def cast_in(kc, st):
                    fw.op("act", lambda: nc.scalar.mul(out=WQ[:, kc, :], in_=st[:, 0:1024], mul=0.125), reads=[K(st)], writes=[K(WQ, kc)])
                    for ty, c0 in enumerate(kcols):
                        if ty % 2 == 0:
                            fw.op("dve", lambda: nc.vector.tensor_copy(out=WK[:, kc, ty, :], in_=st[:, c0:c0 + 256]),
                                  reads=[K(st)], writes=[K(WK, (kc, ty))])
                        else:
                            fw.op("pool", lambda: nc.gpsimd.tensor_copy(out=WK[:, kc, ty, :], in_=st[:, c0:c0 + 256]),
                                  reads=[K(st)], writes=[K(WK, (kc, ty))])
                    fw.op("dve", lambda: nc.vector.tensor_copy(out=WVg[:, kc, :, 0:64], in_=st[:, 1792:2048].rearrange("p (g d) -> p g d", g=4)),
                          reads=[K(st)], writes=[K(WVg, (kc, 0))])
                    fw.op("pool", lambda: nc.gpsimd.tensor_copy(out=WVg[:, kc, :, 64:128], in_=st[:, 2304:2560].rearrange("p (g d) -> p g d", g=4)),
                          reads=[K(st)], writes=[K(WVg, (kc, 1))])
                    fw.op("dve", lambda: nc.vector.tensor_copy(out=WG[:, kc, :], in_=st[:, 2560:2608]), reads=[K(st)], writes=[K(WG, kc)])

                load_w_bf16(g, None, w_in, 2608, [st1, st1], cast_in)
                build_xT(g, esA, X, Xname, row0, XT, tb_bufs["xt"])
                nev = [0]

                def evac_store(pS, ncol, dst_ap, dkey):
                    E = EV[nev[0] % 3]
                    if nev[0] % 2 == 0:
                        fw.op("act", lambda: nc.scalar.copy(out=E[:, 0:ncol], in_=pS[:, 0:ncol]), reads=[K(pS)], writes=[K(E)])
                    else:
                        fw.op("dve", lambda: nc.vector.tensor_copy(out=E[:, 0:ncol], in_=pS[:, 0:ncol]), reads=[K(pS)], writes=[K(E)])
                    nev[0] += 1
                    fw.dma("sp", lambda e: e.dma_start(out=dst_ap, in_=E[:, 0:ncol]), reads=[K(E)], writes=[dkey])

                for t in range(TT):
                    for kc in range(8):
                        fw.op("pe", lambda: nc.tensor.matmul(out=ps[4][:, 0:48], lhsT=XT[:, kc, t * 128:(t + 1) * 128], rhs=WG[:, kc, :],
                                                             start=(kc == 0), stop=(kc == 7)),
                              reads=[K(XT), K(WG)], writes=[K(ps[4])], inc=(kc == 7))
                    fw.op("dve", lambda: nc.vector.tensor_tensor(out=SGT[:, t, :], in0=ps[4][:, 0:48], in1=GB[:], op=ALU.add),
                          reads=[K(ps[4]), K(GB)], writes=[K(SGT, t)])
                fw.op("act", lambda: nc.scalar.activation(out=SGT[:], in_=SGT[:], func=AF.Sigmoid), reads=[K(SGT)], writes=[K(SGT)])
                np_ = [0]
                for c in range(8):
                    for tb in range(4):
                        pS = ps[2 + np_[0] % 2]
                        np_[0] += 1
                        for kc in range(8):
                            fw.op("pe", lambda: nc.tensor.matmul(out=pS[:, :], lhsT=WQ[:, kc, c * 128:(c + 1) * 128],
                                                                 rhs=XT[:, kc, tb * 512:(tb + 1) * 512], start=(kc == 0), stop=(kc == 7)),
                                  reads=[K(WQ), K(XT)], writes=[K(pS)], inc=(kc == 7))
                        evac_store(pS, 512, qS[c * 128:(c + 1) * 128, tb * 512:(tb + 1) * 512], K("qS", (c, tb)))
                for ty in range(4):
                    for c in range(2):
                        for tb in range(4):
                            pS = ps[2 + np_[0] % 2]
                            np_[0] += 1
                            for kc in range(8):
                                fw.op("pe", lambda: nc.tensor.matmul(out=pS[:, :], lhsT=WK[:, kc, ty, c * 128:(c + 1) * 128],
                                                                     rhs=XT[:, kc, tb * 512:(tb + 1) * 512], start=(kc == 0), stop=(kc == 7)),
                                      reads=[K(WK), K(XT)], writes=[K(pS)], inc=(kc == 7))
                            evac_store(pS, 512, kS[ty, c * 128:(c + 1) * 128, tb * 512:(tb + 1) * 512], K("kS", (ty, c, tb)))
                for t in range(TT):
                    pS = ps[2 + np_[0] % 2]
                    np_[0] += 1
                    for kc in range(8):
                        fw.op("pe", lambda: nc.tensor.matmul(out=pS[:, :], lhsT=XT[:, kc, t * 128:(t + 1) * 128],
                                                             rhs=WVg[:, kc, :, :].rearrange("p g d -> p (g d)"), start=(kc == 0), stop=(kc == 7)),
                              reads=[K(XT), K(WVg)], writes=[K(pS)], inc=(kc == 7))
                    evac_store(pS, 512, vS[t * 128:(t + 1) * 128, :], K("vS", t))
            fw.barrier()
            with ExitStack() as esB:
                W1 = sbt(g, esB, "nW1", [64, 2, 32, 256], BF16)
                QTg = sbt(g, esB, "nQTg", [64, 4, T], BF16)
                KT4 = sbt(g, esB, "nKT4", [64, 4, T], BF16)
                VA = sbt(g, esB, "nVA", [128, TT, 2, 65], BF16)
                HID = sbt(g, esB, "nHID", [128, 2, 2, 128], BF16)
                HU = [sbt(g, esB, f"nHU{i}", [128, 128], F32) for i in range(3)]
                KCMPT = sbt(g, esB, "nKCMPT", [64, 128], BF16)
                OACC = sbt(g, esB, "nOACC", [128, 4, 4, 64], F32)
                OB = sbt(g, esB, "nOB", [128, 4, 256], BF16)
                EB_ = [sbt(g, esB, f"nE{i}", [128, 512], BF16) for i in range(4)]
                PT = [sbt(g, esB, f"nPT{i}", [128, 512], BF16) for i in range(4)]
                PTC = [sbt(g, esB, f"nPTC{i}", [128, 512], BF16) for i in range(4)]
                SELXM = [sbt(g, esB, f"nSELXM{i}", [128, 512], BF16) for i in range(3)]
                SELT = sbt(g, esB, "nSELT", [32, 512], BF16)
                RC = sbt(g, esB, "nRC", [128, 4, 97], F32)
                AC = sbt(g, esB, "nAC", [128, 16, 65], F32)
                ZR = sbt(g, esB, "nZR", [128, 16], F32)
                CO = sbt(g, esB, "nCO", [128, 16], F32)
                TMPA = sbt(g, esB, "nTMPA", [128, 16, 64], F32)
                IMP = sbt(g, esB, "nIMP", [128, 32], F32)
                TOP8 = sbt(g, esB, "nTOP8", [128, 8], F32)
                SELM = sbt(g, esB, "nSELM", [128, 32], BF16)
                OTK = [sbt(g, esB, f"nOTK{i}", [128, D], BF16) for i in range(2)]
                fw.op("pool", lambda: nc.gpsimd.memset(VA[:, :, :, 64:65], 1.0), writes=[K(VA, "ones")])
                for j in range(2):
                    for pq in range(4):
                        fw.dma("sp", lambda e: e.dma_start(
                            out=st1[0:64, 0:2048].rearrange("d (p h) -> d p h", p=8),
                            in_=w1[j, pq * 512:(pq + 1) * 512, :].rearrange("(p d) h -> d p h", d=64)), writes=[K(st1)])
                        fw.op("act", lambda: nc.scalar.copy(out=W1[:, j, pq * 8:(pq + 1) * 8, :],
                                                            in_=st1[0:64, 0:2048].rearrange("d (p h) -> d p h", p=8)),
                              reads=[K(st1)], writes=[K(W1, (j, pq))])
                if first[0]:
                    first[0] = False
                    for j in range(2):
                        for hc in range(2):
                            for p in range(32):
                                fw.op("pe", lambda: nc.tensor.matmul(out=ps[4][:, (j * 2 + hc):(j * 2 + hc) + 1],
                                                                     lhsT=W1[:, j, p, hc * 128:(hc + 1) * 128],
                                                                     rhs=POST[:, j, p:p + 1], start=(p == 0), stop=(p == 31)),
                                      reads=[K(W1), K(POST)], writes=[K(ps[4])], inc=(p == 31))
                    fw.op("dve", lambda: nc.vector.tensor_tensor(out=CONSTH[:].rearrange("p j c -> p (j c)"), in0=ps[4][:, 0:4],
                                                                 in1=B1[:].rearrange("p j c -> p (j c)"), op=ALU.add),
                          reads=[K(ps[4]), K(B1)], writes=[K(CONSTH)])
                cnt = {"s": 0, "s3": 0, "e": 0, "p": 0, "x": 0}

                def accum_evac(tb, gi, which):
                    for bk in range(3):
                        n = 6 if bk < 2 else 4
                        fw.op("dve", lambda: nc.vector.tensor_copy(out=AC[:, bk * 6:bk * 6 + n, :],
                                                                   in_=ps[5 + bk][:, 0:n * 65].rearrange("p (a c) -> p a c", c=65)),
                              reads=[K(ps[5 + bk])], writes=[K(AC, bk)])
                    fw.op("dve", lambda: nc.vector.tensor_scalar_max(out=ZR[:], in0=AC[:, :, 64], scalar1=1e-30), reads=[K(AC)], writes=[K(ZR)])
                    fw.op("dve", lambda: nc.vector.reciprocal(out=ZR[:], in_=ZR[:]), reads=[K(ZR)], writes=[K(ZR)])
                    gview = SGT[:, tb * 4:(tb + 1) * 4, gi * 12 + which:gi * 12 + 12:3]
                    fw.op("dve", lambda: nc.vector.tensor_tensor(out=CO[:].rearrange("p (t h) -> p t h", t=4),
                                                                 in0=ZR[:].rearrange("p (t h) -> p t h", t=4), in1=gview, op=ALU.mult),
                          reads=[K(ZR), K(SGT)], writes=[K(CO)])
                    fw.op("dve", lambda: nc.vector.tensor_tensor(out=TMPA[:], in0=AC[:, :, 0:64],
                                                                 in1=CO[:].unsqueeze(2).to_broadcast([128, 16, 64]), op=ALU.mult),
                          reads=[K(AC), K(CO)], writes=[K(TMPA)])
                    fw.op("pool", lambda: nc.gpsimd.tensor_tensor(out=OACC[:].rearrange("p t h d -> p (t h) d"),
                                                                  in0=OACC[:].rearrange("p t h d -> p (t h) d"), in1=TMPA[:], op=ALU.add),
                          reads=[K(OACC), K(TMPA)], writes=[K(OACC)])

                def acc_ap(h, tt):
                    a = tt * 4 + h
                    return ps[5 + a // 6], (a % 6) * 65

                def attend(tb, kts, kty, vslot, maskfn, which, gi, span):
                    for bk in range(3):
                        fw.op("dve", lambda: nc.vector.memset(ps[5 + bk][:, :], 0.0), writes=[K(ps[5 + bk])])
                    units = [(kt, h) for kt in kts for h in range(4)]

                    def cols(kt):
                        lo, hi = span(kt)
                        return lo * 128, (hi + 1) * 128

                    def emit_S(u):
                        kt, h = u
                        c_lo, c_hi = cols(kt)
                        pS = ps[1 + cnt["s3"] % 3]
                        cnt["s3"] += 1
                        fw.op("pe", lambda: nc.tensor.matmul(out=pS[:, c_lo:c_hi], lhsT=KT4[:, kty, kt * 128:(kt + 1) * 128],
                                                             rhs=QTg[:, h, tb * 512 + c_lo:tb * 512 + c_hi], start=True, stop=True),
                              reads=[K(KT4), K(QTg)], writes=[K(pS)])
                        return pS

                    pend = [emit_S(units[0])]
                    if len(units) > 1:
                        pend.append(emit_S(units[1]))
                    mk = None
                    masks = {}
                    for i, (kt, h) in enumerate(units):
                        pS = pend.pop(0)
                        if i + 2 < len(units):
                            pend.append(emit_S(units[i + 2]))
                        c_lo, c_hi = cols(kt)
                        if h == 0:
                            if kt not in masks:
                                masks[kt] = maskfn(kt, c_lo, c_hi)
                            mk = masks.pop(kt)
                        E = EB_[cnt["e"] % 4]
                        cnt["e"] += 1
                        fw.op("act", lambda: nc.scalar.activation(out=E[:, c_lo:c_hi], in_=pS[:, c_lo:c_hi], func=AF.Exp),
                              reads=[K(pS)], writes=[K(E)])
                        P = PT[cnt["p"] % 4]
                        cnt["p"] += 1
                        fw.op("dve", lambda: nc.vector.tensor_tensor(out=P[:, c_lo:c_hi], in0=E[:, c_lo:c_hi], in1=mk[0], op=ALU.mult),
                              reads=[K(E), mk[1]], writes=[K(P)])
                        if h == 1:
                            nk = kts.index(kt) + 1
                            if nk < len(kts):
                                masks[kts[nk]] = maskfn(kts[nk], *cols(kts[nk]))
                        lo, hi = span(kt)
                        for tt in range(lo, hi + 1):
                            pa, c0 = acc_ap(h, tt)
                            last = max(k2 for k2 in kts if span(k2)[0] <= tt <= span(k2)[1])
                            fw.op("pe", lambda: nc.tensor.matmul(out=pa[:, c0:c0 + 65], lhsT=P[:, tt * 128:(tt + 1) * 128],
                                                                 rhs=VA[:, kt, vslot, :], start=False, stop=(kt == last),
                                                                 skip_group_check=True),
                                  reads=[K(P), K(VA)], writes=[K(pa, c0)], inc=(kt == last))
                    accum_evac(tb, gi, which)

                for gi in range(4):
                    for h in range(4):
                        hg = gi * 4 + h
                        fw.dma("sp", lambda e: e.dma_start(out=QTg[:, h, :], in_=qS[hg * 64:(hg + 1) * 64, :]),
                               reads=[K("qS")], writes=[K(QTg, h)])
                    for ty in range(4):
                        fw.dma("sp", lambda e: e.dma_start(out=KT4[:, ty, :], in_=kS[ty, gi * 64:(gi + 1) * 64, :]),
                               reads=[K("kS")], writes=[K(KT4, ty)])
                    for a in range(2):
                        fw.dma("sp", lambda e: e.dma_start(
                            out=VA[:, :, a, 0:64],
                            in_=vS[:, gi * 128 + a * 64:gi * 128 + (a + 1) * 64].rearrange("(t p) d -> p t d", p=128)),
                            reads=[K("vS")], writes=[K(VA, ("v", a))])
                    for j in range(2):
                        for hc in range(2):
                            pS = ps[2 + (j * 2 + hc) % 2]
                            for p in range(32):
                                fw.op("pe", lambda: nc.tensor.matmul(out=pS[:, 0:127], lhsT=W1[:, j, p, hc * 128:(hc + 1) * 128],
                                                                     rhs=KT4[:, j, p:p + 16 * 126 + 1:16], start=(p == 0), stop=(p == 31)),
                                      reads=[K(W1), K(KT4, j)], writes=[K(pS)], inc=(p == 31))
                            u, u2, sg = HU
                            fw.op("act", lambda: nc.scalar.activation(out=u[:, 0:127], in_=pS[:, 0:127], func=AF.Identity,
                                                                      bias=CONSTH[:, j, hc:hc + 1]), reads=[K(pS), K(CONSTH)], writes=[K(u)])
                            fw.op("dve", lambda: nc.vector.tensor_tensor(out=u2[:, 0:127], in0=u[:, 0:127], in1=u[:, 0:127], op=ALU.mult),
                                  reads=[K(u)], writes=[K(u2)])
                            fw.op("dve", lambda: nc.vector.tensor_scalar(out=u2[:, 0:127], in0=u2[:, 0:127], scalar1=0.044715, scalar2=1.0,
                                                                         op0=ALU.mult, op1=ALU.add), reads=[K(u2)], writes=[K(u2)])
                            fw.op("dve", lambda: nc.vector.tensor_tensor(out=u2[:, 0:127], in0=u2[:, 0:127], in1=u[:, 0:127], op=ALU.mult),
                                  reads=[K(u2), K(u)], writes=[K(u2)])
                            fw.op("act", lambda: nc.scalar.activation(out=sg[:, 0:127], in_=u2[:, 0:127], func=AF.Sigmoid, scale=1.5957691216),
                                  reads=[K(u2)], writes=[K(sg)])
                            fw.op("dve", lambda: nc.vector.tensor_tensor(out=HID[:, j, hc, 0:127], in0=u[:, 0:127], in1=sg[:, 0:127], op=ALU.mult),
                                  reads=[K(u), K(sg)], writes=[K(HID, (j, hc))])
                    pS = ps[2]
                    for hc in range(2):
                        fw.op("pe", lambda: nc.tensor.matmul(out=pS[0:64, 0:127], lhsT=W2[:, 0, hc, :], rhs=HID[:, 0, hc, 0:127],
                                                             start=(hc == 0), stop=(hc == 1)),
                              reads=[K(W2), K(HID)], writes=[K(pS)], inc=(hc == 1))
                    fw.op("act", lambda: nc.scalar.activation(out=KCMPT[:, 0:127], in_=pS[0:64, 0:127], func=AF.Identity, bias=B2K[:, 0:1]),
                          reads=[K(pS), K(B2K)], writes=[K(KCMPT)])
                    pS = ps[3]
                    for hc in range(2):
                        fw.op("pe", lambda: nc.tensor.matmul(out=pS[0:127, 0:64], lhsT=HID[:, 1, hc, 0:127], rhs=W2[:, 1, hc, :],
                                                             start=(hc == 0), stop=(hc == 1)),
                              reads=[K(W2), K(HID)], writes=[K(pS)], inc=(hc == 1))
                    fw.op("dve", lambda: nc.vector.tensor_tensor(out=VCA[0:127, 0:64], in0=pS[0:127, 0:64], in1=B2V[0:127, :], op=ALU.add),
                          reads=[K(pS), K(B2V)], writes=[K(VCA, "v")])
                    for tb in range(4):
                        for h in range(4):
                            pS = ps[2 + cnt["s"] % 2]
                            cnt["s"] += 1
                            fw.op("pe", lambda: nc.tensor.matmul(out=pS[0:127, :], lhsT=KCMPT[:, 0:127],
                                                                 rhs=QTg[:, h, tb * 512:(tb + 1) * 512], start=True, stop=True),
                                  reads=[K(KCMPT), K(QTg)], writes=[K(pS)])
                            E = EB_[cnt["e"] % 3]
                            cnt["e"] += 1
                            fw.op("act", lambda: nc.scalar.activation(out=E[0:127, :], in_=pS[0:127, :], func=AF.Exp), reads=[K(pS)], writes=[K(E)])
                            fw.op("dve", lambda: nc.vector.tensor_tensor(out=PTC[h][0:127, :], in0=E[0:127, :],
                                                                         in1=CMPM[0:127, tb * 512:(tb + 1) * 512], op=ALU.mult),
                                  reads=[K(E), K(CMPM)], writes=[K(PTC[h])])
                        for tt in range(4):
                            tile = tb * 4 + tt
                            for h in range(4):
                                fw.op("pe", lambda: nc.tensor.matmul(out=ps[4][:, h * 97:(h + 1) * 97], lhsT=PTC[h][0:127, tt * 128:(tt + 1) * 128],
                                                                     rhs=VCA[0:127, :], start=True, stop=True),
                                      reads=[K(PTC[h]), K(VCA)], writes=[K(ps[4])], inc=(h == 3))
                            fw.op("dve", lambda: nc.vector.tensor_copy(out=RC[:], in_=ps[4][:, 0:388].rearrange("p (h c) -> p h c", h=4)),
                                  reads=[K(ps[4])], writes=[K(RC)])
                            fw.op("dve", lambda: nc.vector.tensor_scalar_max(out=ZR[:, 0:4], in0=RC[:, :, 64], scalar1=1e-30),
                                  reads=[K(RC)], writes=[K(ZR)])
                            fw.op("dve", lambda: nc.vector.reciprocal(out=ZR[:, 0:4], in_=ZR[:, 0:4]), reads=[K(ZR)], writes=[K(ZR)])
                            fw.op("dve", lambda: nc.vector.tensor_scalar(out=IMP[:], in0=RC[:, 0, 65:97], scalar1=ZR[:, 0:1], scalar2=None,
                                                                         op0=ALU.mult), reads=[K(RC), K(ZR)], writes=[K(IMP)])
                            for h in range(1, 4):
                                fw.op("dve", lambda: nc.vector.scalar_tensor_tensor(out=IMP[:], in0=RC[:, h, 65:97], scalar=ZR[:, h:h + 1],
                                                                                    in1=IMP[:], op0=ALU.mult, op1=ALU.add),
                                      reads=[K(RC), K(ZR), K(IMP)], writes=[K(IMP)])
                            fw.op("dve", lambda: nc.vector.tensor_tensor(out=IMP[:], in0=IMP[:], in1=MULA[:, 0, tile, :], op=ALU.mult),
                                  reads=[K(IMP), K(MULA)], writes=[K(IMP)])
                            fw.op("dve", lambda: nc.vector.tensor_tensor(out=IMP[:], in0=IMP[:], in1=MULA[:, 1, tile, :], op=ALU.add),
                                  reads=[K(IMP), K(MULA)], writes=[K(IMP)])
                            fw.op("dve", lambda: nc.vector.max(out=TOP8[:], in_=IMP[:]), reads=[K(IMP)], writes=[K(TOP8)])
                            fw.op("dve", lambda: nc.vector.tensor_scalar(out=SELM[:], in0=IMP[:], scalar1=TOP8[:, 7:8], scalar2=None,
                                                                         op0=ALU.is_ge), reads=[K(IMP), K(TOP8)], writes=[K(SELM)])
                            fw.op("pe", lambda: nc.tensor.transpose(out=ps[0][0:32, tt * 128:(tt + 1) * 128], in_=SELM[:], identity=g.identb[:]),
                                  reads=[K(SELM), K(g.identb)], writes=[K(ps[0])])
                            fw.op("dve", lambda: nc.vector.tensor_tensor(out=CO[:, 0:4], in0=ZR[:, 0:4], in1=SGT[:, tile, gi * 12:gi * 12 + 12:3],
                                                                         op=ALU.mult), reads=[K(ZR), K(SGT)], writes=[K(CO)])
                            fw.op("dve", lambda: nc.vector.tensor_tensor(out=OACC[:, tt, :, :], in0=RC[:, :, 0:64],
                                                                         in1=CO[:, 0:4].unsqueeze(2).to_broadcast([128, 4, 64]), op=ALU.mult),
                                  reads=[K(RC), K(CO)], writes=[K(OACC)])
                        fw.op("act", lambda: nc.scalar.copy(out=SELT[:], in_=ps[0][0:32, 0:512]), reads=[K(ps[0])], writes=[K(SELT)])

                        def sel_mask(kt, c_lo, c_hi, tb=tb):
                            delta = tb * 512 - kt * 128
                            M = SELXM[cnt["x"] % 3]
                            cnt["x"] += 1
                            fw.op("pe", lambda: nc.tensor.matmul(out=ps[4][:, c_lo:c_hi], lhsT=EXPB[:, kt * 128:(kt + 1) * 128], rhs=SELT[:, c_lo:c_hi],
                                                                 start=True, stop=True), reads=[K(EXPB), K(SELT)], writes=[K(ps[4])])
                            if delta <= 0:
                                di = (delta + 384) // 128
                                fw.op("dve", lambda: nc.vector.tensor_tensor(out=M[:, c_lo:c_hi], in0=ps[4][:, c_lo:c_hi], in1=WINM[:, di, c_lo:c_hi], op=ALU.mult),
                                      reads=[K(ps[4]), K(WINM)], writes=[K(M)])
                            else:
                                fw.op("act", lambda: nc.scalar.copy(out=M[:, c_lo:c_hi], in_=ps[4][:, c_lo:c_hi]), reads=[K(ps[4])], writes=[K(M)])
                            return (M[:, c_lo:c_hi], K(M))

                        def win_mask(kt, c_lo, c_hi, tb=tb):
                            di = (tb * 512 - kt * 128 + 384) // 128
                            return (WINM[:, di, c_lo:c_hi], K(WINM))

                        attend(tb, list(range(max(0, tb * 4 - 4), tb * 4 + 4)), 3, 1, win_mask, 2, gi,
                               lambda kt, tb=tb: (max(0, kt - tb * 4), min(3, kt + 4 - tb * 4)))
                        attend(tb, list(range(0, tb * 4 + 4)), 2, 0, sel_mask, 1, gi,
                               lambda kt, tb=tb: (max(0, kt - tb * 4), 3))
                        fw.op("act", lambda: nc.scalar.copy(out=OB[:], in_=OACC[:].rearrange("p t h d -> p t (h d)")),
                              reads=[K(OACC)], writes=[K(OB)])
                        fw.dma("sp", lambda e: e.dma_start(
                            out=oS[tb * 512:(tb + 1) * 512, gi * 256:(gi + 1) * 256].rearrange("(t p) c -> p t c", p=128), in_=OB[:]),
                            reads=[K(OB)], writes=[K("oS", (tb, gi))])
                def pre(t):
                    fw.dma("sp", lambda e: e.dma_start(out=OTK[t % 2][:], in_=oS[t * 128:(t + 1) * 128, :]), reads=[K("oS")], writes=[K(OTK[t % 2])])
                    tail_load_x(g, X, Xname, row0, t, tb_bufs)

                pre(0)
                for t in range(TT):
                    ok = OTK[t % 2]
                    if t + 1 < TT:
                        pre(t + 1)
                    mixer_tail(g, esB, l, X, Xname, XO, XOname, row0, t, ok[:], WO, lng, lnb, tb_bufs, K(ok), xloaded=True)
            fw.barrier()


MIXERS[0] = phase_nsa


class ColView:
    def __init__(self, tile, n):
        self.tile, self.n, self.name = tile, n, tile.name

    def __getitem__(self, key):
        return self.tile[:, 0:self.n][key]


class RowView:
    def __init__(self, tile, n):
        self.tile, self.n, self.name = tile, n, tile.name

    def __getitem__(self, key):
        return self.tile[0:4, 0:self.n][key]


def row_scan(g, a, b, n, op):
    nc, fw = g.nc, g.fw
    s = 1
    src, dst = a, b
    while s < n:
        fw.op("dve", lambda: nc.vector.tensor_tensor(out=dst[:, s:n], in0=src[:, s:n], in1=src[:, 0:n - s], op=op),
              reads=[K(src)], writes=[K(dst, "hi")])
        fw.op("dve", lambda: nc.vector.tensor_copy(out=dst[:, 0:s], in_=src[:, 0:s]), reads=[K(src)], writes=[K(dst, "lo")])
        src, dst = dst, src
        s *= 2
    return src, dst


def phase_mlstm(g, l, X, Xname, XO, XOname):
    nc, fw, ps = g.nc, g.fw, g.ps
    sl = l // 3
    nseq = g.NT // T
    w_in = g.w["ml_w_in"][sl]
    qkS, vS, sgS = g.qT_scr, g.v2_scr, g.sg_scr
    LNS = float(np.log(128.0 ** -0.5))
    with ExitStack() as es:
        st1 = sbt(g, es, "mST", [128, 3080], F32)
        CAUS = sbt(g, es, "mCAUS", [128, 4, 512], BF16)
        fw.dma("sp", lambda e: e.dma_start(out=st1[:, 0:2048], in_=g.nsac[:, 2048:4096]), writes=[K(st1)])
        fw.op("dve", lambda: nc.vector.tensor_copy(out=CAUS[:], in_=st1[:, 0:2048].rearrange("p (a b) -> p a b", a=4)),
              reads=[K(st1)], writes=[K(CAUS)])
        CW = sbt(g, es, "mCW", [128, 8, 4], F32)
        CB = sbt(g, es, "mCB", [128, 8], F32)
        GBI = sbt(g, es, "mGBI", [4, 1], F32)
        GBF = sbt(g, es, "mGBF", [4, 1], F32)
        NG = sbt(g, es, "mNG", [128, D], F32)
        ONESB = sbt(g, es, "mONES", [128, 1], BF16)
        SELH = sbt(g, es, "mSELH", [4, 4, 128], F32)
        cT = sbt(g, es, "mcT", [128, TT, 4], F32)
        emmT = sbt(g, es, "memmT", [128, TT, 4], F32)
        NA = sbt(g, es, "mNA", [4, T], F32)
        for j in range(4):
            fw.dma("sp", lambda e: e.dma_start(out=CW[:, :, j], in_=g.w["ml_conv_w"][sl, j, :].rearrange("(c p) -> p c", p=128),
                                               allow_slow_non_contiguous=True), writes=[K(CW, j)])
        fw.dma("sp", lambda e: e.dma_start(out=CB[:], in_=g.w["ml_conv_b"][sl].rearrange("(c p) -> p c", p=128),
                                           allow_slow_non_contiguous=True), writes=[K(CB)])
        fw.dma("sp", lambda e: e.dma_start(out=GBI[:], in_=g.w["ml_gate_b"][sl, 0:4].rearrange("(h o) -> h o", o=1),
                                           allow_slow_non_contiguous=True), writes=[K(GBI)])
        fw.dma("sp", lambda e: e.dma_start(out=GBF[:], in_=g.w["ml_gate_b"][sl, 4:8].rearrange("(h o) -> h o", o=1),
                                           allow_slow_non_contiguous=True), writes=[K(GBF)])
        fw.dma("sp", lambda e: e.dma_start(out=NG[:], in_=g.w["ml_norm_g"][sl, :].partition_broadcast(128)), writes=[K(NG)])
        fw.op("dve", lambda: nc.vector.memset(ONESB[:], 1.0), writes=[K(ONESB)])
        fw.op("dve", lambda: nc.vector.tensor_copy(out=SELH[:], in_=g.ident[0:4, 0:4].unsqueeze(2).to_broadcast([4, 4, 128])),
              reads=[K(g.ident)], writes=[K(SELH)])
        fw.op("dve", lambda: nc.vector.tensor_scalar_mul(out=GBF[:], in0=GBF[:], scalar1=-1.0), reads=[K(GBF)], writes=[K(GBF)])
        WO, lng, lnb, tb_bufs = load_tail_weights(g, es, l, g.w["ml_w_out"][sl], [st1, st1])

        for s in range(nseq):
            row0 = s * T
            with ExitStack() as esA:
                WI = sbt(g, esA, "mWI", [128, 8, 3080], BF16)
                XT = sbt(g, esA, "mXT", [128, 8, T], BF16)
                PRE = sbt(g, esA, "mPRE", [128, T + 3], F32)
                ACC = sbt(g, esA, "mACC", [128, T], F32)
                QKB = sbt(g, esA, "mQKB", [128, T], BF16)
                EV = [sbt(g, esA, f"mEV{i}", [128, 512], BF16) for i in range(3)]
                R0 = sbt(g, esA, "mR0", [4, T], F32)
                R1 = sbt(g, esA, "mR1", [4, T], F32)
                R2 = RowView(ACC, T)
                R3 = RowView(PRE, T)
                load_w_bf16(g, None, w_in, 3080, [st1, st1],
                            lambda kc, st: (fw.op("act", lambda: nc.scalar.copy(out=WI[:, kc, 0:1540], in_=st[:, 0:1540]),
                                                  reads=[K(st)], writes=[K(WI, (kc, 0))]),
                                            fw.op("dve", lambda: nc.vector.tensor_copy(out=WI[:, kc, 1540:3080], in_=st[:, 1540:3080]),
                                                  reads=[K(st)], writes=[K(WI, (kc, 1))])))
                build_xT(g, esA, X, Xname, row0, XT, tb_bufs["xt"])
                fw.op("dve", lambda: nc.vector.memset(PRE[:, 0:3], 0.0), writes=[K(PRE, "pad")])
                np_ = [0]
                for c in range(8):
                    for tb in range(4):
                        pS = ps[2 + np_[0] % 2]
                        np_[0] += 1
                        for kc in range(8):
                            fw.op("pe", lambda: nc.tensor.matmul(out=pS[:, :], lhsT=WI[:, kc, c * 128:(c + 1) * 128],
                                                                 rhs=XT[:, kc, tb * 512:(tb + 1) * 512], start=(kc == 0), stop=(kc == 7)),
                                  reads=[K(WI), K(XT)], writes=[K(pS)], inc=(kc == 7))
                        fw.op("act", lambda: nc.scalar.copy(out=PRE[:, 3 + tb * 512:3 + (tb + 1) * 512], in_=pS[:, :]),
                              reads=[K(pS)], writes=[K(PRE, tb)])
                    fw.op("dve", lambda: nc.vector.tensor_scalar(out=ACC[:], in0=PRE[:, 3:T + 3], scalar1=CW[:, c, 3:4], scalar2=None,
                                                                 op0=ALU.mult), reads=[K(PRE), K(CW)], writes=[K(ACC)])
                    for j in range(3):
                        eng = ("dve", nc.vector)
                        fw.op(eng[0], lambda: eng[1].scalar_tensor_tensor(out=ACC[:], in0=PRE[:, j:T + j], scalar=CW[:, c, j:j + 1],
                                                                          in1=ACC[:], op0=ALU.mult, op1=ALU.add),
                              reads=[K(PRE), K(CW), K(ACC)], writes=[K(ACC)])
                    fw.op("act", lambda: nc.scalar.activation(out=QKB[:], in_=ACC[:], func=AF.Silu, bias=CB[:, c:c + 1]),
                          reads=[K(ACC), K(CB)], writes=[K(QKB)])
                    fw.dma("sp", lambda e: e.dma_start(out=qkS[c * 128:(c + 1) * 128, :], in_=QKB[:]), reads=[K(QKB)], writes=[K("qkS", c)])
                nev = [0]

                def evac_store(pS, dst_ap, dkey, sig):
                    E = EV[nev[0] % 3]
                    nev[0] += 1
                    if sig:
                        fw.op("act", lambda: nc.scalar.activation(out=E[:], in_=pS[:, :], func=AF.Sigmoid), reads=[K(pS)], writes=[K(E)])
                    else:
                        fw.op("dve", lambda: nc.vector.tensor_copy(out=E[:], in_=pS[:, :]), reads=[K(pS)], writes=[K(E)])
                    fw.dma("sp", lambda e: e.dma_start(out=dst_ap, in_=E[:]), reads=[K(E)], writes=[dkey])

                for t in range(TT):
                    for (c0, dstS, nm, sig) in ((1024, vS, "vS2", False), (2048, sgS, "sgS", True)):
                        for nh in range(2):
                            pS = ps[2 + np_[0] % 2]
                            np_[0] += 1
                            for kc in range(8):
                                fw.op("pe", lambda: nc.tensor.matmul(out=pS[:, :], lhsT=XT[:, kc, t * 128:(t + 1) * 128],
                                                                     rhs=WI[:, kc, c0 + nh * 512:c0 + (nh + 1) * 512],
                                                                     start=(kc == 0), stop=(kc == 7)),
                                      reads=[K(XT), K(WI)], writes=[K(pS)], inc=(kc == 7))
                            evac_store(pS, dstS[t * 128:(t + 1) * 128, nh * 512:(nh + 1) * 512], K(nm, (t, nh)), sig)
                for (c0, R) in ((3072, R0), (3076, R1)):
                    for tb in range(4):
                        pS = ps[2 + np_[0] % 2]
                        np_[0] += 1
                        for kc in range(8):
                            fw.op("pe", lambda: nc.tensor.matmul(out=pS[0:4, :], lhsT=WI[:, kc, c0:c0 + 4],
                                                                 rhs=XT[:, kc, tb * 512:(tb + 1) * 512], start=(kc == 0), stop=(kc == 7)),
                                  reads=[K(WI), K(XT)], writes=[K(pS)], inc=(kc == 7))
                        if c0 == 3072:
                            fw.op("act", lambda: nc.scalar.activation(out=R[:, tb * 512:(tb + 1) * 512], in_=pS[0:4, :], func=AF.Identity,
                                                                      bias=GBI[:, 0:1]), reads=[K(pS), K(GBI)], writes=[K(R, tb)])
                        else:
                            fw.op("act", lambda: nc.scalar.activation(out=R[:, tb * 512:(tb + 1) * 512], in_=pS[0:4, :], func=AF.Exp,
                                                                      bias=GBF[:, 0:1], scale=-1.0), reads=[K(pS), K(GBF)], writes=[K(R, tb)])
                fw.op("act", lambda: nc.scalar.activation(out=R1[:], in_=R1[:], func=AF.Ln, bias=1.0), reads=[K(R1)], writes=[K(R1)])
                fw.op("dve", lambda: nc.vector.tensor_scalar_mul(out=R1[:], in0=R1[:], scalar1=-1.0), reads=[K(R1)], writes=[K(R1)])
                Bt, free = row_scan(g, R1, R2, T, ALU.add)
                other = R3
                fw.op("dve", lambda: nc.vector.tensor_tensor(out=other[:], in0=R0[:], in1=Bt[:], op=ALU.subtract),
                      reads=[K(R0), K(Bt)], writes=[K(other)])
                Cm, free2 = row_scan(g, other, free, T, ALU.max)
                fw.op("dve", lambda: nc.vector.tensor_scalar(out=NA[:], in0=Cm[:], scalar1=0.0, scalar2=-1.0, op0=ALU.max, op1=ALU.mult),
                      reads=[K(Cm)], writes=[K(NA)])
                fw.op("dve", lambda: nc.vector.tensor_tensor(out=free2[:], in0=NA[:], in1=Bt[:], op=ALU.subtract),
                      reads=[K(NA), K(Bt)], writes=[K(free2)])
                fw.op("act", lambda: nc.scalar.activation(out=free2[:], in_=free2[:], func=AF.Exp), reads=[K(free2)], writes=[K(free2)])
                fw.op("dve", lambda: nc.vector.tensor_tensor(out=R0[:], in0=R0[:], in1=Bt[:], op=ALU.subtract),
                      reads=[K(R0), K(Bt)], writes=[K(R0)])
                fw.op("dve", lambda: nc.vector.tensor_scalar_add(out=R0[:], in0=R0[:], scalar1=LNS), reads=[K(R0)], writes=[K(R0)])
                for (src, dstT) in ((R0, cT), (free2, emmT)):
                    for t in range(TT):
                        fw.op("pe", lambda: nc.tensor.transpose(out=ps[4][:, t * 4:(t + 1) * 4], in_=src[0:4, t * 128:(t + 1) * 128],
                                                                identity=g.ident[0:4, 0:4]),
                              reads=[K(src), K(g.ident)], writes=[K(ps[4])], inc=(t == TT - 1))
                    fw.op("dve", lambda: nc.vector.tensor_copy(out=dstT[:].rearrange("p t h -> p (t h)"), in_=ps[4][:, 0:TT * 4]),
                          reads=[K(ps[4])], writes=[K(dstT)])
            fw.barrier()
            with ExitStack() as esB:
                QT = sbt(g, esB, "mQT", [128, 4, T], BF16)
                KT = sbt(g, esB, "mKT", [128, 4, T], BF16)
                V = sbt(g, esB, "mV", [128, TT, D], BF16)
                EF = [sbt(g, esB, f"mEF{i}", [128, 512], F32) for i in range(3)]
                PT = [sbt(g, esB, f"mPT{i}", [128, 512], BF16) for i in range(4)]
                HN = sbt(g, esB, "mHN", [128, 4, D], F32)
                DEN = sbt(g, esB, "mDEN", [128, 4], F32)
                SQ = sbt(g, esB, "mSQ", [128, D], F32)
                MS = sbt(g, esB, "mMS", [128, 4], F32)
                SGt = [sbt(g, esB, f"mSGt{i}", [128, D], BF16) for i in range(2)]
                OTK = [sbt(g, esB, f"mOTK{i}", [128, D], BF16) for i in range(2)]
                for h in range(4):
                    fw.dma("sp", lambda e: e.dma_start(out=QT[:, h, :], in_=qkS[h * 128:(h + 1) * 128, :]), reads=[K("qkS")], writes=[K(QT, h)])
                    fw.dma("sp", lambda e: e.dma_start(out=KT[:, h, :], in_=qkS[512 + h * 128:512 + (h + 1) * 128, :]),
                           reads=[K("qkS")], writes=[K(KT, h)])
                for t4 in range(4):
                    fw.dma("sp", lambda e: e.dma_start(out=V[:, t4 * 4:(t4 + 1) * 4, :],
                                                       in_=vS[t4 * 512:(t4 + 1) * 512, :].rearrange("(t p) d -> p t d", p=128)),
                           reads=[K("vS2")], writes=[K(V, t4)])
                cnt = {"s": 0, "e": 0, "p": 0}
                for tb in range(4):
                    for h in range(4):
                        fw.op("pe", lambda: nc.tensor.matmul(out=ps[4][:, :], lhsT=SELH[:, h, :], rhs=NA[:, tb * 512:(tb + 1) * 512],
                                                             start=True, stop=True), reads=[K(SELH), K(NA)], writes=[K(ps[4])])
                        for bk in (5, 6, 7):
                            fw.op("dve", lambda: nc.vector.memset(ps[bk][:, :], 0.0), writes=[K(ps[bk])])
                        def emit_S(kt, h=h, tb=tb):
                            pS_ = ps[1 + cnt["s"] % 3]
                            cnt["s"] += 1
                            cl = max(0, kt - tb * 4) * 128
                            fw.op("pe", lambda: nc.tensor.matmul(out=pS_[:, cl:512], lhsT=KT[:, h, kt * 128:(kt + 1) * 128],
                                                                 rhs=QT[:, h, tb * 512 + cl:(tb + 1) * 512], start=True, stop=True),
                                  reads=[K(KT), K(QT)], writes=[K(pS_)])
                            return pS_

                        pend = [emit_S(0), emit_S(1)]
                        for kt in range(tb * 4 + 4):
                            pS = pend.pop(0)
                            if kt + 2 < tb * 4 + 4:
                                pend.append(emit_S(kt + 2))
                            E = EF[cnt["e"] % 3]
                            cnt["e"] += 1
                            cl = max(0, kt - tb * 4) * 128
                            fw.op("act", lambda: nc.scalar.activation(out=E[:, cl:512], in_=ps[4][:, cl:512], func=AF.Exp, bias=cT[:, kt, h:h + 1]),
                                  reads=[K(ps[4]), K(cT)], writes=[K(E)])
                            P = PT[cnt["p"] % 4]
                            cnt["p"] += 1
                            fw.op("dve", lambda: nc.vector.tensor_tensor(out=P[:, cl:512], in0=pS[:, cl:512], in1=E[:, cl:512], op=ALU.mult),
                                  reads=[K(pS), K(E)], writes=[K(P)])
                            if kt >= tb * 4:
                                di = (tb * 512 - kt * 128 + 384) // 128
                                fw.op("pool", lambda: nc.gpsimd.tensor_tensor(out=P[:, cl:512], in0=P[:, cl:512], in1=CAUS[:, di, cl:512], op=ALU.mult),
                                      reads=[K(P), K(CAUS)], writes=[K(P)])
                            for tt in range(4):
                                if kt > tb * 4 + tt:
                                    continue
                                last = (kt == tb * 4 + tt)
                                pn = ps[5 + tt // 2]
                                c0 = (tt % 2) * 256
                                fw.op("pe", lambda: nc.tensor.matmul(out=pn[:, c0:c0 + 256], lhsT=P[:, tt * 128:(tt + 1) * 128],
                                                                     rhs=V[:, kt, h * 256:(h + 1) * 256], start=False, stop=last,
                                                                     skip_group_check=True),
                                      reads=[K(P), K(V)], writes=[K(pn, c0)])
                                fw.op("pe", lambda: nc.tensor.matmul(out=ps[7][:, tt:tt + 1], lhsT=P[:, tt * 128:(tt + 1) * 128],
                                                                     rhs=ONESB[:, 0:1], start=False, stop=last, skip_group_check=True),
                                      reads=[K(P), K(ONESB)], writes=[K(ps[7], tt)], inc=last)
                        fw.op("dve", lambda: nc.vector.tensor_scalar_mul(out=MS[:], in0=ps[7][:, 0:4], scalar1=-1.0),
                              reads=[K(ps[7])], writes=[K(MS)])
                        fw.op("dve", lambda: nc.vector.tensor_tensor(out=DEN[:], in0=ps[7][:, 0:4], in1=MS[:], op=ALU.max),
                              reads=[K(ps[7]), K(MS)], writes=[K(DEN)])
                        fw.op("dve", lambda: nc.vector.tensor_tensor(out=DEN[:], in0=DEN[:], in1=emmT[:, tb * 4:(tb + 1) * 4, h],
                                                                     op=ALU.max), reads=[K(DEN), K(emmT)], writes=[K(DEN)])
                        fw.op("dve", lambda: nc.vector.reciprocal(out=DEN[:], in_=DEN[:]), reads=[K(DEN)], writes=[K(DEN)])
                        for tt in range(4):
                            pn = ps[5 + tt // 2]
                            c0 = (tt % 2) * 256
                            fw.op("act", lambda: nc.scalar.activation(out=HN[:, tt, h * 256:(h + 1) * 256], in_=pn[:, c0:c0 + 256],
                                                                      func=AF.Copy, scale=DEN[:, tt:tt + 1]),
                                  reads=[K(pn), K(DEN)], writes=[K(HN, (tt, h))])
                    def pre(t):
                        fw.dma("sp", lambda e: e.dma_start(out=SGt[t % 2][:], in_=sgS[t * 128:(t + 1) * 128, :]), reads=[K("sgS")], writes=[K(SGt[t % 2])])
                        tail_load_x(g, X, Xname, row0, t, tb_bufs)

                    pre(tb * 4)
                    for tt in range(4):
                        t = tb * 4 + tt
                        sgt, ok = SGt[t % 2], OTK[t % 2]
                        if tt < 3:
                            pre(t + 1)
                        fw.op("pool", lambda: nc.gpsimd.tensor_tensor(out=SQ[:], in0=HN[:, tt, :], in1=HN[:, tt, :], op=ALU.mult),
                              reads=[K(HN)], writes=[K(SQ)])
                        fw.op("dve", lambda: nc.vector.tensor_reduce(out=MS[:], in_=SQ[:].rearrange("p (h d) -> p h d", h=4), axis=AX.X, op=ALU.add),
                              reads=[K(SQ)], writes=[K(MS)])
                        fw.op("dve", lambda: nc.vector.tensor_scalar(out=MS[:], in0=MS[:], scalar1=1.0 / 256, scalar2=1e-6, op0=ALU.mult, op1=ALU.add),
                              reads=[K(MS)], writes=[K(MS)])
                        fw.op("act", lambda: nc.scalar.sqrt(out=MS[:], in_=MS[:]), reads=[K(MS)], writes=[K(MS)])
                        fw.op("dve", lambda: nc.vector.reciprocal(out=MS[:], in_=MS[:]), reads=[K(MS)], writes=[K(MS)])
                        fw.op("dve", lambda: nc.vector.tensor_tensor(out=SQ[:].rearrange("p (h d) -> p h d", h=4),
                                                                     in0=HN[:, tt, :].rearrange("p (h d) -> p h d", h=4),
                                                                     in1=MS[:].unsqueeze(2).to_broadcast([128, 4, 256]), op=ALU.mult),
                              reads=[K(HN), K(MS)], writes=[K(SQ)])
                        fw.op("pool", lambda: nc.gpsimd.tensor_tensor(out=SQ[:], in0=SQ[:], in1=NG[:], op=ALU.mult),
                              reads=[K(SQ), K(NG)], writes=[K(SQ)])
                        fw.op("dve", lambda: nc.vector.tensor_tensor(out=ok[:], in0=SQ[:], in1=sgt[:], op=ALU.mult),
                              reads=[K(SQ), K(sgt)], writes=[K(ok)])
                        mixer_tail(g, esB, l, X, Xname, XO, XOname, row0, t, ok[:], WO, lng, lnb, tb_bufs, K(ok), xloaded=True)
            fw.barrier()


MIXERS[1] = phase_mlstm


HL = 64


def phase_hgrn(g, l, X, Xname, XO, XOname):
    nc, fw, ps = g.nc, g.fw, g.ps
    sl = l // 3
    nseq = g.NT // T
    w_in = g.w["hg_w_in"][sl]
    qdS = g.qT_scr
    kdS = g.kT_scr.rearrange("a c t -> (a c) t")
    vS, sgS, kkS = g.v2_scr, g.sg_scr, g.o_scr
    NCH = T // HL
    with ExitStack() as es:
        st1 = sbt(g, es, "hST", [128, 4096], F32)
        NG = sbt(g, es, "hNG", [128, D], F32)
        LBb = sbt(g, es, "hLBb", [128, D], F32)
        OMb = sbt(g, es, "hOMb", [128, D], F32)
        LBp = sbt(g, es, "hLBp", [128, 8], F32)
        OMp = sbt(g, es, "hOMp", [128, 8], F32)
        HLp = sbt(g, es, "hHLp", [128, 8, 4], F32)
        UM = sbt(g, es, "hUM", [128, 128], BF16)
        SUFU = sbt(g, es, "hSUFU", [128, 128], F32)
        EGE = sbt(g, es, "hEGE", [128, 8, NCH], F32)
        fw.dma("sp", lambda e: e.dma_start(out=NG[:], in_=g.w["hg_norm_g"][sl, :].partition_broadcast(128)), writes=[K(NG)])
        fw.op("dve", lambda: nc.vector.tensor_tensor(out=UM[:], in0=g.C[:, 0:128], in1=g.C[:, 128:256], op=ALU.add),
              reads=[K(g.C)], writes=[K(UM)])
        fw.op("dve", lambda: nc.vector.tensor_copy(out=SUFU[:], in_=g.C[:, 3 * 128 + NB_MAX + 8:3 * 128 + NB_MAX + 8 + 128]),
              reads=[K(g.C)], writes=[K(SUFU)])
        for r in range(4):
            fw.dma("sp", lambda e: e.dma_start(out=st1[:, r * 1024:(r + 1) * 1024], in_=g.w["hg_lower"][r, :].partition_broadcast(128)),
                   writes=[K(st1, r)])
            fw.dma("sp", lambda e: e.dma_start(out=HLp[:, :, r], in_=g.w["hg_lower"][r, :].rearrange("(c p) -> p c", p=128),
                                               allow_slow_non_contiguous=True), writes=[K(HLp, r)])
        fw.op("act", lambda: nc.scalar.activation(out=st1[:], in_=st1[:], func=AF.Exp), reads=[K(st1)], writes=[K(st1)])
        fw.op("act", lambda: nc.scalar.activation(out=HLp[:], in_=HLp[:], func=AF.Exp), reads=[K(HLp)], writes=[K(HLp)])
        e4 = st1[:].rearrange("p (r d) -> p r d", r=4)
        fw.op("dve", lambda: nc.vector.tensor_tensor(out=OMb[:], in0=e4[:, 0, :], in1=e4[:, 1, :], op=ALU.add), reads=[K(st1)], writes=[K(OMb)])
        fw.op("dve", lambda: nc.vector.tensor_tensor(out=OMb[:], in0=OMb[:], in1=e4[:, 2, :], op=ALU.add), reads=[K(st1), K(OMb)], writes=[K(OMb)])
        fw.op("dve", lambda: nc.vector.tensor_tensor(out=OMb[:], in0=OMb[:], in1=e4[:, 3, :], op=ALU.add), reads=[K(st1), K(OMb)], writes=[K(OMb)])
        fw.op("dve", lambda: nc.vector.reciprocal(out=OMb[:], in_=OMb[:]), reads=[K(OMb)], writes=[K(OMb)])
        fw.op("dve", lambda: nc.vector.tensor_copy(out=LBb[:], in_=e4[:, 1, :]), reads=[K(st1)], writes=[K(LBb)])
        for i in range(2, l + 1):
            fw.op("dve", lambda: nc.vector.tensor_tensor(out=LBb[:], in0=LBb[:], in1=e4[:, i, :], op=ALU.add), reads=[K(st1), K(LBb)], writes=[K(LBb)])
        fw.op("dve", lambda: nc.vector.tensor_tensor(out=LBb[:], in0=LBb[:], in1=OMb[:], op=ALU.mult), reads=[K(LBb), K(OMb)], writes=[K(LBb)])
        fw.op("dve", lambda: nc.vector.tensor_scalar(out=OMb[:], in0=LBb[:], scalar1=-1.0, scalar2=1.0, op0=ALU.mult, op1=ALU.add),
              reads=[K(LBb)], writes=[K(OMb)])
        fw.op("dve", lambda: nc.vector.tensor_reduce(out=OMp[:], in_=HLp[:], axis=AX.X, op=ALU.add), reads=[K(HLp)], writes=[K(OMp)])
        fw.op("dve", lambda: nc.vector.reciprocal(out=OMp[:], in_=OMp[:]), reads=[K(OMp)], writes=[K(OMp)])
        fw.op("dve", lambda: nc.vector.tensor_reduce(out=LBp[:], in_=HLp[:, :, 1:l + 1], axis=AX.X, op=ALU.add), reads=[K(HLp)], writes=[K(LBp)])
        fw.op("dve", lambda: nc.vector.tensor_tensor(out=LBp[:], in0=LBp[:], in1=OMp[:], op=ALU.mult), reads=[K(LBp), K(OMp)], writes=[K(LBp)])
        fw.op("dve", lambda: nc.vector.tensor_scalar(out=OMp[:], in0=LBp[:], scalar1=-1.0, scalar2=1.0, op0=ALU.mult, op1=ALU.add),
              reads=[K(LBp)], writes=[K(OMp)])
        WO, lng, lnb, tb_bufs = load_tail_weights(g, es, l, g.w["hg_w_out"][sl], [st1, st1])

        for s in range(nseq):
            row0 = s * T
            with ExitStack() as esA:
                WI = sbt(g, esA, "hWI", [128, 8, 4096], BF16)
                XT = sbt(g, esA, "hXT", [128, 8, T], BF16)
                FA = sbt(g, esA, "hFA", [128, T], F32)
                FB = sbt(g, esA, "hFB", [128, T], F32)
                FC = sbt(g, esA, "hFC", [128, T], F32)
                QB = sbt(g, esA, "hQB", [128, T], BF16)
                EV = [sbt(g, esA, f"hEV{i}", [128, 512], BF16) for i in range(3)]
                TM = [ColView(FA, D), ColView(FB, D), ColView(FC, D)]
                TMb = ColView(QB, D)
                load_w_bf16(g, None, w_in, 4096, [st1, st1],
                            lambda kc, st: (fw.op("act", lambda: nc.scalar.copy(out=WI[:, kc, 0:2048], in_=st[:, 0:2048]),
                                                  reads=[K(st)], writes=[K(WI, (kc, 0))]),
                                            fw.op("dve", lambda: nc.vector.tensor_copy(out=WI[:, kc, 2048:4096], in_=st[:, 2048:4096]),
                                                  reads=[K(st)], writes=[K(WI, (kc, 1))])))
                build_xT(g, esA, X, Xname, row0, XT, tb_bufs["xt"])
                np_ = [0]

                def proj_fm(c0, dst):
                    for tb in range(4):
                        pS = ps[2 + np_[0] % 2]
                        np_[0] += 1
                        for kc in range(8):
                            fw.op("pe", lambda: nc.tensor.matmul(out=pS[:, :], lhsT=WI[:, kc, c0:c0 + 128],
                                                                 rhs=XT[:, kc, tb * 512:(tb + 1) * 512], start=(kc == 0), stop=(kc == 7)),
                                  reads=[K(WI), K(XT)], writes=[K(pS)], inc=(kc == 7))
                        fw.op("act", lambda: nc.scalar.copy(out=dst[:, tb * 512:(tb + 1) * 512], in_=pS[:, :]),
                              reads=[K(pS)], writes=[K(dst, tb)])

                for h in range(8):
                    proj_fm(1024 + h * 128, FA)
                    fw.op("act", lambda: nc.scalar.activation(out=FA[:], in_=FA[:], func=AF.Sigmoid), reads=[K(FA)], writes=[K(FA)])
                    fw.op("dve", lambda: nc.vector.tensor_scalar(out=FA[:], in0=FA[:], scalar1=OMp[:, h:h + 1], scalar2=LBp[:, h:h + 1],
                                                                 op0=ALU.mult, op1=ALU.add), reads=[K(FA), K(OMp), K(LBp)], writes=[K(FA)])
                    fw.op("act", lambda: nc.scalar.activation(out=FB[:], in_=FA[:], func=AF.Ln), reads=[K(FA)], writes=[K(FB)])
                    src, dst = FB, FC
                    sft = 1
                    while sft < HL:
                        s3, d3 = (x_[:].rearrange("p (c j) -> p c j", j=HL) for x_ in (src, dst))
                        fw.op("dve", lambda: nc.vector.tensor_tensor(out=d3[:, :, sft:], in0=s3[:, :, sft:], in1=s3[:, :, :HL - sft], op=ALU.add),
                              reads=[K(src)], writes=[K(dst, "hi")])
                        fw.op("act", lambda: nc.scalar.copy(out=d3[:, :, :sft], in_=s3[:, :, :sft]), reads=[K(src)], writes=[K(dst, "lo")])
                        src, dst = dst, src
                        sft *= 2
                    Gt, tmp = src, dst
                    fw.op("act", lambda: nc.scalar.activation(out=EGE[:, h, :], in_=Gt[:].rearrange("p (c j) -> p c j", j=HL)[:, :, HL - 1],
                                                              func=AF.Exp), reads=[K(Gt)], writes=[K(EGE, h)])
                    fw.op("act", lambda: nc.scalar.activation(out=tmp[:], in_=Gt[:], func=AF.Exp, scale=-1.0), reads=[K(Gt)], writes=[K(tmp)])
                    fw.op("dve", lambda: nc.vector.tensor_scalar(out=FA[:], in0=FA[:], scalar1=-1.0, scalar2=1.0, op0=ALU.mult, op1=ALU.add),
                          reads=[K(FA)], writes=[K(FA)])
                    fw.op("dve", lambda: nc.vector.tensor_tensor(out=QB[:], in0=FA[:], in1=tmp[:], op=ALU.mult), reads=[K(FA), K(tmp)], writes=[K(QB)])
                    fw.dma("sp", lambda e: e.dma_start(out=kdS[h * 128:(h + 1) * 128, :], in_=QB[:]), reads=[K(QB)], writes=[K("kdS", h)])
                    fw.op("act", lambda: nc.scalar.activation(out=tmp[:], in_=Gt[:], func=AF.Exp), reads=[K(Gt)], writes=[K(tmp)])
                    proj_fm(h * 128, FA)
                    fw.op("act", lambda: nc.scalar.activation(out=FA[:], in_=FA[:], func=AF.Silu), reads=[K(FA)], writes=[K(FA)])
                    fw.op("dve", lambda: nc.vector.tensor_tensor(out=QB[:], in0=FA[:], in1=tmp[:], op=ALU.mult), reads=[K(FA), K(tmp)], writes=[K(QB)])
                    fw.dma("sp", lambda e: e.dma_start(out=qdS[h * 128:(h + 1) * 128, :], in_=QB[:]), reads=[K(QB)], writes=[K("qdS", h)])
                for t in range(TT):
                    for (c0, dstS, nm, sig) in ((2048, vS, "vS2", False), (3072, sgS, "sgS", True)):
                        for nh in range(2):
                            pS = ps[2 + np_[0] % 2]
                            np_[0] += 1
                            for kc in range(8):
                                fw.op("pe", lambda: nc.tensor.matmul(out=pS[:, :], lhsT=XT[:, kc, t * 128:(t + 1) * 128],
                                                                     rhs=WI[:, kc, c0 + nh * 512:c0 + (nh + 1) * 512],
                                                                     start=(kc == 0), stop=(kc == 7)),
                                      reads=[K(XT), K(WI)], writes=[K(pS)], inc=(kc == 7))
                            E = EV[np_[0] % 3]
                            if sig:
                                fw.op("act", lambda: nc.scalar.activation(out=E[:], in_=pS[:, :], func=AF.Sigmoid), reads=[K(pS)], writes=[K(E)])
                            else:
                                fw.op("dve", lambda: nc.vector.tensor_copy(out=E[:], in_=pS[:, :]), reads=[K(pS)], writes=[K(E)])
                            fw.dma("sp", lambda e: e.dma_start(out=dstS[t * 128:(t + 1) * 128, nh * 512:(nh + 1) * 512], in_=E[:]),
                                   reads=[K(E)], writes=[K(nm, (t, nh))])
                    fT, lfT, sfx = TM
                    for nh in range(2):
                        pS = ps[2 + np_[0] % 2]
                        np_[0] += 1
                        for kc in range(8):
                            fw.op("pe", lambda: nc.tensor.matmul(out=pS[:, :], lhsT=XT[:, kc, t * 128:(t + 1) * 128],
                                                                 rhs=WI[:, kc, 1024 + nh * 512:1024 + (nh + 1) * 512],
                                                                 start=(kc == 0), stop=(kc == 7)),
                                  reads=[K(XT), K(WI)], writes=[K(pS)], inc=(kc == 7))
                        fw.op("act", lambda: nc.scalar.activation(out=fT[:, nh * 512:(nh + 1) * 512], in_=pS[:, :], func=AF.Sigmoid),
                              reads=[K(pS)], writes=[K(fT, nh)])
                    fw.op("dve", lambda: nc.vector.tensor_tensor(out=fT[:], in0=fT[:], in1=OMb[:], op=ALU.mult), reads=[K(fT), K(OMb)], writes=[K(fT)])
                    fw.op("pool", lambda: nc.gpsimd.tensor_tensor(out=fT[:], in0=fT[:], in1=LBb[:], op=ALU.add), reads=[K(fT), K(LBb)], writes=[K(fT)])
                    fw.op("act", lambda: nc.scalar.activation(out=lfT[:], in_=fT[:], func=AF.Ln), reads=[K(fT)], writes=[K(lfT)])
                    for nh in range(2):
                        pS = ps[4 + nh]
                        fw.op("pe", lambda: nc.tensor.matmul(out=pS[:, :], lhsT=SUFU[:], rhs=lfT[:, nh * 512:(nh + 1) * 512], start=True, stop=True),
                              reads=[K(SUFU), K(lfT)], writes=[K(pS)])
                        fw.op("act", lambda: nc.scalar.activation(out=sfx[:, nh * 512:(nh + 1) * 512], in_=pS[:, :], func=AF.Exp),
                              reads=[K(pS)], writes=[K(sfx, nh)])
                    fw.op("dve", lambda: nc.vector.tensor_scalar(out=fT[:], in0=fT[:], scalar1=-1.0, scalar2=1.0, op0=ALU.mult, op1=ALU.add),
                          reads=[K(fT)], writes=[K(fT)])
                    fw.op("dve", lambda: nc.vector.tensor_tensor(out=TMb[:], in0=fT[:], in1=sfx[:], op=ALU.mult), reads=[K(fT), K(sfx)], writes=[K(TMb)])
                    fw.dma("sp", lambda e: e.dma_start(out=kkS[t * 128:(t + 1) * 128, :], in_=TMb[:]), reads=[K(TMb)], writes=[K("kkS", t)])
            fw.barrier()
            with ExitStack() as esB:
                S = sbt(g, esB, "hS", [128, 8, 128], F32)
                Sb = sbt(g, esB, "hSb", [128, 8, 128], BF16)
                QD = sbt(g, esB, "hQD", [128, 8, 512], BF16)
                KD = sbt(g, esB, "hKD", [128, 8, 512], BF16)
                V64 = sbt(g, esB, "hV64", [64, 8, D], BF16)
                KK64 = sbt(g, esB, "hKK64", [64, 8, D], BF16)
                AT = sbt(g, esB, "hAT", [64, 8, 64], BF16)
                HN = sbt(g, esB, "hHN", [64, D], F32)
                SQ = sbt(g, esB, "hSQ", [64, D], F32)
                MS = sbt(g, esB, "hMS", [64, 8], F32)
                SGt = [sbt(g, esB, f"hSGt{i}", [64, D], BF16) for i in range(2)]
                OTK = [sbt(g, esB, f"hOTK{i}", [64, D], BF16) for i in range(2)]
                fw.op("dve", lambda: nc.vector.memset(S[:], 0.0), writes=[K(S)])
                fw.op("dve", lambda: nc.vector.memset(Sb[:], 0.0), writes=[K(Sb)])
                for tb in range(4):
                    fw.dma("sp", lambda e: e.dma_start(out=QD[:], in_=qdS[:, tb * 512:(tb + 1) * 512].rearrange("(h k) t -> k h t", k=128)),
                           reads=[K("qdS")], writes=[K(QD)])
                    fw.dma("sp", lambda e: e.dma_start(out=KD[:], in_=kdS[:, tb * 512:(tb + 1) * 512].rearrange("(h k) t -> k h t", k=128)),
                           reads=[K("kdS")], writes=[K(KD)])
                    fw.dma("sp", lambda e: e.dma_start(out=V64[:], in_=vS[tb * 512:(tb + 1) * 512, :].rearrange("(c s) d -> s c d", s=HL)),
                           reads=[K("vS2")], writes=[K(V64)])
                    fw.dma("sp", lambda e: e.dma_start(out=KK64[:], in_=kkS[tb * 512:(tb + 1) * 512, :].rearrange("(c s) d -> s c d", s=HL)),
                           reads=[K("kkS")], writes=[K(KK64)])
                    for ci in range(8):
                        cg = tb * 8 + ci
                        cs = slice(ci * HL, (ci + 1) * HL)
                        fw.dma("sp", lambda e: e.dma_start(out=SGt[cg % 2][:], in_=sgS[cg * HL:(cg + 1) * HL, :]), reads=[K("sgS")], writes=[K(SGt[cg % 2])])
                        tail_load_x(g, X, Xname, row0, cg, tb_bufs, rows=HL)
                        for h in range(8):
                            fw.op("pe", lambda: nc.tensor.matmul(out=ps[2][0:64, h * 64:(h + 1) * 64], lhsT=KD[:, h, cs], rhs=QD[:, h, cs],
                                                                 start=True, stop=True), reads=[K(KD), K(QD)], writes=[K(ps[2])], inc=(h == 7))
                        fw.op("dve", lambda: nc.vector.tensor_tensor(out=AT[:], in0=ps[2][0:64, :].rearrange("p (h t) -> p h t", h=8),
                                                                     in1=UM[0:64, 0:64].unsqueeze(1).to_broadcast([64, 8, 64]), op=ALU.mult),
                              reads=[K(ps[2]), K(UM)], writes=[K(AT)])
                        for h in range(8):
                            po = ps[3 + h // 4]
                            c0 = (h % 4) * 128
                            fw.op("pe", lambda: nc.tensor.matmul(out=po[0:64, c0:c0 + 128], lhsT=AT[:, h, :], rhs=V64[:, ci, h * 128:(h + 1) * 128],
                                                                 start=True, stop=False), reads=[K(AT), K(V64)], writes=[K(po)], inc=False)
                            fw.op("pe", lambda: nc.tensor.matmul(out=po[0:64, c0:c0 + 128], lhsT=QD[:, h, cs], rhs=Sb[:, h, :],
                                                                 start=False, stop=True), reads=[K(QD), K(Sb)], writes=[K(po)], inc=(h % 4 == 3))
                        for hh in range(2):
                            fw.op("act", lambda: nc.scalar.copy(out=HN[:, hh * 512:(hh + 1) * 512], in_=ps[3 + hh][0:64, :]),
                                  reads=[K(ps[3 + hh])], writes=[K(HN, hh)])
                        for hh in range(2):
                            for h4 in range(4):
                                h = hh * 4 + h4
                                fw.op("pe", lambda: nc.tensor.matmul(out=ps[5][:, h4 * 128:(h4 + 1) * 128], lhsT=KK64[:, ci, h * 128:(h + 1) * 128],
                                                                     rhs=V64[:, ci, h * 128:(h + 1) * 128], start=True, stop=True),
                                      reads=[K(KK64), K(V64)], writes=[K(ps[5])], inc=(h4 == 3))
                            hs = slice(hh * 4, (hh + 1) * 4)
                            fw.op("dve", lambda: nc.vector.tensor_tensor(out=S[:, hs, :], in0=S[:, hs, :],
                                                                         in1=EGE[:, hs, cg:cg + 1].to_broadcast([128, 4, 128]), op=ALU.mult),
                                  reads=[K(S, hh), K(EGE)], writes=[K(S, hh)])
                            fw.op("dve", lambda: nc.vector.tensor_tensor(out=S[:, hs, :], in0=S[:, hs, :],
                                                                         in1=ps[5][:, :].rearrange("p (h v) -> p h v", h=4), op=ALU.add),
                                  reads=[K(S, hh), K(ps[5])], writes=[K(S, hh)])
                            fw.op("act", lambda: nc.scalar.copy(out=Sb[:, hs, :], in_=S[:, hs, :]), reads=[K(S, hh)], writes=[K(Sb, hh)])
                        sgt, ok = SGt[cg % 2], OTK[cg % 2]
                        fw.op("pool", lambda: nc.gpsimd.tensor_tensor(out=SQ[:], in0=HN[:], in1=HN[:], op=ALU.mult), reads=[K(HN)], writes=[K(SQ)])
                        fw.op("dve", lambda: nc.vector.tensor_reduce(out=MS[:], in_=SQ[:].rearrange("p (h d) -> p h d", h=8), axis=AX.X, op=ALU.add),
                              reads=[K(SQ)], writes=[K(MS)])
                        fw.op("dve", lambda: nc.vector.tensor_scalar(out=MS[:], in0=MS[:], scalar1=1.0 / 128, scalar2=1e-6, op0=ALU.mult, op1=ALU.add),
                              reads=[K(MS)], writes=[K(MS)])
                        fw.op("act", lambda: nc.scalar.sqrt(out=MS[:], in_=MS[:]), reads=[K(MS)], writes=[K(MS)])
                        fw.op("dve", lambda: nc.vector.reciprocal(out=MS[:], in_=MS[:]), reads=[K(MS)], writes=[K(MS)])
                        fw.op("dve", lambda: nc.vector.tensor_tensor(out=SQ[:].rearrange("p (h d) -> p h d", h=8),
                                                                     in0=HN[:].rearrange("p (h d) -> p h d", h=8),
                                                                     in1=MS[:].unsqueeze(2).to_broadcast([64, 8, 128]), op=ALU.mult),
                              reads=[K(HN), K(MS)], writes=[K(SQ)])
                        fw.op("pool", lambda: nc.gpsimd.tensor_tensor(out=SQ[:], in0=SQ[:], in1=NG[0:64, :], op=ALU.mult),
                              reads=[K(SQ), K(NG)], writes=[K(SQ)])
                        fw.op("dve", lambda: nc.vector.tensor_tensor(out=ok[:], in0=SQ[:], in1=sgt[:], op=ALU.mult),
                              reads=[K(SQ), K(sgt)], writes=[K(ok)])
                        mixer_tail(g, esB, l, X, Xname, XO, XOname, row0, cg, ok[:], WO, lng, lnb, tb_bufs, K(ok), rows=HL, xloaded=True)
            fw.barrier()


MIXERS[2] = phase_hgrn


N_CORES = 8
MOE_B = 512
W_NAMES = [n for n in WEIGHT_SHAPES]


def full_plan():
    plan = []
    cur = "x"
    for l in range(4):
        plan.append(("mixer", l, cur, "xa"))
        dst = "out" if l == 3 else "xb"
        plan.append(("moe", l, "xa", dst))
        cur = dst
    return plan


def run_forward(inputs, n_cores=N_CORES, seq_per_core=4):
    x = np.ascontiguousarray(np.asarray(inputs["x"], dtype=np.float32))
    NT = seq_per_core * T
    nc, g = build(NT, MOE_B, full_plan(), W_NAMES)
    cst = make_consts(MOE_B)
    nsac = make_nsa_consts()
    weights = {n: np.ascontiguousarray(np.asarray(inputs[n], dtype=np.float32)) for n in W_NAMES}
    in_maps = []
    for c in range(n_cores):
        m = dict(weights)
        m["x"] = x[c * seq_per_core:(c + 1) * seq_per_core].reshape(NT, D)
        m["cst"] = cst
        m["nsac"] = nsac
        in_maps.append(m)
    res = run_bass_kernel_spmd(nc, in_maps, core_ids=list(range(n_cores)))
    outs = [np.asarray(r["out"]).reshape(seq_per_core, T, D) for r in res.results]
    return np.concatenate(outs, axis=0).astype(np.float32)


def kernel(**inputs):
    return run_forward(inputs)
```
